# Optimizing a Trainium2 kernel written in Bass

```python
import math
import jax, jax.numpy as jnp
from jax import lax
import numpy as np

D_MODEL = 1024
BATCH = 16
SEQ = 2048
DEPTH = 4

GRID_W = 64
CTX_LEN = 256
HEAD_DIM = 64
ROPE_THETA = 10000.0
Q_BLOCK = 128
CHUNK = 128
EPS = 1e-6
HALF_W = D_MODEL // 2
GQA_HEADS = HALF_W // HEAD_DIM
GQA_KV_HEADS = GQA_HEADS // 4
DIFF_HEADS = HALF_W // (2 * HEAD_DIM)
RET_HEADS = HALF_W // (2 * HEAD_DIM)
RET_QK_DIM = HEAD_DIM
RET_V_DIM = 2 * HEAD_DIM
SSD_HEAD_DIM = HEAD_DIM
SSD_HEADS = HALF_W // SSD_HEAD_DIM
SSD_GROUPS = 2
SSD_STATE = 128
SSD_CONV = 3
SSD_INNER = SSD_HEADS * SSD_HEAD_DIM
SSD_XBC = SSD_INNER + 2 * SSD_GROUPS * SSD_STATE
N_EXPERTS = 16
EC_CAPACITY = 2
EXPERT_FF = ((8 * D_MODEL // 3 + 255) // 256) * 256
N_EVEN = (DEPTH + 1) // 2
N_ODD = DEPTH // 2

ATT_SPLITS = (GQA_HEADS * HEAD_DIM, GQA_KV_HEADS * HEAD_DIM, GQA_KV_HEADS * HEAD_DIM,
              DIFF_HEADS * 2 * HEAD_DIM, DIFF_HEADS * 2 * HEAD_DIM, DIFF_HEADS * 2 * HEAD_DIM)
ATT_IN = sum(ATT_SPLITS)
REC_SPLITS = (RET_HEADS * RET_QK_DIM, RET_HEADS * RET_QK_DIM, RET_HEADS * RET_V_DIM,
              RET_HEADS * RET_V_DIM, SSD_INNER, SSD_XBC, 2 * SSD_HEADS)
REC_IN = sum(REC_SPLITS)

kernel_name = "hybrid_dit_prefix_gqa_diff_ret_ssd_ecmoe"


def _split(z, sizes):
    idx = np.cumsum(sizes)[:-1].tolist()
    return jnp.split(z, idx, axis=-1)


def _rmsnorm(x, w=None):
    xf = x.astype(jnp.float32)
    y = xf * lax.rsqrt(jnp.mean(xf * xf, axis=-1, keepdims=True) + EPS)
    if w is not None:
        y = y * w
    return y.astype(x.dtype)


def _modulate(x, w, shift, scale):
    return _rmsnorm(x, w) * (1 + scale) + shift


def _axial_rope_tables(n):
    t = jnp.arange(n)
    row = (t // GRID_W).astype(jnp.float32)
    col = (t % GRID_W).astype(jnp.float32)
    n_freq = HEAD_DIM // 4
    inv = ROPE_THETA ** (-jnp.arange(n_freq, dtype=jnp.float32) / n_freq)
    ang = jnp.concatenate([row[:, None] * inv, col[:, None] * inv], axis=-1)
    return jnp.cos(ang), jnp.sin(ang)


def _rope(x, cos, sin):
    shape = (cos.shape[0],) + (1,) * (x.ndim - 3) + (cos.shape[1],)
    c, s = cos.reshape(shape), sin.reshape(shape)
    x1, x2 = jnp.split(x, 2, axis=-1)
    return jnp.concatenate([x1 * c - x2 * s, x2 * c + x1 * s], axis=-1).astype(x.dtype)


def _attend(q, k, v, shared_k):
    scale = q.shape[-1] ** -0.5
    k_sub = 'bkhd' if shared_k else 'bkhmd'

    def one_block(qb):
        s = jnp.einsum(f'bqhmd,{k_sub}->bhmqk', qb, k, preferred_element_type=jnp.float32) * scale
        p = jax.nn.softmax(s, axis=-1).astype(v.dtype)
        return jnp.einsum('bhmqk,bkhv->bqhmv', p, v)

    B, Sq = q.shape[:2]
    nb = Sq // Q_BLOCK
    qb = jnp.moveaxis(q.reshape(B, nb, Q_BLOCK, *q.shape[2:]), 1, 0)
    out = lax.map(one_block, qb)
    return jnp.moveaxis(out, 0, 1).reshape(B, Sq, *out.shape[3:])


def _chunk_scan(q, k, v, log_a, h0):
    B, S = q.shape[:2]
    n = S // CHUNK
    tril = jnp.tril(jnp.ones((CHUNK, CHUNK), dtype=bool))

    def to_chunks(a):
        return jnp.moveaxis(a.reshape(B, n, CHUNK, *a.shape[2:]), 1, 0)

    def step(h, inp):
        qc, kc, vc, lac = inp
        cum = jnp.cumsum(lac.astype(jnp.float32), axis=1)
        cum_t = jnp.moveaxis(cum, 1, -1)
        seg = jnp.exp(jnp.where(tril, cum_t[..., :, None] - cum_t[..., None, :], -jnp.inf))
        qk = jnp.einsum('btgk,bsgk->bgts', qc, kc)
        y = (jnp.einsum('bgts,bgrts,bsgrv->btgrv', qk, seg, vc)
             + jnp.einsum('btgk,bgrkv->btgrv', qc, h) * jnp.exp(cum)[..., None])
        last = cum[:, -1]
        h_new = (h * jnp.exp(last)[..., None, None]
                 + jnp.einsum('bsgk,bsgr,bsgrv->bgrkv', kc, jnp.exp(last[:, None] - cum), vc))
        return h_new, y.astype(v.dtype)

    h, ys = lax.scan(step, h0, tuple(to_chunks(a) for a in (q, k, v, log_a)))
    return jnp.moveaxis(ys, 0, 1).reshape(B, S, *ys.shape[3:]), h


def _prefix_scan(lat, ctx, reverse):
    if reverse:
        lat = tuple(jnp.flip(a, 1) for a in lat)
        ctx = tuple(jnp.flip(a, 1) for a in ctx)
    q, v = ctx[0], ctx[2]
    h0 = jnp.zeros((q.shape[0], q.shape[2], v.shape[3], q.shape[3], v.shape[4]), jnp.float32)
    y_c, h_c = _chunk_scan(*ctx, h0)
    y_l, _ = _chunk_scan(*lat, h_c)
    if reverse:
        y_l, y_c = jnp.flip(y_l, 1), jnp.flip(y_c, 1)
    return y_l, y_c


def _bidir(lat_f, ctx_f, lat_b, ctx_b):
    yl_f, yc_f = _prefix_scan(lat_f, ctx_f, False)
    yl_b, yc_b = _prefix_scan(lat_b, ctx_b, True)
    return yl_f + yl_b, yc_f + yc_b


def _dwconv(x, w, b):
    K = w.shape[0]
    out = lax.conv_general_dilated(x, w[:, None, :].astype(x.dtype), window_strides=(1,),
                                   padding=[(K // 2, K // 2)],
                                   dimension_numbers=('NWC', 'WIO', 'NWC'),
                                   feature_group_count=x.shape[-1])
    return out + b


def _attention_mixer(hl, hc, cos, sin, w_in, w_out, q_norm_w, k_norm_w, lam, diff_norm_w,
                     lambda_init, need_ctx):
    def project(h, use_rope):
        B, n, _ = h.shape
        aq, ak, av, bq, bk, bv = _split(h @ w_in, ATT_SPLITS)
        aq = _rmsnorm(aq.reshape(B, n, GQA_HEADS, HEAD_DIM), q_norm_w)
        ak = _rmsnorm(ak.reshape(B, n, GQA_KV_HEADS, HEAD_DIM), k_norm_w)
        bq = bq.reshape(B, n, DIFF_HEADS, 2, HEAD_DIM)
        bk = bk.reshape(B, n, DIFF_HEADS, 2, HEAD_DIM)
        if use_rope:
            aq, ak, bq, bk = (_rope(t, cos, sin) for t in (aq, ak, bq, bk))
        aq = aq.reshape(B, n, GQA_KV_HEADS, GQA_HEADS // GQA_KV_HEADS, HEAD_DIM)
        av = av.reshape(B, n, GQA_KV_HEADS, HEAD_DIM)
        bv = bv.reshape(B, n, DIFF_HEADS, 2 * HEAD_DIM)
        return (aq, bq), (ak, av, bk, bv)

    lat_q, lat_kv = project(hl, True)
    ctx_q, ctx_kv = project(hc, False)
    lamf = lam.astype(jnp.float32)
    lam_val = jnp.exp(jnp.sum(lamf[0] * lamf[1])) - jnp.exp(jnp.sum(lamf[2] * lamf[3])) + lambda_init

    def mix(q_side, kv):
        aq, bq = q_side
        ak, av, bk, bv = kv
        B, n = aq.shape[:2]
        a = _attend(aq, ak, av, True).reshape(B, n, -1)
        o = _attend(bq, bk, bv, False)
        o = o[..., 0, :] - lam_val.astype(o.dtype) * o[..., 1, :]
        b = (_rmsnorm(o, diff_norm_w) * (1 - lambda_init)).reshape(B, n, -1)
        return jnp.concatenate([a, b], axis=-1) @ w_out

    kv_all = tuple(jnp.concatenate([l, c], axis=1) for l, c in zip(lat_kv, ctx_kv))
    yl = mix(lat_q, kv_all)
    yc = mix(ctx_q, ctx_kv) if need_ctx else None
    return yl, yc


def _recurrent_mixer(hl, hc, cos, sin, w_in, w_out, ret_decay_logit, conv_w, conv_b, dt_bias,
                     a_log, d_skip, ssd_norm_w, need_ctx):
    log_gamma = jax.nn.log_sigmoid(ret_decay_logit.astype(jnp.float32))
    a_neg = -jnp.exp(a_log.astype(jnp.float32))
    r_per_g = SSD_HEADS // SSD_GROUPS

    def project(h, use_rope):
        B, n, _ = h.shape
        rq, rk, rv, rg, z, xbc, dt = _split(h @ w_in, REC_SPLITS)
        rq = rq.reshape(B, n, RET_HEADS, RET_QK_DIM)
        rk = rk.reshape(B, n, RET_HEADS, RET_QK_DIM) * RET_QK_DIM ** -0.5
        if use_rope:
            rq, rk = _rope(rq, cos, sin), _rope(rk, cos, sin)
        rv = rv.reshape(B, n, RET_HEADS, 1, RET_V_DIM)
        ret = [(rq, rk, rv, jnp.broadcast_to(log_gamma[d][:, None], (B, n, RET_HEADS, 1)))
               for d in (0, 1)]
        xbc = jax.nn.silu(_dwconv(xbc, conv_w, conv_b))
        xs, bm, cm = _split(xbc, (SSD_INNER, SSD_GROUPS * SSD_STATE, SSD_GROUPS * SSD_STATE))
        xs = xs.reshape(B, n, SSD_HEADS, SSD_HEAD_DIM)
        bm = bm.reshape(B, n, SSD_GROUPS, SSD_STATE)
        cm = cm.reshape(B, n, SSD_GROUPS, SSD_STATE)
        dt = jax.nn.softplus(dt.reshape(B, n, 2, SSD_HEADS).astype(jnp.float32) + dt_bias)
        ssd = [(cm, bm,
                (xs * dt[:, :, d, :, None].astype(xs.dtype)).reshape(B, n, SSD_GROUPS, r_per_g, SSD_HEAD_DIM),
                (dt[:, :, d] * a_neg[d]).reshape(B, n, SSD_GROUPS, r_per_g))
               for d in (0, 1)]
        return ret, ssd, rg, z, xs

    lat_ret, lat_ssd, lat_rg, lat_z, lat_xs = project(hl, True)
    ctx_ret, ctx_ssd, ctx_rg, ctx_z, ctx_xs = project(hc, False)
    ret_l, ret_c = _bidir(lat_ret[0], ctx_ret[0], lat_ret[1], ctx_ret[1])
    ssd_l, ssd_c = _bidir(lat_ssd[0], ctx_ssd[0], lat_ssd[1], ctx_ssd[1])

    def combine(ret_y, ssd_y, rg, z, xs):
        B, n = rg.shape[:2]
        r = _rmsnorm(ret_y.reshape(B, n, RET_HEADS, RET_V_DIM)).reshape(B, n, -1) * jax.nn.silu(rg)
        s = ssd_y.reshape(B, n, SSD_HEADS, SSD_HEAD_DIM) + d_skip[:, None] * xs
        s = s.reshape(B, n, -1) * jax.nn.silu(z)
        s = _rmsnorm(s.reshape(B, n, SSD_GROUPS, -1)).reshape(B, n, -1) * ssd_norm_w
        return jnp.concatenate([r, s], axis=-1) @ w_out

    yl = combine(ret_l, ssd_l, lat_rg, lat_z, lat_xs)
    yc = combine(ret_c, ssd_c, ctx_rg, ctx_z, ctx_xs) if need_ctx else None
    return yl, yc


def _expert_choice_ffn(h, router_w, w_gate, w_up, w_down):
    B, n, D = h.shape
    cap = EC_CAPACITY * n // N_EXPERTS
    aff = jax.nn.softmax((h @ router_w).astype(jnp.float32), axis=-1)
    g, idx = lax.top_k(jnp.swapaxes(aff, 1, 2), cap)
    xin = jax.vmap(lambda xs, i: xs[i])(h, idx)
    a = jnp.einsum('becd,edf->becf', xin, w_gate)
    u = jnp.einsum('becd,edf->becf', xin, w_up)
    y = jnp.einsum('becf,efd->becd', jax.nn.silu(a) * u, w_down) * g[..., None].astype(h.dtype)
    return jax.vmap(lambda val, i: jax.ops.segment_sum(val.reshape(-1, D), i.reshape(-1),
                                                       num_segments=n))(y, idx)


def setup_inputs(seed: int = 0) -> dict:
    key = jax.random.key(seed)
    keys = jax.random.split(key, 32)
    f32 = jnp.float32
    D = D_MODEL

    def nrm(i, shape, scale):
        return jax.random.normal(keys[i], shape, f32) * scale

    ret_init = jnp.log(2.0 ** (5.0 + jnp.arange(RET_HEADS, dtype=f32)) - 1.0)
    dt0 = jnp.exp(jax.random.uniform(keys[17], (N_ODD, 2, SSD_HEADS), f32,
                                     math.log(1e-3), math.log(1e-1)))
    return {
        "x": nrm(0, (BATCH, SEQ, D), 1.0),
        "c": nrm(1, (BATCH, D), 1.0),
        "ctx": nrm(2, (BATCH, CTX_LEN, D), 1.0),
        "c_ctx": nrm(3, (D,), 1.0),
        "ada_w": nrm(4, (DEPTH, D, 6 * D), 0.5 * D ** -0.5),
        "ada_b": nrm(5, (DEPTH, 6 * D), 0.02),
        "norm1_w": 1.0 + nrm(6, (DEPTH, D), 0.02),
        "norm2_w": 1.0 + nrm(7, (DEPTH, D), 0.02),
        "att_w_in": nrm(8, (N_EVEN, D, ATT_IN), D ** -0.5),
        "att_w_out": nrm(9, (N_EVEN, 2 * HALF_W, D), (2 * HALF_W) ** -0.5),
        "att_q_norm_w": 1.0 + nrm(10, (N_EVEN, HEAD_DIM), 0.02),
        "att_k_norm_w": 1.0 + nrm(11, (N_EVEN, HEAD_DIM), 0.02),
        "diff_lambda": nrm(12, (N_EVEN, 4, HEAD_DIM), 0.1),
        "diff_norm_w": 1.0 + nrm(13, (N_EVEN, 2 * HEAD_DIM), 0.02),
        "rec_w_in": nrm(14, (N_ODD, D, REC_IN), D ** -0.5),
        "rec_w_out": nrm(15, (N_ODD, 2 * HALF_W, D), (2 * HALF_W) ** -0.5),
        "ret_decay_logit": ret_init + nrm(16, (N_ODD, 2, RET_HEADS), 0.05),
        "ssd_conv_w": nrm(18, (N_ODD, SSD_CONV, SSD_XBC), SSD_CONV ** -0.5),
        "ssd_conv_b": nrm(19, (N_ODD, SSD_XBC), 0.02),
        "ssd_dt_bias": dt0 + jnp.log(-jnp.expm1(-dt0)),
        "ssd_a_log": jnp.log(jax.random.uniform(keys[20], (N_ODD, 2, SSD_HEADS), f32, 1.0, 16.0)),
        "ssd_d_skip": 1.0 + nrm(21, (N_ODD, SSD_HEADS), 0.1),
        "ssd_norm_w": 1.0 + nrm(22, (N_ODD, SSD_INNER), 0.02),
        "router_w": nrm(23, (DEPTH, D, N_EXPERTS), D ** -0.5),
        "expert_w_gate": nrm(24, (DEPTH, N_EXPERTS, D, EXPERT_FF), D ** -0.5),
        "expert_w_up": nrm(25, (DEPTH, N_EXPERTS, D, EXPERT_FF), D ** -0.5),
        "expert_w_down": nrm(26, (DEPTH, N_EXPERTS, EXPERT_FF, D), EXPERT_FF ** -0.5),
        "final_norm_w": 1.0 + nrm(27, (D,), 0.02),
    }


def reference(x, c, ctx, c_ctx, ada_w, ada_b, norm1_w, norm2_w, att_w_in, att_w_out,
              att_q_norm_w, att_k_norm_w, diff_lambda, diff_norm_w, rec_w_in, rec_w_out,
              ret_decay_logit, ssd_conv_w, ssd_conv_b, ssd_dt_bias, ssd_a_log, ssd_d_skip,
              ssd_norm_w, router_w, expert_w_gate, expert_w_up, expert_w_down, final_norm_w):
    n_lat = x.shape[1]
    cos, sin = _axial_rope_tables(n_lat)
    cl, cc = jax.nn.silu(c), jax.nn.silu(c_ctx)
    xl, xc = x, ctx
    for layer in range(DEPTH):
        need_ctx = layer < DEPTH - 1
        i = layer // 2
        mod_l = jnp.split((cl @ ada_w[layer] + ada_b[layer])[:, None, :], 6, axis=-1)
        mod_c = jnp.split(cc @ ada_w[layer] + ada_b[layer], 6, axis=-1)
        hl = _modulate(xl, norm1_w[layer], mod_l[0], mod_l[1])
        hc = _modulate(xc, norm1_w[layer], mod_c[0], mod_c[1])
        if layer % 2 == 0:
            lambda_init = 0.8 - 0.6 * math.exp(-0.3 * layer)
            yl, yc = _attention_mixer(hl, hc, cos, sin, att_w_in[i], att_w_out[i], att_q_norm_w[i],
                                      att_k_norm_w[i], diff_lambda[i], diff_norm_w[i],
                                      lambda_init, need_ctx)
        else:
            yl, yc = _recurrent_mixer(hl, hc, cos, sin, rec_w_in[i], rec_w_out[i],
                                      ret_decay_logit[i], ssd_conv_w[i], ssd_conv_b[i],
                                      ssd_dt_bias[i], ssd_a_log[i], ssd_d_skip[i], ssd_norm_w[i],
                                      need_ctx)
        xl = xl + mod_l[2] * yl
        hl = _modulate(xl, norm2_w[layer], mod_l[3], mod_l[4])
        xl = xl + mod_l[5] * _expert_choice_ffn(hl, router_w[layer], expert_w_gate[layer],
                                                expert_w_up[layer], expert_w_down[layer])
        if need_ctx:
            xc = xc + mod_c[2] * yc
            hc = _modulate(xc, norm2_w[layer], mod_c[3], mod_c[4])
            xc = xc + mod_c[5] * _expert_choice_ffn(hc, router_w[layer], expert_w_gate[layer],
                                                    expert_w_up[layer], expert_w_down[layer])
    return _rmsnorm(xl, final_norm_w)
```

```python
import math
import contextlib
import numpy as np
import concourse.bass as bass
import concourse.mybir as mybir
from concourse.bass_utils import run_bass_kernel_spmd

F32 = mybir.dt.float32
BF16 = mybir.dt.bfloat16
AF = mybir.ActivationFunctionType
ALU = mybir.AluOpType
AX = mybir.AxisListType
NEG = -30000.0
EPS = 1e-6


class Cfg:
    def __init__(self, BATCH=16, SEQ=2048, DEPTH=4, CTX=256, FF=2816, SPC=2, NE=16):
        self.D = 1024
        self.BATCH, self.NL, self.DEPTH, self.NC, self.FF, self.SPC, self.NE = BATCH, SEQ, DEPTH, CTX, FF, SPC, NE
        self.T = SEQ + CTX
        self.NCORES = BATCH // SPC
        self.GRID_W = 64
        self.KD = 8
        self.ATT_IN = 2304
        self.REC_IN = 3088
        self.capl = 2 * SEQ // NE
        self.capc = 2 * CTX // NE
        self.NLB = SEQ // 128
        self.NCB = CTX // 128
        self.NB = self.NLB + self.NCB
        self.tiles = []
        for c0 in range(0, SEQ, 512):
            self.tiles.append((c0, min(512, SEQ - c0), 0))
        for c0 in range(0, CTX, 512):
            self.tiles.append((SEQ + c0, min(512, CTX - c0), 1))
        self.FC = FF // 128
        self.G = min(SPC, 2)
        self.gcols = self.G * (self.capl + self.capc)
        self.cols = (SPC // self.G) * self.gcols


class Buf:
    __slots__ = ("w", "r")

    def __init__(self):
        self.w = None
        self.r = {}


class Sched:
    ENG = ("pe", "act", "dve", "pool", "sp")
    LIMIT = 20000
    NS = 8

    def __init__(self, nc, es):
        self.nc, self.es = nc, es
        self.E = {"pe": nc.tensor, "act": nc.scalar, "dve": nc.vector, "pool": nc.gpsimd, "sp": nc.sync}
        self.sems = {}
        self.cnt = {e: 0 for e in self.ENG}
        self.epoch = {e: 0 for e in self.ENG}
        self.pending = {e: False for e in self.ENG}
        self.seen = {e: {} for e in self.ENG}
        self.latest = {}
        self.bufs = {}
        self.dmacnt = {e: 0 for e in self.ENG}
        self.nins = 0

    def semh(self, k):
        if k not in self.sems:
            self.sems[k] = self.es.enter_context(self.nc.semaphore("s_" + "_".join(str(x) for x in k)))
        return self.sems[k]

    PSUM_NAMES = {"acc", "ssp", "swp", "nm_ss", "modps", "Sps", "Ops", "Lps", "assp", "dacc", "lps", "tps", "rtp", "rsp", "gps",
                  "aps", "ups", "cps", "yps", "stp", "syT", "rS", "rO", "otp"}

    def _split(self, r, w):
        r2, w2 = [], list(w)
        for key in r:
            name = key if isinstance(key, str) else key[0]
            if name in self.PSUM_NAMES:
                if key not in w2:
                    w2.append(key)
            else:
                r2.append(key)
        return r2, w2

    def _need(self, eng, r, w):
        need = {}

        def req(tok):
            if tok is None:
                return
            k, v = tok
            if eng == "pe" and k[0] == "pe":
                return
            if need.get(k, 0) < v:
                need[k] = v
        for key in r:
            b = self.bufs.get(key)
            if b is None:
                b = self.bufs[key] = Buf()
            req(b.w)
        for key in w:
            b = self.bufs.get(key)
            if b is None:
                b = self.bufs[key] = Buf()
            req(b.w)
            for tok in b.r.values():
                req(tok)
        seen = self.seen[eng]
        for k, v in need.items():
            if seen.get(k, 0) < v:
                self.E[eng].wait_ge(self.semh(k), v)
                seen[k] = v
                self.nins += 1

    def _mark(self, tok, r, w):
        k, v = tok
        if self.latest.get(k, 0) < v:
            self.latest[k] = v
        for key in r:
            self.bufs[key].r[k] = tok
        for key in w:
            b = self.bufs[key]
            b.w = tok
            b.r = {}

    def skip(self):
        import os
        if not hasattr(self, "maxops"):
            self.maxops = int(os.environ.get("KOPS", "100000000"))
            self.opc = 0
        self.opc += 1
        import os
        if os.environ.get("KDBG") and abs(self.opc - self.maxops) < 8:
            import traceback
            fr = traceback.extract_stack()[-4]
            print("OP", self.opc, fr.lineno, fr.line)
        if self.opc == self.maxops:
            print("LAST OP before cutoff ^^^")
        return self.opc > self.maxops

    def op(self, eng, fn, r=(), w=(), inc=True):
        if self.skip():
            return
        r, w = self._split(r, w)
        self._need(eng, r, w)
        ins = fn()
        self.nins += 1
        k = (eng, self.epoch[eng])
        if inc:
            self.cnt[eng] += 1
            ins.then_inc(self.semh(k), 1)
            self.pending[eng] = False
            tok = (k, self.cnt[eng])
        else:
            self.pending[eng] = True
            tok = (k, self.cnt[eng] + 1)
        self._mark(tok, r, w)
        if inc and self.cnt[eng] >= self.LIMIT:
            self.epoch[eng] += 1
            self.cnt[eng] = 0

    def dma(self, q, out, in_, r=(), w=()):
        if self.skip():
            return
        r, w = self._split(r, w)
        self._need(q, r, w)
        j = self.dmacnt[q]
        self.dmacnt[q] += 1
        slot, rnd = j % self.NS, j // self.NS
        k = ("dma", q, slot)
        if rnd > 0 and self.seen[q].get(k, 0) < 16 * rnd:
            self.E[q].wait_ge(self.semh(k), 16 * rnd)
            self.seen[q][k] = 16 * rnd
        self.E[q].dma_start(out=out, in_=in_, allow_slow_non_contiguous=True).then_inc(self.semh(k), 16)
        self.nins += 1
        self._mark((k, 16 * (rnd + 1)), r, w)

    def barrier(self):
        for e in self.ENG:
            assert not self.pending[e], e
        for e in self.ENG:
            seen = self.seen[e]
            for k, v in self.latest.items():
                if e == "pe" and k[0] == "pe":
                    continue
                if seen.get(k, 0) < v:
                    self.E[e].wait_ge(self.semh(k), v)
                    seen[k] = v
                    self.nins += 1
        self.bufs = {}


def _consts(cfg):
    NL, NC, T = cfg.NL, cfg.NC, cfg.T
    c = {}
    c["ones"] = np.ones((128, 128), np.float32)
    bo = np.zeros((128, 128), np.float32)
    bo[:64, :64] = 1
    bo[64:, 64:] = 1
    c["blk64"] = bo
    c["ident"] = np.eye(128, dtype=np.float32)
    t = np.arange(NL)
    row = (t // cfg.GRID_W).astype(np.float32)
    col = (t % cfg.GRID_W).astype(np.float32)
    inv = (10000.0 ** (-np.arange(16, dtype=np.float32) / 16)).astype(np.float32)
    ang = np.concatenate([row[:, None] * inv, col[:, None] * inv], -1).astype(np.float32)
    cos, sin = np.cos(ang).astype(np.float32), np.sin(ang).astype(np.float32)
    C = np.ones((128, T), np.float32)
    S = np.zeros((128, T), np.float32)
    for p in range(128):
        i = p % 32
        C[p, :NL] = cos[:, i]
        S[p, :NL] = -sin[:, i] if (p % 64) < 32 else sin[:, i]
    c["ropeC"], c["ropeS"] = C, S
    perm = np.zeros((128, 128), np.float32)
    for m in range(128):
        k = m + 32 if (m % 64) < 32 else m - 32
        perm[k, m] = 1
    c["perm"] = perm
    TF = np.concatenate([NC + np.arange(NL), np.arange(NC)]).astype(np.float32)
    TB = np.concatenate([NC + (NL - 1 - np.arange(NL)), NC - 1 - np.arange(NC)]).astype(np.float32)
    c["tfrow"] = np.broadcast_to(TF[None, :], (128, T)).copy()
    c["tbrow"] = np.broadcast_to(TB[None, :], (128, T)).copy()
    c["tfcol"] = TF.reshape(cfg.NB, 128).T.copy()
    c["tbcol"] = TB.reshape(cfg.NB, 128).T.copy()
    mF = np.zeros((128, 4, 512), np.float32)
    mB = np.zeros((128, 4, 512), np.float32)
    s = np.arange(128)[:, None]
    tt = np.arange(512)[None, :]
    for j in range(4):
        mF[:, j, :] = np.where(128 * j + s <= tt, 0.0, NEG)
        mB[:, j, :] = np.where(128 * j + s >= tt, 0.0, NEG)
    c["maskF"], c["maskB"] = mF, mB
    c["iota"] = np.broadcast_to(np.arange(256, dtype=np.float32)[None, :], (128, 256)).copy()
    us = np.zeros((128, 128), np.float32)
    for m in range(128):
        us[:m, m] = 1
    c["ustrict"] = us
    return c


class Prog:
    def __init__(self, cfg):
        self.cfg = cfg
        self.nc = bass.Bass("TRN2", target_bir_lowering=False)
        self.es = contextlib.ExitStack()
        self.s = Sched(self.nc, self.es)
        self.dr = {}
        self.uid = 0

    def din(self, name, shape, dt=F32):
        self.dr[name] = self.nc.dram_tensor(name, list(shape), dt, kind="ExternalInput").ap()
        return self.dr[name]

    def dscr(self, name, shape, dt):
        self.dr[name] = self.nc.dram_tensor(name, list(shape), dt, kind="Internal").ap()
        return self.dr[name]

    def sb(self, st, name, shape, dt):
        self.uid += 1
        return st.enter_context(self.nc.sbuf_tensor(f"{name}_{self.uid}", list(shape), dt))

    def ps(self, st, name, shape, dt=F32):
        self.uid += 1
        full = 512 if dt == F32 else 1024
        t = st.enter_context(self.nc.psum_tensor(f"{name}_{self.uid}", [128, full], dt))
        n = 1
        for d in shape[1:]:
            n *= d
        v = t[:shape[0], :n]
        if len(shape) == 3:
            v = v.rearrange("p (a b) -> p a b", b=shape[2])
        return v

    def mm(self, out, lhsT, rhs, start, stop, r, w, inc=None):
        if inc is None:
            inc = stop
        if self.s.__dict__.get("maxops", 10**9) < 10**8 and not stop:
            inc = True
        self.s.op("pe", lambda: self.nc.tensor.matmul(out, lhsT, rhs, start=start, stop=stop), r=r, w=w, inc=inc)

    def tr(self, out, in_, ident, r, w, inc=True):
        self.s.op("pe", lambda: self.nc.tensor.transpose(out, in_, ident), r=r, w=w, inc=inc)

    def act(self, out, in_, func, r, w, bias=None, scale=None, accum_out=None):
        kw = {}
        if bias is not None:
            kw["bias"] = bias
        if scale is not None:
            kw["scale"] = scale
        if accum_out is not None:
            kw["accum_out"] = accum_out
        self.s.op("act", lambda: self.nc.scalar.activation(out=out, in_=in_, func=func, **kw), r=r, w=w)

    def ts(self, out, in0, s1, s2, op0, op1=None, r=(), w=(), eng="dve"):
        e = self.nc.vector if eng == "dve" else self.nc.gpsimd
        if op1 is None:
            self.s.op(eng, lambda: e.tensor_scalar(out=out, in0=in0, scalar1=s1, scalar2=None, op0=op0), r=r, w=w)
        else:
            self.s.op(eng, lambda: e.tensor_scalar(out=out, in0=in0, scalar1=s1, scalar2=s2, op0=op0, op1=op1), r=r, w=w)

    def tt(self, out, in0, in1, op, r, w, eng="dve"):
        e = self.nc.vector if eng == "dve" else self.nc.gpsimd
        self.s.op(eng, lambda: e.tensor_tensor(out=out, in0=in0, in1=in1, op=op), r=r, w=w)

    def stt(self, out, in0, scalar, in1, op0, op1, r, w, eng="dve"):
        e = self.nc.vector if eng == "dve" else self.nc.gpsimd
        self.s.op(eng, lambda: e.scalar_tensor_tensor(out=out, in0=in0, scalar=scalar, in1=in1, op0=op0, op1=op1), r=r, w=w)

    def cp(self, out, in_, r, w, eng="dve"):
        if eng == "act":
            self.s.op("act", lambda: self.nc.scalar.copy(out=out, in_=in_), r=r, w=w)
        else:
            e = self.nc.vector if eng == "dve" else self.nc.gpsimd
            self.s.op(eng, lambda: e.tensor_copy(out=out, in_=in_), r=r, w=w)

    def ld(self, out, in_, r, w, q="sp"):
        self.s.dma(q, out, in_, r=r, w=w)

    def load_const(self, st, name, shape, dt=F32, cast=False):
        t = self.sb(st, name, shape, BF16 if cast else dt)
        self.ld(t[:], self.dr[name][:], r=[("dram", name)], w=[("sb", t.name)], q="pool" if cast else "sp")
        return t

    def rstd(self, sq_list, lhsT, ps_ap, out_ap, addc, rsq, rps, rout):
        n = len(sq_list)
        for i, a in enumerate(sq_list):
            self.mm(ps_ap, lhsT, a, start=(i == 0), stop=(i == n - 1), r=rsq, w=rps)
        self.act(out_ap, ps_ap, AF.Sqrt, bias=self.epsc[:, self.epsidx[addc]:self.epsidx[addc] + 1], r=rps + ["epsc"], w=rout)
        self.recip(out_ap, out_ap, r=rout, w=rout)

    def declare(self):
        cfg = self.cfg
        D, T, SPC, L = cfg.D, cfg.T, cfg.SPC, cfg.DEPTH
        NV = SPC + 1
        NEV, NOD = (L + 1) // 2, max(L // 2, 1)
        d = self.din
        d("xT0", [SPC, D, T]); d("cT", [128, 8, NV]); d("ada_w", [L, D, 6 * D]); d("ada_bT", [128, L, 48])
        d("n1w", [128, L, 8]); d("n2w", [128, L, 8]); d("fnw", [128, 8])
        d("att_w_in", [NEV, D, cfg.ATT_IN]); d("att_w_out", [NEV, D, D])
        d("qnw", [128, NEV]); d("knw", [128, NEV]); d("dlam", [128, NEV, 256]); d("dnw", [128, NEV])
        d("rec_w_in", [NOD, D, cfg.REC_IN]); d("rec_w_out", [NOD, D, D])
        d("rdl", [128, NOD, 8]); d("cwT", [128, NOD, 8, 3]); d("cbT", [128, NOD, 8])
        d("dtb", [16, NOD]); d("alog", [16, NOD]); d("alogr", [128, NOD, 16]); d("dsk", [128, NOD, 4]); d("snw", [128, NOD, 4])
        d("router_w", [L, D, cfg.NE]); d("wg", [L, cfg.NE, D, cfg.FF]); d("wu", [L, cfg.NE, D, cfg.FF]); d("wd", [L, cfg.NE, cfg.FF, D])
        for k, v in _consts(cfg).items():
            d(k, v.shape)
        self.out = self.nc.dram_tensor("outT", [SPC, D, cfg.NL], F32, kind="ExternalOutput").ap()
        self.dscr("xT", [SPC, D, T], F32)
        self.dscr("qk", [SPC, 16 * 128, T], BF16)
        self.dscr("aT", [SPC, D, T], BF16)
        self.dscr("rawT", [SPC, D, T], F32)
        self.dscr("sideT", [SPC, 12 * 128, T], F32)
        self.dscr("cumT", [SPC, 16, T], F32)
        self.dscr("thr", [2, cfg.NE], F32)
        self.dscr("slotd", [SPC, 128, cfg.NB * cfg.NE], F32)
        self.dscr("gmd", [SPC, 128, cfg.NB * cfg.NE], F32)
        self.dscr("xin", [cfg.NE, D, cfg.cols], BF16)
        self.dscr("yexp", [cfg.NE, cfg.cols, D], BF16)

    def col0(self, sm, seg):
        cfg = self.cfg
        base = (sm // cfg.G) * cfg.gcols
        sl = sm % cfg.G
        return base + (sl * cfg.capl if seg == 0 else cfg.G * cfg.capl + sl * cfg.capc)

    def P(self, l, kind, k, v):
        return self.par[:, l, kind * 8 + k, v:v + 1]

    def setup(self):
        cfg, s = self.cfg, self.s
        D, L, NV = cfg.D, cfg.DEPTH, cfg.SPC + 1
        pst = self.es
        self.par = self.sb(pst, "par", [128, L, 48, NV], F32)
        self.ones_bf = self.sb(pst, "ones_bf", [128, 128], BF16)
        self.ident_bf = self.sb(pst, "ident_bf", [128, 128], BF16)
        self.ident_f = self.sb(pst, "ident_f", [128, 128], F32)
        self.fnw = self.sb(pst, "fnws", [128, 8], F32)
        self.epsc = self.sb(pst, "epsc", [128, 4], F32)
        self.epsidx = {}
        for ii, val in enumerate((D * EPS, 64 * EPS, 128 * EPS, 256 * EPS)):
            self.epsidx[val] = ii
            self.s.op("dve", lambda: self.nc.vector.memset(self.epsc[:, ii:ii + 1], val), r=[], w=["epsc"])
        self.ld(self.ones_bf[:], self.dr["ones"][:], r=[], w=["ones_bf"], q="pool")
        self.ld(self.ident_bf[:], self.dr["ident"][:], r=[], w=["ident_bf"], q="pool")
        self.ld(self.ident_f[:], self.dr["ident"][:], r=[], w=["ident_f"])
        self.ld(self.fnw[:], self.dr["fnw"][:], r=[], w=["fnw"])
        self.ts(self.fnw[:], self.fnw[:], math.sqrt(D), None, ALU.mult, r=["fnw"], w=["fnw"])
        with contextlib.ExitStack() as st:
            cT = self.sb(st, "cT", [128, 8, NV], F32)
            clT = self.sb(st, "clT", [128, 8, NV], BF16)
            n1s = self.sb(st, "n1s", [128, L, 8], F32)
            n2s = self.sb(st, "n2s", [128, L, 8], F32)
            abT = self.sb(st, "abT", [128, L, 48], F32)
            wts = [self.sb(st, f"adaw{i}", [128, 8, 768], BF16) for i in range(2)]
            pss = [self.ps(st, f"modps{i}", [128, 6, NV]) for i in range(2)]
            self.ld(cT[:], self.dr["cT"][:], r=[], w=["cT"])
            self.ld(n1s[:], self.dr["n1w"][:], r=[], w=["n1s"])
            self.ld(n2s[:], self.dr["n2w"][:], r=[], w=["n2s"])
            self.ld(abT[:], self.dr["ada_bT"][:], r=[], w=["abT"])
            self.act(clT[:], cT[:], AF.Silu, r=["cT"], w=["clT"])
            self.ts(n1s[:], n1s[:], math.sqrt(D), None, ALU.mult, r=["n1s"], w=["n1s"])
            self.ts(n2s[:], n2s[:], math.sqrt(D), None, ALU.mult, r=["n2s"], w=["n2s"])
            it = 0
            for l in range(L):
                src = self.dr["ada_w"][l].rearrange("(k p) n -> p k n", p=128)
                for g in range(8):
                    wt, pt = wts[it % 2], pss[it % 2]
                    wk, pk = ("adaw", it % 2), ("modps", it % 2)
                    it += 1
                    for k in range(8):
                        self.ld(wt[:, k, :], src[:, k, g * 768:(g + 1) * 768], r=[], w=[wk], q="pool")
                    for jj in range(6):
                        for k in range(8):
                            self.mm(pt[:, jj, :], wt[:, k, jj * 128:(jj + 1) * 128], clT[:, k, :], start=(k == 0), stop=(k == 7),
                                    r=[wk, "clT"], w=[pk], inc=(k == 7))
                    for v in range(NV):
                        self.tt(self.par[:, l, g * 6:(g + 1) * 6, v], pt[:, :, v], abT[:, l, g * 6:(g + 1) * 6], ALU.add,
                                r=[pk, "abT"], w=["par"])
                for v in range(NV):
                    self.stt(self.par[:, l, 8:16, v], self.par[:, l, 8:16, v], 1.0, n1s[:, l, :], ALU.add, ALU.mult, r=["par", "n1s"], w=["par"])
                    self.stt(self.par[:, l, 32:40, v], self.par[:, l, 32:40, v], 1.0, n2s[:, l, :], ALU.add, ALU.mult, r=["par", "n2s"], w=["par"])
            for sm in range(cfg.SPC):
                self.ld(self.dr["xT"][sm], self.dr["xT0"][sm], r=[], w=[("xT", sm)])
            s.barrier()

    def norm_mod(self, xt, xkey, w, bufs, A_fn, B_fn, out_fn, outkey):
        sq, ssps, rs, tmp = bufs["sq"], bufs["ssps"], bufs["rs"], bufs["tmp"]
        D = self.cfg.D
        self.act(sq[:, :, :w], xt[:, :, :w], AF.Square, r=[xkey], w=["nm_sq"])
        self.rstd([sq[:, k, :w] for k in range(8)], self.ones_bf[:], ssps[:, :w], rs[:, :w], D * EPS, ["nm_sq", "ones_bf"], ["nm_ss"], ["nm_rs"])
        for k in range(8):
            if B_fn is None:
                self.stt(out_fn(k), xt[:, k, :w], A_fn(k), rs[:, :w], ALU.mult, ALU.mult, r=[xkey, "nm_rs", "par", "fnw"], w=[outkey])
            else:
                self.stt(tmp[:, k, :w], xt[:, k, :w], A_fn(k), rs[:, :w], ALU.mult, ALU.mult, r=[xkey, "nm_rs", "par"], w=[("nm_tmp", k)])
                self.act(out_fn(k), tmp[:, k, :w], AF.Identity, bias=B_fn(k), r=[("nm_tmp", k), "par"], w=[outkey])

    def nm_bufs(self, st):
        return {"sq": self.sb(st, "nm_sq", [128, 8, 512], BF16), "ssps": self.ps(st, "nm_ss", [128, 512]),
                "rs": self.sb(st, "nm_rs", [128, 512], F32), "tmp": self.sb(st, "nm_tmp", [128, 8, 512], F32)}

    def phase_norm1(self, st, l, sm):
        cfg = self.cfg
        hT = self.sb(st, "hT", [128, 8, cfg.T], BF16)
        with contextlib.ExitStack() as st2:
            nb = self.nm_bufs(st2)
            xts = [self.sb(st2, f"xt{i}", [128, 8, 512], F32) for i in range(2)]
            src = self.dr["xT"][sm].rearrange("(k p) t -> p k t", p=128)
            for i, (c0, w, seg) in enumerate(cfg.tiles):
                xt, xk = xts[i % 2], ("xt", i % 2)
                v = cfg.SPC if seg == 1 else sm
                self.ld(xt[:, :, :w], src[:, :, c0:c0 + w], r=[("xT", sm)], w=[xk])
                self.norm_mod(xt, xk, w, nb, lambda k: self.P(l, 1, k, v), lambda k: self.P(l, 0, k, v),
                              lambda k: hT[:, k, c0:c0 + w], ("hT", i))
            self.s.barrier()
        return hT

    def tile_of_block(self, b):
        for i, (c0, w, seg) in enumerate(self.cfg.tiles):
            if c0 <= b * 128 < c0 + w:
                return i
        raise ValueError(b)

    def phase_inproj(self, st, l, sm, hT, wname, widx, nin, rowspec, vspec, vt):
        cfg = self.cfg
        T = cfg.T
        with contextlib.ExitStack() as st2:
            wi = self.sb(st2, "wi", [128, 8, nin], BF16)
            src = self.dr[wname][widx].rearrange("(k p) n -> p k n", p=128)
            for k in range(8):
                self.ld(wi[:, k, :], src[:, k, :], r=[], w=[("wi", k)], q="pool")
            wkeys = [("wi", k) for k in range(8)]
            C = self.load_const(st2, "ropeC", [128, T])
            S = self.load_const(st2, "ropeS", [128, T])
            blk = self.load_const(st2, "blk64", [128, 128], cast=True)
            perm = self.load_const(st2, "perm", [128, 128], cast=True)
            ck = [("sb", C.name), ("sb", S.name), ("sb", blk.name), ("sb", perm.name)]
            acc = [self.ps(st2, f"acc{i}", [128, 512]) for i in range(2)]
            ssp = self.ps(st2, "ssp", [128, 512])
            swp = self.ps(st2, "swp", [128, 512])
            sqb = [self.sb(st2, f"sqb{i}", [128, 512], BF16) for i in range(2)]
            xwb = [self.sb(st2, f"xwb{i}", [128, 512], BF16) for i in range(2)]
            rsb = [self.sb(st2, f"rsb{i}", [128, 512], F32) for i in range(2)]
            t1b = [self.sb(st2, f"t1b{i}", [128, 512], F32) for i in range(2)]
            t2b = [self.sb(st2, f"t2b{i}", [128, 512], F32) for i in range(2)]
            yb = [self.sb(st2, f"yb{i}", [128, 512], F32) for i in range(2)]
            orow = [self.sb(st2, f"orow{i}", [128, T], BF16) for i in range(2)]
            needf = any(k_ not in ("normrope", "rope") for (k_, _, _, _) in rowspec)
            frow = [self.sb(st2, "frow0", [128, T] if needf else [128, 8], F32)] * 2
            grow = [self.sb(st2, "grow0", [128, T] if needf else [128, 8], F32)] * 2
            it = 0
            import os
            ksub = int(os.environ.get("KSUB", "999"))
            for ci, (kind, j, arg, dst) in enumerate(rowspec):
                if ci >= ksub:
                    break
                M = 16 if kind == "dt" else 128
                o, ok = orow[ci % 2], ("orow", ci % 2)
                fr, fk = frow[0], ("frow", 0)
                gr, gk = grow[0], ("grow", 0)
                for ti, (c0, w, seg) in enumerate(cfg.tiles):
                    a, ak = acc[it % 2], ("acc", it % 2)
                    q = it % 2
                    it += 1
                    for k in range(8):
                        self.mm(a[:M, :w], wi[:, k, j * 128:j * 128 + M], hT[:, k, c0:c0 + w], start=(k == 0), stop=(k == 7),
                                r=[wkeys[k], ("hT", ti)], w=[ak], inc=(k == 7))
                    if kind in ("normrope", "rope"):
                        if kind == "normrope":
                            self.act(sqb[q][:, :w], a[:, :w], AF.Square, r=[ak], w=[("sqb", q)])
                            self.ts(xwb[q][:, :w], a[:, :w], arg, None, ALU.mult, r=[ak, "pcols"], w=[("xwb", q)])
                            self.rstd([sqb[q][:, :w]], blk[:], ssp[:, :w], rsb[q][:, :w], 64 * EPS, [("sqb", q), ck[2]], ["ssp"], [("rsb", q)])
                        else:
                            self.ts(xwb[q][:, :w], a[:, :w], float(arg), None, ALU.mult, r=[ak], w=[("xwb", q)])
                        self.mm(swp[:, :w], perm[:], xwb[q][:, :w], start=True, stop=True, r=[("xwb", q), ck[3]], w=["swp"])
                        self.tt(t1b[q][:, :w], xwb[q][:, :w], C[:, c0:c0 + w], ALU.mult, r=[("xwb", q), ck[0]], w=[("t1b", q)])
                        self.tt(t2b[q][:, :w], swp[:, :w], S[:, c0:c0 + w], ALU.mult, r=["swp", ck[1]], w=[("t2b", q)])
                        if kind == "normrope":
                            self.tt(yb[q][:, :w], t1b[q][:, :w], t2b[q][:, :w], ALU.add, r=[("t1b", q), ("t2b", q)], w=[("yb", q)], eng="dve")
                            self.stt(o[:, c0:c0 + w], yb[q][:, :w], 8.0, rsb[q][:, :w], ALU.mult, ALU.mult, r=[("yb", q), ("rsb", q)], w=[ok])
                        else:
                            self.tt(o[:, c0:c0 + w], t1b[q][:, :w], t2b[q][:, :w], ALU.add, r=[("t1b", q), ("t2b", q)], w=[ok], eng="dve")
                    else:
                        self.cp(fr[:M, c0:c0 + w], a[:M, :w], r=[ak], w=[fk], eng="act")
                if kind in ("normrope", "rope"):
                    self.ld(self.dr["qk"][sm, dst * 128:(dst + 1) * 128, :], o[:], r=[ok], w=[("qk", sm, dst)])
                elif kind == "side":
                    self.ld(self.dr["sideT"][sm, dst * 128:(dst + 1) * 128, :], fr[:], r=[fk], w=[("sideT", sm, dst)])
                elif kind == "conv":
                    cw, cb, cidx = arg
                    self.act(gr[:], fr[:], AF.Identity, bias=cb[:, cidx:cidx + 1], scale=cw[:, cidx, 1:2], r=[fk, "pcols"], w=[gk])
                    for (s0, n) in ((0, cfg.NL), (cfg.NL, cfg.NC)):
                        self.stt(gr[:, s0 + 1:s0 + n], fr[:, s0:s0 + n - 1], cw[:, cidx, 0:1], gr[:, s0 + 1:s0 + n], ALU.mult, ALU.add, r=[fk, gk, "pcols"], w=[gk])
                        self.stt(gr[:, s0:s0 + n - 1], fr[:, s0 + 1:s0 + n], cw[:, cidx, 2:3], gr[:, s0:s0 + n - 1], ALU.mult, ALU.add, r=[fk, gk, "pcols"], w=[gk])
                    if dst[0] == "xs":
                        self.act(fr[:], gr[:], AF.Silu, r=[gk], w=[fk])
                        self.ld(self.dr["sideT"][sm, dst[1] * 128:(dst[1] + 1) * 128, :], fr[:], r=[fk], w=[("sideT", sm, dst[1])])
                        for b in range(cfg.NB):
                            self.tr(swp[:, 0:128], fr[:, b * 128:(b + 1) * 128], self.ident_f[:], r=[fk, "ident_f"], w=["swp"])
                            self.cp(self.recst["xstok"][:, b, cidx * 128:(cidx + 1) * 128], swp[:, 0:128], r=["swp"], w=[("xstok", b)])
                    else:
                        self.act(o[:], gr[:], AF.Silu, r=[gk], w=[ok])
                        self.ld(self.dr["qk"][sm, dst[1] * 128:(dst[1] + 1) * 128, :], o[:], r=[ok], w=[("qk", sm, dst[1])])
                elif kind == "dt":
                    self.rec_dt(st2, l, sm, fr, fk, ssp, gr, gk, C, ck[0], S, ck[1])
            it = 0
            for b in range(cfg.NB if ksub > 100 or ksub < 0 else 0):
                ti = self.tile_of_block(b)
                for (c0, n, d0) in vspec:
                    a, ak = acc[it % 2], ("acc", it % 2)
                    for k in range(8):
                        self.mm(a[:, :n], hT[:, k, b * 128:(b + 1) * 128], wi[:, k, c0:c0 + n], start=(k == 0), stop=(k == 7),
                                r=[wkeys[k], ("hT", ti)], w=[ak], inc=(k == 7))
                    self.cp(vt[:, b, d0:d0 + n], a[:, :n], r=[ak], w=[("vt", b)], eng=("act" if it % 2 else "dve"))
                    it += 1
            self.s.barrier()

    def recip(self, out, in_, r, w):
        self.s.op("dve", lambda: self.nc.vector.reciprocal(out=out, in_=in_), r=r, w=w)

    def phase_attn(self, l, sm, vt):
        cfg = self.cfg
        T, NB, NLB = cfg.T, cfg.NB, cfg.NLB
        lp = self.lp
        with contextlib.ExitStack() as st2:
            KT = [self.sb(st2, f"KT{i}", [64, T], BF16) for i in range(2)]
            QT = [self.sb(st2, f"QT{i}", [64, T], BF16) for i in range(2)]
            pt = [self.sb(st2, f"pt{i}", [128, 512], BF16) for i in range(3)]
            Sps = [self.ps(st2, f"Sps{i}", [128, 512]) for i in range(2)]
            Ops = [self.ps(st2, f"Ops{i}", [128, 512]) for i in range(2)]
            Lps = [self.ps(st2, f"Lps{i}", [128, 512]) for i in range(2)]
            ssp = self.ps(st2, "assp", [128, 512])
            rl = [self.sb(st2, f"rl{i}", [128, 512], F32) for i in range(2)]
            o1 = self.sb(st2, "o1", [128, 512], F32)
            dd = self.sb(st2, "dd", [128, 512], F32)
            sqd = self.sb(st2, "sqd", [128, 512], BF16)
            rsd = self.sb(st2, "rsd", [128, 512], F32)
            o0row = self.sb(st2, "o0row", [128, T], F32)
            ob = [self.sb(st2, f"ob{i}", [128, 512], BF16) for i in range(2)]
            heads = []
            for g in range(2):
                for jj in range(4):
                    j = 4 * g + jj
                    heads.append((j * 64, 512 + g * 64, g * 64, 64, "gqa", j, 0))
            for h in range(4):
                for m in range(2):
                    heads.append((640 + h * 128 + m * 64, 1152 + h * 128 + m * 64, 128 + h * 128, 128, "diff", h, m))
            it = 0
            et = 0
            lastk = None
            nk = 0
            for hi, (qrow, krow, vc0, dv, kind, idx, m) in enumerate(heads):
                if krow != lastk:
                    nk += 1
                    lastk = krow
                    self.ld(KT[nk % 2][:], self.dr["qk"][sm, krow:krow + 64, :], r=[("qk", sm, krow // 128)], w=[("KT", nk % 2)])
                K, Kk = KT[nk % 2], ("KT", nk % 2)
                Q, Qk = QT[hi % 2], ("QT", hi % 2)
                self.ld(Q[:], self.dr["qk"][sm, qrow:qrow + 64, :], r=[("qk", sm, qrow // 128)], w=[Qk])
                for ti, (c0, w, seg) in enumerate(cfg.tiles):
                    kbs = list(range(NB)) if seg == 0 else list(range(NLB, NB))
                    O, Ok = Ops[et % 2], ("Ops", et % 2)
                    L, Lk = Lps[et % 2], ("Lps", et % 2)
                    for n, kb in enumerate(kbs):
                        S, Sk = Sps[it % 2], ("Sps", it % 2)
                        p, pk = pt[it % 3], ("pt", it % 3)
                        it += 1
                        self.mm(S[:, :w], K[:, kb * 128:(kb + 1) * 128], Q[:, c0:c0 + w], True, True, r=[Kk, Qk], w=[Sk])
                        self.act(p[:, :w], S[:, :w], AF.Exp, scale=0.125, r=[Sk], w=[pk])
                        last = (n == len(kbs) - 1)
                        self.mm(O[:dv, :w], vt[:, kb, vc0:vc0 + dv], p[:, :w], start=(n == 0), stop=last, r=[pk, ("vt", kb)], w=[Ok], inc=False)
                        self.mm(L[:dv, :w], self.ones_bf[:, :dv], p[:, :w], start=(n == 0), stop=last, r=[pk, "ones_bf"], w=[Lk], inc=True)
                    r_, rk = rl[et % 2], ("rl", et % 2)
                    o_, obk = ob[et % 2], ("ob", et % 2)
                    et += 1
                    self.recip(r_[:dv, :w], L[:dv, :w], r=[Lk], w=[rk])
                    if kind == "gqa":
                        self.tt(o_[:dv, :w], O[:dv, :w], r_[:dv, :w], ALU.mult, r=[Ok, rk], w=[obk])
                        self.ld(self.dr["aT"][sm, idx * 64:(idx + 1) * 64, c0:c0 + w], o_[:dv, :w], r=[obk], w=[("aT", sm, ti)])
                    elif m == 0:
                        self.tt(o0row[:, c0:c0 + w], O[:, :w], r_[:, :w], ALU.mult, r=[Ok, rk], w=[("o0row", ti)])
                    else:
                        self.tt(o1[:, :w], O[:, :w], r_[:, :w], ALU.mult, r=[Ok, rk], w=["o1"])
                        self.stt(dd[:, :w], o1[:, :w], lp["lamneg"], o0row[:, c0:c0 + w], ALU.mult, ALU.add, r=["o1", ("o0row", ti), "pcols"], w=["dd"])
                        self.act(sqd[:, :w], dd[:, :w], AF.Square, r=["dd"], w=["sqd"])
                        self.rstd([sqd[:, :w]], self.ones_bf[:], ssp[:, :w], rsd[:, :w], 128 * EPS, ["sqd", "ones_bf"], ["assp"], ["rsd"])
                        self.stt(o_[:, :w], dd[:, :w], lp["dwv"], rsd[:, :w], ALU.mult, ALU.mult, r=["dd", "rsd", "pcols"], w=[obk])
                        self.ld(self.dr["aT"][sm, 512 + idx * 128:512 + (idx + 1) * 128, c0:c0 + w], o_[:, :w], r=[obk], w=[("aT", sm, ti)])
            self.s.barrier()

    def layer_params_even(self, l):
        i = l // 2
        lam_init = 0.8 - 0.6 * math.exp(-0.3 * l)
        pc = self.pcol
        lp = {"qn": pc[:, 0:1], "kn": pc[:, 1:2], "lamneg": pc[:, 2:3], "dwv": pc[:, 3:4]}
        with contextlib.ExitStack() as st:
            dl = self.sb(st, "dl", [128, 256], F32)
            pr = self.sb(st, "pr", [128, 128], F32)
            self.ld(dl[:], self.dr["dlam"][:, i, :], r=[], w=["dl"])
            self.ld(pc[:, 0:1], self.dr["qnw"][:, i:i + 1], r=[], w=["pcols"])
            self.ld(pc[:, 1:2], self.dr["knw"][:, i:i + 1], r=[], w=["pcols"])
            self.ld(pc[:, 3:4], self.dr["dnw"][:, i:i + 1], r=[], w=["pcols"])
            self.tt(pr[:, 0:64], dl[:, 0:64], dl[:, 64:128], ALU.mult, r=["dl"], w=["pr"])
            self.tt(pr[:, 64:128], dl[:, 128:192], dl[:, 192:256], ALU.mult, r=["dl"], w=["pr"])
            self.s.op("dve", lambda: self.nc.vector.reduce_sum(out=pc[:, 4:5], in_=pr[:, 0:64], axis=AX.X), r=["pr"], w=["pcols"])
            self.s.op("dve", lambda: self.nc.vector.reduce_sum(out=pc[:, 5:6], in_=pr[:, 64:128], axis=AX.X), r=["pr"], w=["pcols"])
            self.act(pc[:, 4:6], pc[:, 4:6], AF.Exp, r=["pcols"], w=["pcols"])
            self.tt(pc[:, 2:3], pc[:, 5:6], pc[:, 4:5], ALU.subtract, r=["pcols"], w=["pcols"])
            self.ts(pc[:, 2:3], pc[:, 2:3], -lam_init, None, ALU.add, r=["pcols"], w=["pcols"])
            self.ts(pc[:, 3:4], pc[:, 3:4], math.sqrt(128.0) * (1 - lam_init), None, ALU.mult, r=["pcols"], w=["pcols"])
            self.s.barrier()
        self.lp = lp

    def phase_outproj(self, st, l, sm, wname, widx, h2tok, afftok, odd):
        cfg = self.cfg
        T = cfg.T
        with contextlib.ExitStack() as st2:
            wo = self.sb(st2, "wo", [128, 8, cfg.D], BF16)
            wr = self.sb(st2, "wr", [128, 8, cfg.NE], BF16)
            src = self.dr[wname][widx].rearrange("(k p) n -> p k n", p=128)
            for k in range(8):
                self.ld(wo[:, k, :], src[:, k, :], r=[], w=[("wo", k)], q="pool")
            self.ld(wr[:], self.dr["router_w"][l].rearrange("(k p) n -> p k n", p=128), r=[], w=["wr"], q="pool")
            nb = self.nm_bufs(st2)
            nbf = 1 if odd else 2
            xts = [self.sb(st2, f"dxt{i}", [128, 8, 512], F32) for i in range(nbf)]
            ats = [self.sb(st2, f"dat{i}", [128, 8, 512], BF16) for i in range(nbf)]
            h2 = [self.sb(st2, f"h2{i}", [128, 8, 512], BF16) for i in range(nbf)]
            acc = [self.ps(st2, f"dacc{i}", [128, 512]) for i in range(2)]
            lps = self.ps(st2, "lps", [128, 16])
            tps = [self.ps(st2, f"tps{i}", [128, 1024], BF16) for i in range(2)]
            ex = self.sb(st2, "ex", [128, 16], F32)
            rsum = self.sb(st2, "rsum", [128, 1], F32)
            if odd:
                ob = self.odd_bufs(st2)
            xsrc = self.dr["xT"][sm].rearrange("(k p) t -> p k t", p=128)
            asrc = self.dr["aT"][sm].rearrange("(k p) t -> p k t", p=128)
            it = 0
            bt = 0
            for ti, (c0, w, seg) in enumerate(cfg.tiles):
                v = cfg.SPC if seg == 1 else sm
                xt, xk = xts[ti % nbf], ("dxt", ti % nbf)
                at, atk = ats[ti % nbf], ("dat", ti % nbf)
                hh, hk = h2[ti % nbf], ("h2", ti % nbf)
                self.ld(xt[:, :, :w], xsrc[:, :, c0:c0 + w], r=[("xT", sm)], w=[xk])
                if odd:
                    self.odd_pre(l, sm, ob, at, atk, c0, w)
                else:
                    self.ld(at[:, :, :w], asrc[:, :, c0:c0 + w], r=[("aT", sm, ti)], w=[atk])
                for j in range(8):
                    a, ak = acc[it % 2], ("dacc", it % 2)
                    it += 1
                    for k in range(8):
                        self.mm(a[:, :w], wo[:, k, j * 128:(j + 1) * 128], at[:, k, :w], start=(k == 0), stop=(k == 7),
                                r=[("wo", k), atk], w=[ak], inc=(k == 7))
                    self.stt(xt[:, j, :w], a[:, :w], self.P(l, 2, j, v), xt[:, j, :w], ALU.mult, ALU.add, r=[ak, xk, "par"], w=[xk])
                self.ld(xsrc[:, :, c0:c0 + w], xt[:, :, :w], r=[xk], w=[("xT", sm)])
                self.norm_mod(xt, xk, w, nb, lambda k: self.P(l, 4, k, v), lambda k: self.P(l, 3, k, v), lambda k: hh[:, k, :w], hk)
                for bb in range(w // 128):
                    b = (c0 // 128) + bb
                    for k in range(8):
                        self.mm(lps[:, :], hh[:, k, bb * 128:(bb + 1) * 128], wr[:, k, :], start=(k == 0), stop=(k == 7),
                                r=[hk, "wr"], w=["lps"], inc=(k == 7))
                    self.act(ex[:], lps[:], AF.Exp, r=["lps"], w=["ex"])
                    self.s.op("dve", lambda: self.nc.vector.reduce_sum(out=rsum[:], in_=ex[:], axis=AX.X), r=["ex"], w=["rsum"])
                    self.recip(rsum[:], rsum[:], r=["rsum"], w=["rsum"])
                    self.ts(afftok[:, b, :], ex[:], rsum[:, 0:1], None, ALU.mult, r=["ex", "rsum"], w=[("aff", b)])
                    tp, tk = tps[bt % 2], ("tps", bt % 2)
                    bt += 1
                    for k in range(8):
                        self.tr(tp[:, k * 128:(k + 1) * 128], hh[:, k, bb * 128:(bb + 1) * 128], self.ident_bf[:], r=[hk, "ident_bf"], w=[tk], inc=(k == 7))
                    self.cp(h2tok[:, b, :], tp[:], r=[tk], w=[("h2tok", b)], eng=("act" if bt % 2 else "dve"))
            self.s.barrier()

    def phase_route(self, l, sm, h2tok, afftok, slot, gm):
        cfg = self.cfg
        NE, NB, NLB = cfg.NE, cfg.NB, cfg.NLB
        with contextlib.ExitStack() as st2:
            affT = self.sb(st2, "affT", [NE, cfg.T], F32)
            work = self.sb(st2, "work", [NE, cfg.NL], F32)
            mx = self.sb(st2, "mx", [NE, 8], F32)
            thr = self.sb(st2, "thr", [NE, 2], F32)
            thrb = self.sb(st2, "thrb", [128, 2, NE], F32)
            mask = self.sb(st2, "mask", [128, NB, NE], BF16)
            maskf = self.sb(st2, "maskf", [128, NB, NE], F32)
            ones = self.ones_bf
            us = self.load_const(st2, "ustrict", [128, 128], cast=True)
            iota = self.load_const(st2, "iota", [128, 256])
            tp = self.ps(st2, "rtp", [NE, 128])
            sp = self.ps(st2, "rsp", [128, NE])
            gps = [self.ps(st2, f"gps{i}", [128, 256]) for i in range(2)]
            sel = [self.sb(st2, f"sel{i}", [128, NLB, cfg.capl], BF16) for i in range(2)]
            xg = [self.sb(st2, f"xg{i}", [128, 8, cfg.capl], BF16) for i in range(2)]
            for b in range(NB):
                self.tr(tp[:, :], afftok[:, b, :], self.ident_f[:], r=[("aff", b), "ident_f"], w=["rtp"])
                self.cp(affT[:, b * 128:(b + 1) * 128], tp[:, :], r=["rtp"], w=["affT"])
            for seg, (t0, n, cap) in enumerate(((0, cfg.NL, cfg.capl), (cfg.NL, cfg.NC, cfg.capc))):
                self.cp(work[:, :n], affT[:, t0:t0 + n], r=["affT"], w=["work"])
                nit = cap // 8 + 1
                for i in range(nit):
                    self.s.op("dve", lambda: self.nc.vector.max(out=mx[:], in_=work[:, :n]), r=["work"], w=["mx"])
                    if i == nit - 2:
                        self.cp(thr[:, seg:seg + 1], mx[:, 7:8], r=["mx"], w=["thr"])
                    if i < nit - 1:
                        self.s.op("dve", lambda: self.nc.vector.match_replace(out=work[:, :n], in_to_replace=mx[:], in_values=work[:, :n], imm_value=-1.0),
                                  r=["mx", "work"], w=["work"])
                self.stt(thr[:, seg:seg + 1], thr[:, seg:seg + 1], 1.0, mx[:, 0:1], ALU.mult, ALU.add, r=["thr", "mx"], w=["thr"])
            self.ts(thr[:], thr[:], 0.5, None, ALU.mult, r=["thr"], w=["thr"])
            for seg in range(2):
                self.ld(self.dr["thr"][seg].rearrange("(e o) -> e o", o=1), thr[:, seg:seg + 1], r=["thr"], w=["thr_d"])
            for seg in range(2):
                self.ld(thrb[:, seg, :], self.dr["thr"][seg].partition_broadcast(128), r=["thr_d"], w=["thrb"])
            for b in range(NB):
                seg = 0 if b < NLB else 1
                self.tt(maskf[:, b, :], afftok[:, b, :], thrb[:, seg, :], ALU.is_gt, r=[("aff", b), "thrb"], w=[("mask", b)])
                self.cp(mask[:, b, :], maskf[:, b, :], r=[("mask", b)], w=[("maskb", b)])
                self.tt(gm[:, b, :], afftok[:, b, :], maskf[:, b, :], ALU.mult, r=[("aff", b), ("mask", b)], w=[("gm", b)], eng="dve")
            for seg, blocks in enumerate((list(range(NLB)), list(range(NLB, NB)))):
                for bi, b in enumerate(blocks):
                    for bj in range(bi + 1):
                        self.mm(sp[:, :], (us if bj == bi else ones)[:], mask[:, blocks[bj], :], start=(bj == 0), stop=(bj == bi),
                                r=[("maskb", blocks[bj]), ("sb", us.name), "ones_bf"], w=["rsp"], inc=(bj == bi))
                    self.cp(slot[:, b, :], sp[:, :], r=["rsp"], w=[("slot", b)])
            it = 0
            for e in range(NE):
                for seg, (blocks, cap, col0) in enumerate(((list(range(NLB)), cfg.capl, self.col0(sm, 0)),
                                                           (list(range(NLB, NB)), cfg.capc, self.col0(sm, 1)))):
                    sl, slk = sel[it % 2], ("sel", it % 2)
                    x_, xk = xg[it % 2], ("xg", it % 2)
                    it += 1
                    for bi, b in enumerate(blocks):
                        self.ts(sl[:, bi, :cap], iota[:, :cap], slot[:, b, e:e + 1], maskf[:, b, e:e + 1], ALU.is_equal, ALU.mult,
                                r=[("slot", b), ("mask", b), ("sb", iota.name)], w=[slk])
                    for j in range(8):
                        g, gk = gps[j % 2], ("gps", j % 2)
                        for bi, b in enumerate(blocks):
                            self.mm(g[:, :cap], h2tok[:, b, j * 128:(j + 1) * 128], sl[:, bi, :cap], start=(bi == 0), stop=(bi == len(blocks) - 1),
                                    r=[("h2tok", b), slk], w=[gk], inc=(bi == len(blocks) - 1))
                        self.cp(x_[:, j, :cap], g[:, :cap], r=[gk], w=[xk], eng=("act" if j % 2 else "dve"))
                    self.ld(self.dr["xin"][e].rearrange("(k p) c -> p k c", p=128)[:, :, col0:col0 + cap], x_[:, :, :cap], r=[xk], w=[("xin", e, sm, seg)])
            self.ld(self.dr["slotd"][sm], slot[:].rearrange("p b e -> p (b e)"), r=[("slot", b) for b in range(NB)], w=[("slotd", sm)])
            self.ld(self.dr["gmd"][sm], gm[:].rearrange("p b e -> p (b e)"), r=[("gm", b) for b in range(NB)], w=[("gmd", sm)])
            self.s.barrier()

    def phase_ffn(self, l):
        for grp in range(self.cfg.SPC // self.cfg.G):
            self.go() and self.phase_ffn_g(l, grp)

    def phase_ffn_g(self, l, grp):
        cfg = self.cfg
        NE, FC, cols, D = cfg.NE, cfg.FC, cfg.gcols, cfg.D
        gb = grp * cfg.gcols
        main = min(cols, 512)
        csplit = [(0, main)] + ([(main, cols - main)] if cols > main else [])
        cchunks = [(c, min(128, cols - c)) for c in range(0, cols, 128)]
        nq = 4 if FC >= 8 else 2
        FH = (FC + nq - 1) // nq
        units = [(f0, min(FH, FC - f0)) for f0 in range(0, FC, FH)]
        with contextlib.ExitStack() as st2:
            NU = 4
            wb = [self.sb(st2, f"wb{i}", [128, 8, FH * 128], BF16) for i in range(NU)]
            wdb = [self.sb(st2, f"wdb{i}", [128, FC, 512], BF16) for i in range(2)]
            xin = [self.sb(st2, f"fxin{i}", [128, 8, cols], BF16) for i in range(2)]
            actT = self.sb(st2, "actT", [128, FC, cols], BF16)
            sa = [self.sb(st2, f"sa{i}", [128, cols], F32) for i in range(2)]
            ysb = [self.sb(st2, f"ysb{i}", [128, len(cchunks), 512], BF16) for i in range(2)]
            aps = [self.ps(st2, f"aps{i}", [128, 512]) for i in range(2)]
            ups = [self.ps(st2, f"ups{i}", [128, 512]) for i in range(2)]
            cps = self.ps(st2, "cps", [128, 2, 256])
            yps = [self.ps(st2, f"yps{i}", [128, 512]) for i in range(len(cchunks))] if len(cchunks) <= 3 else None
            if yps is None:
                yps = aps + ups + [self.ps(st2, "yps4", [128, 512])]
                ypk = [("aps", 0), ("aps", 1), ("ups", 0), ("ups", 1), ("yps", 4)]
            else:
                ypk = [("yps", i) for i in range(len(cchunks))]
            ui = 0
            di = 0
            fi = 0
            for e in range(NE):
                x_, xk = xin[e % 2], ("fxin", e % 2)
                self.ld(x_[:], self.dr["xin"][e].rearrange("(k p) c -> p k c", p=128)[:, :, gb:gb + cols], r=[("xin", e, sm, sg) for sm in range(cfg.SPC) for sg in range(2)], w=[xk])
                for (f0, nf) in units:
                    wgt, wgk = wb[ui % NU], ("wb", ui % NU)
                    ui += 1
                    wut, wuk = wb[ui % NU], ("wb", ui % NU)
                    ui += 1
                    for (wt, wk, name) in ((wgt, wgk, "wg"), (wut, wuk, "wu")):
                        src = self.dr[name][l, e].rearrange("(k p) f -> p k f", p=128)
                        for k in range(8):
                            self.ld(wt[:, k, :nf * 128], src[:, k, f0 * 128:(f0 + nf) * 128], r=[], w=[wk], q="pool")
                    for f in range(nf):
                        a, ak = aps[fi % 2], ("aps", fi % 2)
                        u, uk = ups[fi % 2], ("ups", fi % 2)
                        s_, sk = sa[fi % 2], ("sa", fi % 2)
                        fi += 1
                        for (ps_, pk, wt, wk, ci) in ((a, ak, wgt, wgk, 0), (u, uk, wut, wuk, 1)):
                            for (cc0, cn) in csplit:
                                tgt = ps_[:, :cn] if cc0 == 0 else cps[:, ci, :cn]
                                tk = pk if cc0 == 0 else "cps"
                                for k in range(8):
                                    self.mm(tgt, wt[:, k, f * 128:(f + 1) * 128], x_[:, k, cc0:cc0 + cn], start=(k == 0), stop=(k == 7),
                                            r=[wk, xk], w=[tk], inc=(k == 7))
                        for (cc0, cn) in csplit:
                            asrc = a[:, :cn] if cc0 == 0 else cps[:, 0, :cn]
                            usrc = u[:, :cn] if cc0 == 0 else cps[:, 1, :cn]
                            akk = ak if cc0 == 0 else "cps"
                            ukk = uk if cc0 == 0 else "cps"
                            self.act(s_[:, cc0:cc0 + cn], asrc, AF.Silu, r=[akk], w=[sk])
                            self.tt(actT[:, f0 + f, cc0:cc0 + cn], s_[:, cc0:cc0 + cn], usrc, ALU.mult, r=[sk, ukk], w=["actT"])
                for dh in range(D // 512):
                    wd_, wdk = wdb[di % 2], ("wdb", di % 2)
                    y_, yk = ysb[di % 2], ("ysb", di % 2)
                    di += 1
                    src = self.dr["wd"][l, e].rearrange("(f p) d -> p f d", p=128)
                    step = 4
                    for f in range(0, FC, step):
                        nf_ = min(step, FC - f)
                        self.ld(wd_[:, f:f + nf_, :], src[:, f:f + nf_, dh * 512:(dh + 1) * 512], r=[], w=[wdk], q="pool")
                    for f in range(FC):
                        for ci, (cc0, cn) in enumerate(cchunks):
                            self.mm(yps[ci][:cn, :], actT[:, f, cc0:cc0 + cn], wd_[:, f, :], start=(f == 0), stop=(f == FC - 1),
                                    r=["actT", wdk], w=[ypk[ci]], inc=(f == FC - 1))
                    for ci, (cc0, cn) in enumerate(cchunks):
                        self.cp(y_[:cn, ci, :], yps[ci][:cn, :], r=[ypk[ci]], w=[yk], eng=("act" if ci % 2 else "dve"))
                    for ci, (cc0, cn) in enumerate(cchunks):
                        self.ld(self.dr["yexp"][e, gb + cc0:gb + cc0 + cn, dh * 512:(dh + 1) * 512], y_[:cn, ci, :], r=[yk], w=[("yexp", e)])
            self.s.barrier()

    def phase_scatter(self, l, sm, slot_unused, gm_unused):
        cfg = self.cfg
        NE, NLB, NB, D = cfg.NE, cfg.NLB, cfg.NB, cfg.D
        with contextlib.ExitStack() as st2:
            slot = self.sb(st2, "sslot", [128, NB, NE], F32)
            gm = self.sb(st2, "sgm", [128, NB, NE], F32)
            self.ld(slot[:].rearrange("p b e -> p (b e)"), self.dr["slotd"][sm], r=[], w=[("slot", sm)])
            self.ld(gm[:].rearrange("p b e -> p (b e)"), self.dr["gmd"][sm], r=[], w=[("gm", sm)])
            iota = self.load_const(st2, "iota", [128, 256])
            segs = []
            for seg, (cap, col0) in enumerate(((cfg.capl, self.col0(sm, 0)), (cfg.capc, self.col0(sm, 1)))):
                ck = min(128, cap)
                ncb = cap // ck
                y = self.sb(st2, f"sy{seg}", [ck, NE * ncb, D], BF16)
                for e in range(NE):
                    for cb in range(ncb):
                        self.ld(y[:, e * ncb + cb, :], self.dr["yexp"][e, col0 + cb * ck:col0 + (cb + 1) * ck, :], r=[("yexp", e)], w=[("sy", seg)])
                segs.append((cap, ck, ncb, y))
            mxi = max(NE * sg[2] for sg in segs)
            selT = self.sb(st2, "selT", [128, mxi, 512], BF16)
            sg_ = [self.sb(st2, f"sg{i}", [128, 256], BF16) for i in range(2)]
            tps = [self.ps(st2, f"stp{i}", [128, 8, 128], BF16) for i in range(2)]
            yT = [self.ps(st2, f"syT{i}", [128, 512]) for i in range(4)]
            xts = [self.sb(st2, f"sxt{i}", [128, 8, 512], F32) for i in range(2)]
            xsrc = self.dr["xT"][sm].rearrange("(k p) t -> p k t", p=128)
            it = 0
            gi = 0
            for ti, (c0, w, seg) in enumerate(cfg.tiles):
                cap, ck, ncb, y = segs[seg]
                v = cfg.SPC if seg == 1 else sm
                nidx = NE * ncb
                xt, xk = xts[ti % 2], ("sxt", ti % 2)
                self.ld(xt[:, :, :w], xsrc[:, :, c0:c0 + w], r=[("xT", sm)], w=[xk])
                for bb in range(w // 128):
                    b = c0 // 128 + bb
                    for g0 in range(0, nidx, 8):
                        tp, tk = tps[gi % 2], ("stp", gi % 2)
                        gi += 1
                        ng = min(8, nidx - g0)
                        for q in range(ng):
                            idx = g0 + q
                            e, cb = idx // ncb, idx % ncb
                            if cb == 0:
                                sg, sgk = sg_[it % 2], ("sg", it % 2)
                                it += 1
                                self.ts(sg[:, :cap], iota[:, :cap], slot[:, b, e:e + 1], gm[:, b, e:e + 1], ALU.is_equal, ALU.mult,
                                        r=[("slot", sm), ("gm", sm), ("sb", iota.name)], w=[sgk])
                            self.tr(tp[:ck, q, :], sg[:, cb * ck:(cb + 1) * ck], self.ident_bf[:], r=[sgk, "ident_bf"], w=[tk], inc=True)
                        self.cp(selT[:ck, g0:g0 + ng, bb * 128:(bb + 1) * 128], tp[:ck, :ng, :], r=[tk], w=["selT"], eng=("act" if gi % 2 else "dve"))
                for jg in range(2):
                    for idx in range(nidx):
                        for jj in range(4):
                            j = jg * 4 + jj
                            self.mm(yT[jj][:, :w], y[:ck, idx, j * 128:(j + 1) * 128], selT[:ck, idx, :w], start=(idx == 0), stop=(idx == nidx - 1),
                                    r=[("sy", seg), "selT"], w=[("syT", jj)], inc=(idx == nidx - 1))
                    for jj in range(4):
                        j = jg * 4 + jj
                        self.stt(xt[:, j, :w], yT[jj][:, :w], self.P(l, 5, j, v), xt[:, j, :w], ALU.mult, ALU.add, r=[("syT", jj), xk, "par"], w=[xk])
                self.ld(xsrc[:, :, c0:c0 + w], xt[:, :, :w], r=[xk], w=[("xT", sm)])
            self.s.barrier()

    def phase_final(self):
        cfg = self.cfg
        with contextlib.ExitStack() as st2:
            nb = self.nm_bufs(st2)
            xts = [self.sb(st2, f"fxt{i}", [128, 8, 512], F32) for i in range(2)]
            ots = [self.sb(st2, f"fot{i}", [128, 8, 512], F32) for i in range(2)]
            it = 0
            for sm in range(cfg.SPC):
                xsrc = self.dr["xT"][sm].rearrange("(k p) t -> p k t", p=128)
                osrc = self.out[sm].rearrange("(k p) t -> p k t", p=128)
                for ti, (c0, w, seg) in enumerate(cfg.tiles):
                    if seg == 1:
                        continue
                    xt, xk = xts[it % 2], ("fxt", it % 2)
                    ot, ok = ots[it % 2], ("fot", it % 2)
                    it += 1
                    self.ld(xt[:, :, :w], xsrc[:, :, c0:c0 + w], r=[("xT", sm)], w=[xk])
                    self.norm_mod(xt, xk, w, nb, lambda k: self.fnw[:, k:k + 1], None, lambda k: ot[:, k, :w], ok)
                    self.ld(osrc[:, :, c0:c0 + w], ot[:, :, :w], r=[ok], w=[("out", sm)])
            self.s.barrier()

    def go(self):
        self.ph += 1
        return self.ph <= self.stop

    def build(self):
        cfg = self.cfg
        self.declare()
        self.pcol = self.sb(self.es, "pcol", [128, 64], F32)
        import os
        self.stop = int(os.environ.get('KSTOP', '999'))
        self.ph = 0
        self.setup()
        NB, NE = cfg.NB, cfg.NE
        slot = [None] * cfg.SPC
        gm = [None] * cfg.SPC
        for l in range(cfg.DEPTH):
            odd = (l % 2 == 1)
            i = l // 2
            if odd:
                self.go() and self.layer_params_odd(l)
            else:
                self.go() and self.layer_params_even(l)
            for sm in range(cfg.SPC):
                with contextlib.ExitStack() as st:
                    vt = self.sb(st, "vt", [128, NB, 512 if odd else 640], BF16)
                    if odd:
                        self.recst = {"xstok": self.sb(st, "xstok", [128, NB, 512], BF16), "dttok": self.sb(st, "dttok", [128, NB, 16], F32),
                                      "ncum": self.sb(st, "ncum", [128, NB, 16], F32)}
                    with contextlib.ExitStack() as st1:
                        hT = self.phase_norm1(st1, l, sm) if self.go() else None
                        if odd:
                            self.go() and self.rec_inproj(st1, l, sm, hT, vt)
                        else:
                            lp = self.lp
                            rows = [("normrope", j, lp["qn"], j) for j in range(4)] + [("normrope", 4, lp["kn"], 4)]
                            rows += [("rope", 6 + j, 1.0, 5 + j) for j in range(4)] + [("rope", 10 + j, 1.0, 9 + j) for j in range(4)]
                            self.go() and self.phase_inproj(st1, l, sm, hT, "att_w_in", i, cfg.ATT_IN, rows, [(5 * 128, 128, 0), (14 * 128, 512, 128)], vt)
                    if odd:
                        self.go() and self.phase_rec(l, sm, vt)
                    else:
                        self.go() and self.phase_attn(l, sm, vt)
                with contextlib.ExitStack() as st:
                    h2tok = self.sb(st, "h2tok", [128, NB, cfg.D], BF16)
                    afftok = self.sb(st, "afftok", [128, NB, NE], F32)
                    slot[sm] = self.sb(st, "slot", [128, NB, NE], F32)
                    gm[sm] = self.sb(st, "gm", [128, NB, NE], F32)
                    self.go() and self.phase_outproj(st, l, sm, "rec_w_out" if odd else "att_w_out", i, h2tok, afftok, odd)
                    self.go() and self.phase_route(l, sm, h2tok, afftok, slot[sm], gm[sm])
            self.phase_ffn(l)
            for sm in range(cfg.SPC):
                self.go() and self.phase_scatter(l, sm, slot[sm], gm[sm])
        self.go() and self.phase_final()
        self.es.close()
        return self.nc


def _prep(cfg, inp):
    f = np.float32
    L, SPC = cfg.DEPTH, cfg.SPC

    def colT(a):
        a = np.asarray(a, f)
        sh = a.shape
        a = a.reshape(sh[:-1] + (sh[-1] // 128, 128))
        return np.ascontiguousarray(np.moveaxis(a, -1, 0))

    def rep(a, n=128):
        a = np.asarray(a, f)
        return np.ascontiguousarray(np.broadcast_to(a[None], (n,) + a.shape))
    shared = {}
    shared["ada_w"] = np.asarray(inp["ada_w"], f)
    shared["ada_bT"] = colT(inp["ada_b"])
    shared["n1w"] = colT(inp["norm1_w"]); shared["n2w"] = colT(inp["norm2_w"]); shared["fnw"] = colT(inp["final_norm_w"])
    shared["att_w_in"] = np.asarray(inp["att_w_in"], f); shared["att_w_out"] = np.asarray(inp["att_w_out"], f)
    shared["qnw"] = np.ascontiguousarray(np.tile(np.asarray(inp["att_q_norm_w"], f), (1, 2)).T)
    shared["knw"] = np.ascontiguousarray(np.tile(np.asarray(inp["att_k_norm_w"], f), (1, 2)).T)
    shared["dlam"] = rep(np.asarray(inp["diff_lambda"], f).reshape(-1, 256))
    shared["dnw"] = np.ascontiguousarray(np.asarray(inp["diff_norm_w"], f).T)
    if L >= 2:
        shared["rec_w_in"] = np.asarray(inp["rec_w_in"], f); shared["rec_w_out"] = np.asarray(inp["rec_w_out"], f)
        shared["rdl"] = rep(np.asarray(inp["ret_decay_logit"], f).reshape(-1, 8))
        shared["cwT"] = np.ascontiguousarray(np.moveaxis(colT(np.moveaxis(np.asarray(inp["ssd_conv_w"], f), 1, 0)), 1, -1))
        shared["cbT"] = colT(inp["ssd_conv_b"])
        shared["dtb"] = np.ascontiguousarray(np.asarray(inp["ssd_dt_bias"], f).reshape(-1, 16).T)
        shared["alog"] = np.ascontiguousarray(np.asarray(inp["ssd_a_log"], f).reshape(-1, 16).T)
        shared["alogr"] = rep(np.asarray(inp["ssd_a_log"], f).reshape(-1, 16))
        shared["dsk"] = np.ascontiguousarray(np.moveaxis(np.repeat(np.asarray(inp["ssd_d_skip"], f), 64, axis=-1).reshape(-1, 4, 128), -1, 0))
        shared["snw"] = colT(inp["ssd_norm_w"])
    else:
        z = lambda *s: np.zeros(s, f)
        shared.update(rec_w_in=z(1, cfg.D, cfg.REC_IN), rec_w_out=z(1, cfg.D, cfg.D), rdl=z(128, 1, 8), cwT=z(128, 1, 8, 3), cbT=z(128, 1, 8),
                      dtb=z(16, 1), alog=z(16, 1), alogr=z(128, 1, 16), dsk=z(128, 1, 4), snw=z(128, 1, 4))
    shared["router_w"] = np.asarray(inp["router_w"], f)
    shared["wg"] = np.asarray(inp["expert_w_gate"], f); shared["wu"] = np.asarray(inp["expert_w_up"], f); shared["wd"] = np.asarray(inp["expert_w_down"], f)
    shared.update(_consts(cfg))
    x, ctx, c = np.asarray(inp["x"], f), np.asarray(inp["ctx"], f), np.asarray(inp["c"], f)
    cc = np.asarray(inp["c_ctx"], f)
    maps = []
    for core in range(cfg.NCORES):
        b0 = core * SPC
        m = dict(shared)
        m["xT0"] = np.ascontiguousarray(np.concatenate([x[b0:b0 + SPC], ctx[b0:b0 + SPC]], axis=1).transpose(0, 2, 1))
        cv = np.concatenate([c[b0:b0 + SPC], cc[None]], 0)
        m["cT"] = np.ascontiguousarray(cv.reshape(SPC + 1, 8, 128).transpose(2, 1, 0))
        maps.append(m)
    return maps


def run(cfg, inp):
    prog = Prog(cfg)
    nc = prog.build()
    maps = _prep(cfg, inp)
    res = run_bass_kernel_spmd(nc, maps, core_ids=list(range(cfg.NCORES)))
    outs = [np.asarray(r["outT"]).transpose(0, 2, 1) for r in res.results]
    return np.ascontiguousarray(np.concatenate(outs, 0)).astype(np.float32)


def kernel(**inputs):
    cfg = Cfg(SPC=2)
    return run(cfg, inputs)


def _layer_params_odd(self, l):
    i = l // 2
    pc = self.pcol
    lp = {"lg": pc[:, 8:16], "neglg": pc[:, 16:24], "dtb": pc[:16, 24:25], "aneg": pc[:16, 25:26], "isF": pc[:16, 26:27], "isB": pc[:16, 27:28],
          "one": pc[:, 63:64], "dsk": pc[:, 32:36], "snw": pc[:, 36:40]}
    if not hasattr(self, "cw"):
        self.cw = self.sb(self.es, "cw", [128, 8, 3], F32)
        self.cb = self.sb(self.es, "cb", [128, 8], F32)
    V = self.nc.vector
    self.s.op("dve", lambda: V.memset(pc[:, 63:64], 1.0), r=[], w=["pcols"])
    self.s.op("dve", lambda: V.memset(pc[:16, 26:27], 0.0), r=[], w=["pcols"])
    self.s.op("dve", lambda: V.memset(pc[:8, 26:27], 1.0), r=[], w=["pcols"])
    self.s.op("dve", lambda: V.memset(pc[:16, 27:28], 1.0), r=[], w=["pcols"])
    self.s.op("dve", lambda: V.memset(pc[:8, 27:28], 0.0), r=[], w=["pcols"])
    self.ld(pc[:, 8:16], self.dr["rdl"][:, i, :], r=[], w=["pcols"])
    self.ld(pc[:16, 24:25], self.dr["dtb"][:, i:i + 1], r=[], w=["pcols"])
    self.ld(pc[:16, 25:26], self.dr["alog"][:, i:i + 1], r=[], w=["pcols"])
    self.ld(pc[:, 32:36], self.dr["dsk"][:, i, :], r=[], w=["pcols"])
    self.ld(pc[:, 36:40], self.dr["snw"][:, i, :], r=[], w=["pcols"])
    self.ld(self.cw[:], self.dr["cwT"][:, i], r=[], w=["pcols"])
    self.ld(self.cb[:], self.dr["cbT"][:, i, :], r=[], w=["pcols"])
    self.act(pc[:, 16:24], pc[:, 8:16], AF.Exp, scale=-1.0, r=["pcols"], w=["pcols"])
    self.act(pc[:, 16:24], pc[:, 16:24], AF.Ln, bias=pc[:, 63:64], r=["pcols"], w=["pcols"])
    self.ts(pc[:, 8:16], pc[:, 16:24], -1.0, None, ALU.mult, r=["pcols"], w=["pcols"])
    self.act(pc[:16, 25:26], pc[:16, 25:26], AF.Exp, r=["pcols"], w=["pcols"])
    self.ts(pc[:16, 25:26], pc[:16, 25:26], -1.0, None, ALU.mult, r=["pcols"], w=["pcols"])
    self.ts(pc[:, 36:40], pc[:, 36:40], 16.0, None, ALU.mult, r=["pcols"], w=["pcols"])
    self.s.barrier()
    self.lp = lp


def _rec_inproj(self, st1, l, sm, hT, vt):
    cfg = self.cfg
    i = l // 2
    rows = [("rope", 0, 1.0, 0), ("rope", 1, 1.0, 1), ("rope", 2, 0.125, 2), ("rope", 3, 0.125, 3)]
    rows += [("side", 8 + c, None, c) for c in range(4)] + [("side", 12 + c, None, 4 + c) for c in range(4)]
    rows += [("conv", 16 + c, (self.cw, self.cb, c), ("xs", 8 + c)) for c in range(4)]
    rows += [("conv", 20 + c, (self.cw, self.cb, 4 + c), ("qk", 4 + c)) for c in range(4)]
    rows += [("dt", 24, None, None)]
    self.phase_inproj(st1, l, sm, hT, "rec_w_in", i, cfg.REC_IN, rows, [(4 * 128, 512, 0)], vt)


def _rec_dt(self, st2, l, sm, fr, fk, ssp, gr, gk, Cb, Ck, Sb, Sk):
    cfg = self.cfg
    T, NL, NC, NB = cfg.T, cfg.NL, cfg.NC, cfg.NB
    lp, rs = self.lp, self.recst
    with contextlib.ExitStack() as st:
        dtT, la, pa, pb = fr[:16, :], gr[:16, :], Cb[:16, :], Sb[:16, :]
        cF = self.sb(st, "cF", [16, T], F32)
        self.act(dtT[:], fr[:16, :], AF.Exp, bias=lp["dtb"], r=[fk, "pcols"], w=[fk])
        self.act(dtT[:], dtT[:], AF.Ln, bias=lp["one"][:16], r=[fk, "pcols"], w=[fk])
        self.ts(la[:], dtT[:], lp["aneg"], None, ALU.mult, r=[fk, "pcols"], w=[gk])
        cur, ck_ = la, gk
        bufs = [(pa, Ck), (pb, Sk)]
        bi = 0
        sh = 1
        while sh < max(NL, NC):
            nxt, nk = bufs[bi % 2]
            bi += 1
            for (s0, n) in ((0, NL), (NL, NC)):
                if sh < n:
                    self.tt(nxt[:, s0 + sh:s0 + n], cur[:, s0 + sh:s0 + n], cur[:, s0:s0 + n - sh], ALU.add, r=[ck_], w=[nk])
                    self.cp(nxt[:, s0:s0 + sh], cur[:, s0:s0 + sh], r=[ck_], w=[nk])
                else:
                    self.cp(nxt[:, s0:s0 + n], cur[:, s0:s0 + n], r=[ck_], w=[nk])
            cur, ck_ = nxt, nk
            sh *= 2
        P, Pk = cur, ck_
        oth, ok_ = bufs[bi % 2]
        totc, totl = P[:, T - 1:T], P[:, NL - 1:NL]
        self.ts(cF[:, 0:NL], P[:, 0:NL], totc, None, ALU.add, r=[Pk], w=["cF"])
        self.cp(cF[:, NL:T], P[:, NL:T], r=[Pk], w=["cF"])
        self.tt(oth[:], la[:], P[:], ALU.subtract, r=[gk, Pk], w=[ok_])
        self.ts(oth[:, 0:NL], oth[:, 0:NL], totc, totl, ALU.add, ALU.add, r=[ok_, Pk], w=[ok_])
        self.ts(oth[:, NL:T], oth[:, NL:T], totc, None, ALU.add, r=[ok_, Pk], w=[ok_])
        self.ts(cF[:], cF[:], lp["isF"], None, ALU.mult, r=["cF", "pcols"], w=["cF"])
        self.stt(cF[:], oth[:], lp["isB"], cF[:], ALU.mult, ALU.add, r=[ok_, "cF", "pcols"], w=["cF"])
        self.ld(self.dr["cumT"][sm], cF[:], r=["cF"], w=[("cumT", sm)])
        self.ts(la[:], cF[:], -1.0, None, ALU.mult, r=["cF"], w=[gk])
        for b in range(NB):
            self.tr(ssp[:, 0:16], dtT[:, b * 128:(b + 1) * 128], self.ident_f[:16, :16], r=[fk, "ident_f"], w=["ssp"])
            self.cp(rs["dttok"][:, b, :], ssp[:, 0:16], r=["ssp"], w=[("dttok", b)])
            self.tr(ssp[:, 16:32], la[:, b * 128:(b + 1) * 128], self.ident_f[:16, :16], r=[gk, "ident_f"], w=["ssp"])
            self.cp(rs["ncum"][:, b, :], ssp[:, 16:32], r=["ssp"], w=[("ncum", b)])


Prog.layer_params_odd = _layer_params_odd
Prog.rec_inproj = _rec_inproj
Prog.rec_dt = _rec_dt


def _vis(tf_s, tf_t):
    if tf_s.max() <= tf_t.min():
        return "full"
    if tf_s.min() > tf_t.max():
        return "none"
    return "part"


def _phase_rec(self, l, sm, vt):
    cfg = self.cfg
    T, NB, NL, NC = cfg.T, cfg.NB, cfg.NL, cfg.NC
    lp, rs = self.lp, self.recst
    cst = _consts(cfg)
    TF, TB = cst["tfrow"][0], cst["tbrow"][0]
    with contextlib.ExitStack() as st2:
        tfrow = self.load_const(st2, "tfrow", [128, T])
        tbrow = self.load_const(st2, "tbrow", [128, T])
        tfcol = self.load_const(st2, "tfcol", [128, NB])
        tbcol = self.load_const(st2, "tbcol", [128, NB])
        maskF = self.load_const(st2, "maskF", [128, 4, 512])
        maskB = self.load_const(st2, "maskB", [128, 4, 512])
        ckeys = [("sb", t.name) for t in (tfrow, tbrow, tfcol, tbcol, maskF, maskB)]
        vssd = self.sb(st2, "vssd", [128, NB, 16, 64], BF16)
        for b in range(NB):
            for dh in range(16):
                H = dh % 8
                self.ts(vssd[:, b, dh, :], rs["xstok"][:, b, H * 64:(H + 1) * 64], rs["dttok"][:, b, dh:dh + 1], None, ALU.mult,
                        r=[("xstok", b), ("dttok", b)], w=[("vssd", b)])
        KT = [self.sb(st2, f"rKT{i}", [128, T], BF16) for i in range(2)]
        QT = [self.sb(st2, f"rQT{i}", [128, T], BF16) for i in range(2)]
        nbF = self.sb(st2, "nbF", [128, NB], F32)
        nbB = self.sb(st2, "nbB", [128, NB], F32)
        Sps = [self.ps(st2, f"rS{i}", [128, 512]) for i in range(2)]
        Ops = [self.ps(st2, f"rO{i}", [128, 512]) for i in range(4)]
        Dt = [self.sb(st2, f"rD{i}", [128, 512], F32) for i in range(4)]
        pre = [self.sb(st2, f"rpre{i}", [128, 512], F32) for i in range(2)]
        pt = [self.sb(st2, f"rpt{i}", [128, 512], BF16) for i in range(3)]
        osb = [self.sb(st2, f"rosb{i}", [128, 512], F32) for i in range(2)]
        crow = [self.sb(st2, f"crow{i}", [128, 512], F32) for i in range(8)]
        cnt = {"s": 0, "d": 0, "p": 0, "pt": 0, "o": 0}

        def decay(row_ap, rowkey, scale, bias, mode, mask_ap):
            d, dk = Dt[cnt["d"] % 4], ("rD", cnt["d"] % 4)
            cnt["d"] += 1
            src, sk = row_ap, rowkey
            if mode == "part":
                p_, pk_ = pre[cnt["p"] % 2], ("rpre", cnt["p"] % 2)
                cnt["p"] += 1
                if scale is None:
                    self.tt(p_[:, :mask_ap.shape[1]], row_ap, mask_ap, ALU.add, r=[rowkey] + ckeys, w=[pk_])
                else:
                    self.stt(p_[:, :mask_ap.shape[1]], row_ap, scale, mask_ap, ALU.mult, ALU.add, r=[rowkey, "pcols"] + ckeys, w=[pk_])
                src, sk = p_[:, :mask_ap.shape[1]], pk_
                self.act(d[:, :mask_ap.shape[1]], src, AF.Exp, bias=bias, r=[sk, "nb", "pcols"] + [("ncum", b_) for b_ in range(NB)], w=[dk])
            else:
                wdt = row_ap.shape[1]
                if scale is None:
                    self.act(d[:, :wdt], src, AF.Exp, bias=bias, r=[sk, "nb"] + [("ncum", b_) for b_ in range(NB)], w=[dk])
                else:
                    self.act(d[:, :wdt], src, AF.Exp, bias=bias, scale=scale, r=[sk, "nb", "pcols"], w=[dk])
            return d, dk

        def classify(c0, w, kb):
            tt_f, tt_b = TF[c0:c0 + w], TB[c0:c0 + w]
            ts_f, ts_b = TF[kb * 128:(kb + 1) * 128], TB[kb * 128:(kb + 1) * 128]
            vf, vb = _vis(ts_f, tt_f), _vis(ts_b, tt_b)
            j = (kb * 128 - c0) // 128
            return vf, vb, j

        for h in range(4):
            K, Kk = KT[h % 2], ("rKT", h % 2)
            Q, Qk = QT[h % 2], ("rQT", h % 2)
            self.ld(K[:64, :], self.dr["qk"][sm, 256 + h * 64:256 + (h + 1) * 64, :], r=[("qk", sm, 2 + h // 2)], w=[Kk])
            self.ld(Q[:64, :], self.dr["qk"][sm, h * 64:(h + 1) * 64, :], r=[("qk", sm, h // 2)], w=[Qk])
            self.ts(nbF[:], tfcol[:], lp["neglg"][:, h:h + 1], None, ALU.mult, r=ckeys + ["pcols"], w=["nb"])
            self.ts(nbB[:], tbcol[:], lp["neglg"][:, 4 + h:5 + h], None, ALU.mult, r=ckeys + ["pcols"], w=["nb"])
            for ti, (c0, w, seg) in enumerate(cfg.tiles):
                O, Ok = Ops[cnt["o"] % 4], ("rO", cnt["o"] % 4)
                ob_, obk = osb[cnt["o"] % 2], ("rosb", cnt["o"] % 2)
                cnt["o"] += 1
                contrib = []
                for kb in range(NB):
                    vf, vb, j = classify(c0, w, kb)
                    if vf != "none" or vb != "none":
                        contrib.append((kb, vf, vb, j))
                for n, (kb, vf, vb, j) in enumerate(contrib):
                    S, Sk = Sps[cnt["s"] % 2], ("rS", cnt["s"] % 2)
                    cnt["s"] += 1
                    self.mm(S[:, :w], K[:64, kb * 128:(kb + 1) * 128], Q[:64, c0:c0 + w], True, True, r=[Kk, Qk], w=[Sk])
                    ds = []
                    if vf != "none":
                        ds.append(decay(tfrow[:, c0:c0 + w], ckeys[0], lp["lg"][:, h:h + 1], nbF[:, kb:kb + 1], vf, maskF[:, j, :w] if vf == "part" else None))
                    if vb != "none":
                        ds.append(decay(tbrow[:, c0:c0 + w], ckeys[1], lp["lg"][:, 4 + h:5 + h], nbB[:, kb:kb + 1], vb, maskB[:, j, :w] if vb == "part" else None))
                    d, dk = ds[0]
                    if len(ds) == 2:
                        self.tt(d[:, :w], d[:, :w], ds[1][0][:, :w], ALU.add, r=[dk, ds[1][1]], w=[dk])
                    p, pk = pt[cnt["pt"] % 3], ("rpt", cnt["pt"] % 3)
                    cnt["pt"] += 1
                    self.tt(p[:, :w], S[:, :w], d[:, :w], ALU.mult, r=[Sk, dk], w=[pk])
                    self.mm(O[:, :w], vt[:, kb, h * 128:(h + 1) * 128], p[:, :w], start=(n == 0), stop=(n == len(contrib) - 1),
                            r=[pk, ("vt", kb)], w=[Ok], inc=True)
                self.cp(ob_[:, :w], O[:, :w], r=[Ok], w=[obk], eng="act")
                self.ld(self.dr["rawT"][sm, h * 128:(h + 1) * 128, c0:c0 + w], ob_[:, :w], r=[obk], w=[("rawT", sm, ti)])
        for g in range(2):
            K, Kk = KT[g % 2], ("rKT", g % 2)
            Q, Qk = QT[g % 2], ("rQT", g % 2)
            self.ld(K[:], self.dr["qk"][sm, (4 + g) * 128:(5 + g) * 128, :], r=[("qk", sm, 4 + g)], w=[Kk])
            self.ld(Q[:], self.dr["qk"][sm, (6 + g) * 128:(7 + g) * 128, :], r=[("qk", sm, 6 + g)], w=[Qk])
            for ti, (c0, w, seg) in enumerate(cfg.tiles):
                for hh in range(4):
                    for d_ in range(2):
                        dh = d_ * 8 + g * 4 + hh
                        self.ld(crow[d_ * 4 + hh][:, :w], self.dr["cumT"][sm, dh, c0:c0 + w].partition_broadcast(128), r=[("cumT", sm)], w=[("crow", d_ * 4 + hh)])
                contrib = []
                for kb in range(NB):
                    vf, vb, j = classify(c0, w, kb)
                    if vf != "none" or vb != "none":
                        contrib.append((kb, vf, vb, j))
                ncontrib = sum((vf != "none") + (vb != "none") for (_, vf, vb, _) in contrib)
                seen = [0] * 4
                for (kb, vf, vb, j) in contrib:
                    S, Sk = Sps[cnt["s"] % 2], ("rS", cnt["s"] % 2)
                    cnt["s"] += 1
                    self.mm(S[:, :w], K[:, kb * 128:(kb + 1) * 128], Q[:, c0:c0 + w], True, True, r=[Kk, Qk], w=[Sk])
                    for hh in range(4):
                        for d_, (vis, msk) in enumerate(((vf, maskF), (vb, maskB))):
                            if vis == "none":
                                continue
                            dh = d_ * 8 + g * 4 + hh
                            d, dk = decay(crow[d_ * 4 + hh][:, :w], ("crow", d_ * 4 + hh), None, rs["ncum"][:, kb, dh:dh + 1], vis,
                                          msk[:, j, :w] if vis == "part" else None)
                            p, pk = pt[cnt["pt"] % 3], ("rpt", cnt["pt"] % 3)
                            cnt["pt"] += 1
                            self.tt(p[:, :w], S[:, :w], d[:, :w], ALU.mult, r=[Sk, dk], w=[pk])
                            self.mm(Ops[hh][:64, :w], vssd[:, kb, dh, :], p[:, :w], start=(seen[hh] == 0), stop=(seen[hh] == ncontrib - 1),
                                    r=[pk, ("vssd", kb)], w=[("rO", hh)], inc=True)
                            seen[hh] += 1
                for hh in range(4):
                    H = g * 4 + hh
                    ob_, obk = osb[hh % 2], ("rosb", hh % 2)
                    self.cp(ob_[:64, :w], Ops[hh][:64, :w], r=[("rO", hh)], w=[obk], eng="act")
                    self.ld(self.dr["rawT"][sm, 512 + H * 64:512 + (H + 1) * 64, c0:c0 + w], ob_[:64, :w], r=[obk], w=[("rawT", sm, ti)])
        self.s.barrier()


def _odd_bufs(self, st2):
    return {"raw": self.sb(st2, "oraw", [128, 8, 512], F32), "side": self.sb(st2, "oside", [128, 12, 512], F32),
            "sq": self.sb(st2, "osq", [128, 4, 512], BF16), "rs": self.sb(st2, "ors", [128, 512], F32),
            "sg": self.sb(st2, "osg", [128, 512], F32), "t": self.sb(st2, "ot", [128, 512], F32),
            "s2": self.sb(st2, "os2", [128, 4, 512], F32), "ssp": self.ps(st2, "otp", [128, 512])}


def _odd_pre(self, l, sm, ob, at, atk, c0, w):
    lp = self.lp
    raw, side, sq, rs_, sg, t, s2, ssp = (ob[k] for k in ("raw", "side", "sq", "rs", "sg", "t", "s2", "ssp"))
    rsrc = self.dr["rawT"][sm].rearrange("(k p) t -> p k t", p=128)
    ssrc = self.dr["sideT"][sm].rearrange("(k p) t -> p k t", p=128)
    self.ld(raw[:, :, :w], rsrc[:, :, c0:c0 + w], r=[("rawT", sm)], w=["oraw"])
    self.ld(side[:, :, :w], ssrc[:, :, c0:c0 + w], r=[("sideT", sm)], w=["oside"])
    for c in range(4):
        self.act(sq[:, 0, :w], raw[:, c, :w], AF.Square, r=["oraw"], w=["osq"])
        self.rstd([sq[:, 0, :w]], self.ones_bf[:], ssp[:, :w], rs_[:, :w], 128 * EPS, ["osq", "ones_bf"], ["otp"], ["ors"])
        self.act(sg[:, :w], side[:, c, :w], AF.Silu, r=["oside"], w=["osg"])
        self.tt(t[:, :w], raw[:, c, :w], rs_[:, :w], ALU.mult, r=["oraw", "ors"], w=["ot"])
        self.stt(at[:, c, :w], t[:, :w], math.sqrt(128.0), sg[:, :w], ALU.mult, ALU.mult, r=["ot", "osg"], w=[atk])
    for c in range(4):
        self.stt(t[:, :w], side[:, 8 + c, :w], lp["dsk"][:, c:c + 1], raw[:, 4 + c, :w], ALU.mult, ALU.add, r=["oside", "oraw", "pcols"], w=["ot"])
        self.act(sg[:, :w], side[:, 4 + c, :w], AF.Silu, r=["oside"], w=["osg"])
        self.tt(s2[:, c, :w], t[:, :w], sg[:, :w], ALU.mult, r=["ot", "osg"], w=["os2"])
        self.act(sq[:, c, :w], s2[:, c, :w], AF.Square, r=["os2"], w=["osq"])
    for gg in range(2):
        self.rstd([sq[:, 2 * gg, :w], sq[:, 2 * gg + 1, :w]], self.ones_bf[:], ssp[:, :w], rs_[:, :w], 256 * EPS, ["osq", "ones_bf"], ["otp"], ["ors"])
        for c in (2 * gg, 2 * gg + 1):
            self.stt(at[:, 4 + c, :w], s2[:, c, :w], lp["snw"][:, c:c + 1], rs_[:, :w], ALU.mult, ALU.mult, r=["os2", "ors", "pcols"], w=[atk])


Prog.phase_rec = _phase_rec
Prog.odd_bufs = _odd_bufs
Prog.odd_pre = _odd_pre
```

```python
import math
import contextlib
import numpy as np
import concourse.bass as bass
import concourse.mybir as mybir
from concourse.bass_utils import run_bass_kernel_spmd

F32 = mybir.dt.float32
BF16 = mybir.dt.bfloat16
AF = mybir.ActivationFunctionType
ALU = mybir.AluOpType
AX = mybir.AxisListType
NEG = -30000.0
EPS = 1e-6


class Cfg:
    def __init__(self, BATCH=16, SEQ=2048, DEPTH=4, CTX=256, FF=2816, SPC=2, NE=16):
        self.D = 1024
        self.BATCH, self.NL, self.DEPTH, self.NC, self.FF, self.SPC, self.NE = BATCH, SEQ, DEPTH, CTX, FF, SPC, NE
        self.T = SEQ + CTX
        self.NCORES = BATCH // SPC
        self.GRID_W = 64
        self.KD = 8
        self.ATT_IN = 2304
        self.REC_IN = 3088
        self.capl = 2 * SEQ // NE
        self.capc = 2 * CTX // NE
        self.NLB = SEQ // 128
        self.NCB = CTX // 128
        self.NB = self.NLB + self.NCB
        self.tiles = []
        for c0 in range(0, SEQ, 512):
            self.tiles.append((c0, min(512, SEQ - c0), 0))
        for c0 in range(0, CTX, 512):
            self.tiles.append((SEQ + c0, min(512, CTX - c0), 1))
        self.FC = FF // 128
        self.G = min(SPC, 2)
        self.gcols = self.G * (self.capl + self.capc)
        self.cols = (SPC // self.G) * self.gcols


class Buf:
    __slots__ = ("w", "r")

    def __init__(self):
        self.w = None
        self.r = {}


class Sched:
    ENG = ("pe", "act", "dve", "pool", "sp")
    LIMIT = 20000
    NS = 12

    def __init__(self, nc, es):
        self.nc, self.es = nc, es
        self.E = {"pe": nc.tensor, "act": nc.scalar, "dve": nc.vector, "pool": nc.gpsimd, "sp": nc.sync}
        self.sems = {}
        self.cnt = {e: 0 for e in self.ENG}
        self.epoch = {e: 0 for e in self.ENG}
        self.pending = {e: False for e in self.ENG}
        self.seen = {e: {} for e in self.ENG}
        self.latest = {}
        self.bufs = {}
        self.dmacnt = {e: 0 for e in self.ENG}
        self.nins = 0

    def semh(self, k):
        if k not in self.sems:
            self.sems[k] = self.es.enter_context(self.nc.semaphore("s_" + "_".join(str(x) for x in k)))
        return self.sems[k]

    PSUM_NAMES = {"acc", "ssp", "swp", "nm_ss", "modps", "Sps", "Ops", "Lps", "assp", "dacc", "lps", "tps", "rtp", "rsp", "gps",
                  "aps", "ups", "cps", "yps", "stp", "syT", "rS", "rO", "otp"}

    def _split(self, r, w):
        r2, w2 = [], list(w)
        for key in r:
            name = key if isinstance(key, str) else key[0]
            if name in self.PSUM_NAMES:
                if key not in w2:
                    w2.append(key)
            else:
                r2.append(key)
        return r2, w2

    def _need(self, eng, r, w):
        need = {}

        def req(tok):
            if tok is None:
                return
            k, v = tok
            if eng == "pe" and k[0] == "pe":
                return
            if need.get(k, 0) < v:
                need[k] = v
        for key in r:
            b = self.bufs.get(key)
            if b is None:
                b = self.bufs[key] = Buf()
            req(b.w)
        for key in w:
            b = self.bufs.get(key)
            if b is None:
                b = self.bufs[key] = Buf()
            req(b.w)
            for tok in b.r.values():
                req(tok)
        seen = self.seen[eng]
        for k, v in need.items():
            if seen.get(k, 0) < v:
                self.E[eng].wait_ge(self.semh(k), v)
                seen[k] = v
                self.nins += 1

    def _mark(self, tok, r, w):
        k, v = tok
        if self.latest.get(k, 0) < v:
            self.latest[k] = v
        for key in r:
            self.bufs[key].r[k] = tok
        for key in w:
            b = self.bufs[key]
            b.w = tok
            b.r = {}

    def skip(self):
        import os
        if not hasattr(self, "maxops"):
            self.maxops = int(os.environ.get("KOPS", "100000000"))
            self.opc = 0
        self.opc += 1
        import os
        if os.environ.get("KDBG") and abs(self.opc - self.maxops) < 8:
            import traceback
            fr = traceback.extract_stack()[-4]
            print("OP", self.opc, fr.lineno, fr.line)
        if self.opc == self.maxops:
            print("LAST OP before cutoff ^^^")
        return self.opc > self.maxops

    def op(self, eng, fn, r=(), w=(), inc=True):
        if self.skip():
            return
        r, w = self._split(r, w)
        self._need(eng, r, w)
        ins = fn()
        self.nins += 1
        k = (eng, self.epoch[eng])
        if inc:
            self.cnt[eng] += 1
            ins.then_inc(self.semh(k), 1)
            self.pending[eng] = False
            tok = (k, self.cnt[eng])
        else:
            self.pending[eng] = True
            tok = (k, self.cnt[eng] + 1)
        self._mark(tok, r, w)
        if inc and self.cnt[eng] >= self.LIMIT:
            self.epoch[eng] += 1
            self.cnt[eng] = 0

    def dma(self, q, out, in_, r=(), w=()):
        if self.skip():
            return
        r, w = self._split(r, w)
        self._need(q, r, w)
        j = self.dmacnt[q]
        self.dmacnt[q] += 1
        slot, rnd = j % self.NS, j // self.NS
        k = ("dma", q, slot)
        if rnd > 0 and self.seen[q].get(k, 0) < 16 * rnd:
            self.E[q].wait_ge(self.semh(k), 16 * rnd)
            self.seen[q][k] = 16 * rnd
        self.E[q].dma_start(out=out, in_=in_, allow_slow_non_contiguous=True).then_inc(self.semh(k), 16)
        self.nins += 1
        self._mark((k, 16 * (rnd + 1)), r, w)

    def barrier(self):
        for e in self.ENG:
            assert not self.pending[e], e
        for e in self.ENG:
            seen = self.seen[e]
            for k, v in self.latest.items():
                if e == "pe" and k[0] == "pe":
                    continue
                if seen.get(k, 0) < v:
                    self.E[e].wait_ge(self.semh(k), v)
                    seen[k] = v
                    self.nins += 1
        self.bufs = {}


def _consts(cfg):
    NL, NC, T = cfg.NL, cfg.NC, cfg.T
    c = {}
    c["ones"] = np.ones((128, 128), np.float32)
    bo = np.zeros((128, 128), np.float32)
    bo[:64, :64] = 1
    bo[64:, 64:] = 1
    c["blk64"] = bo
    c["ident"] = np.eye(128, dtype=np.float32)
    t = np.arange(NL)
    row = (t // cfg.GRID_W).astype(np.float32)
    col = (t % cfg.GRID_W).astype(np.float32)
    inv = (10000.0 ** (-np.arange(16, dtype=np.float32) / 16)).astype(np.float32)
    ang = np.concatenate([row[:, None] * inv, col[:, None] * inv], -1).astype(np.float32)
    cos, sin = np.cos(ang).astype(np.float32), np.sin(ang).astype(np.float32)
    C = np.ones((128, T), np.float32)
    S = np.zeros((128, T), np.float32)
    for p in range(128):
        i = p % 32
        C[p, :NL] = cos[:, i]
        S[p, :NL] = -sin[:, i] if (p % 64) < 32 else sin[:, i]
    c["ropeC"], c["ropeS"] = C, S
    perm = np.zeros((128, 128), np.float32)
    for m in range(128):
        k = m + 32 if (m % 64) < 32 else m - 32
        perm[k, m] = 1
    c["perm"] = perm
    TF = np.concatenate([NC + np.arange(NL), np.arange(NC)]).astype(np.float32)
    TB = np.concatenate([NC + (NL - 1 - np.arange(NL)), NC - 1 - np.arange(NC)]).astype(np.float32)
    c["tfrow"] = np.broadcast_to(TF[None, :], (128, T)).copy()
    c["tbrow"] = np.broadcast_to(TB[None, :], (128, T)).copy()
    c["tfcol"] = TF.reshape(cfg.NB, 128).T.copy()
    c["tbcol"] = TB.reshape(cfg.NB, 128).T.copy()
    mF = np.zeros((128, 4, 512), np.float32)
    mB = np.zeros((128, 4, 512), np.float32)
    s = np.arange(128)[:, None]
    tt = np.arange(512)[None, :]
    for j in range(4):
        mF[:, j, :] = np.where(128 * j + s <= tt, 0.0, NEG)
        mB[:, j, :] = np.where(128 * j + s >= tt, 0.0, NEG)
    c["maskF"], c["maskB"] = mF, mB
    c["iota"] = np.broadcast_to(np.arange(256, dtype=np.float32)[None, :], (128, 256)).copy()
    us = np.zeros((128, 128), np.float32)
    for m in range(128):
        us[:m, m] = 1
    c["ustrict"] = us
    return c


class Prog:
    def __init__(self, cfg):
        self.cfg = cfg
        self.nc = bass.Bass("TRN2", target_bir_lowering=False)
        self.es = contextlib.ExitStack()
        self.s = Sched(self.nc, self.es)
        self.dr = {}
        self.uid = 0

    def din(self, name, shape, dt=F32):
        self.dr[name] = self.nc.dram_tensor(name, list(shape), dt, kind="ExternalInput").ap()
        return self.dr[name]

    def dscr(self, name, shape, dt):
        self.dr[name] = self.nc.dram_tensor(name, list(shape), dt, kind="Internal").ap()
        return self.dr[name]

    def sb(self, st, name, shape, dt):
        self.uid += 1
        return st.enter_context(self.nc.sbuf_tensor(f"{name}_{self.uid}", list(shape), dt))

    def ps(self, st, name, shape, dt=F32):
        self.uid += 1
        full = 512 if dt == F32 else 1024
        t = st.enter_context(self.nc.psum_tensor(f"{name}_{self.uid}", [128, full], dt))
        n = 1
        for d in shape[1:]:
            n *= d
        v = t[:shape[0], :n]
        if len(shape) == 3:
            v = v.rearrange("p (a b) -> p a b", b=shape[2])
        return v

    def mm(self, out, lhsT, rhs, start, stop, r, w, inc=None):
        if inc is None:
            inc = stop
        if self.s.__dict__.get("maxops", 10**9) < 10**8 and not stop:
            inc = True
        self.s.op("pe", lambda: self.nc.tensor.matmul(out, lhsT, rhs, start=start, stop=stop), r=r, w=w, inc=inc)

    def tr(self, out, in_, ident, r, w, inc=True):
        self.s.op("pe", lambda: self.nc.tensor.transpose(out, in_, ident), r=r, w=w, inc=inc)

    def act(self, out, in_, func, r, w, bias=None, scale=None, accum_out=None):
        kw = {}
        if bias is not None:
            kw["bias"] = bias
        if scale is not None:
            kw["scale"] = scale
        if accum_out is not None:
            kw["accum_out"] = accum_out
        self.s.op("act", lambda: self.nc.scalar.activation(out=out, in_=in_, func=func, **kw), r=r, w=w)

    def ts(self, out, in0, s1, s2, op0, op1=None, r=(), w=(), eng="dve"):
        e = self.nc.vector if eng == "dve" else self.nc.gpsimd
        if op1 is None:
            self.s.op(eng, lambda: e.tensor_scalar(out=out, in0=in0, scalar1=s1, scalar2=None, op0=op0), r=r, w=w)
        else:
            self.s.op(eng, lambda: e.tensor_scalar(out=out, in0=in0, scalar1=s1, scalar2=s2, op0=op0, op1=op1), r=r, w=w)

    def tt(self, out, in0, in1, op, r, w, eng="dve"):
        e = self.nc.vector if eng == "dve" else self.nc.gpsimd
        self.s.op(eng, lambda: e.tensor_tensor(out=out, in0=in0, in1=in1, op=op), r=r, w=w)

    def stt(self, out, in0, scalar, in1, op0, op1, r, w, eng="dve"):
        e = self.nc.vector if eng == "dve" else self.nc.gpsimd
        self.s.op(eng, lambda: e.scalar_tensor_tensor(out=out, in0=in0, scalar=scalar, in1=in1, op0=op0, op1=op1), r=r, w=w)

    def cp(self, out, in_, r, w, eng="dve"):
        if eng == "act":
            self.s.op("act", lambda: self.nc.scalar.copy(out=out, in_=in_), r=r, w=w)
        else:
            e = self.nc.vector if eng == "dve" else self.nc.gpsimd
            self.s.op(eng, lambda: e.tensor_copy(out=out, in_=in_), r=r, w=w)

    def ld(self, out, in_, r, w, q="sp"):
        self.s.dma(q, out, in_, r=r, w=w)

    def load_const(self, st, name, shape, dt=F32, cast=False):
        t = self.sb(st, name, shape, BF16 if cast else dt)
        self.ld(t[:], self.dr[name][:], r=[("dram", name)], w=[("sb", t.name)], q="pool" if cast else "sp")
        return t

    def rstd(self, sq_list, lhsT, ps_ap, out_ap, addc, rsq, rps, rout):
        n = len(sq_list)
        for i, a in enumerate(sq_list):
            self.mm(ps_ap, lhsT, a, start=(i == 0), stop=(i == n - 1), r=rsq, w=rps)
        self.act(out_ap, ps_ap, AF.Sqrt, bias=self.epsc[:, self.epsidx[addc]:self.epsidx[addc] + 1], r=rps + ["epsc"], w=rout)
        self.recip(out_ap, out_ap, r=rout, w=rout)

    def declare(self):
        cfg = self.cfg
        D, T, SPC, L = cfg.D, cfg.T, cfg.SPC, cfg.DEPTH
        NV = SPC + 1
        NEV, NOD = (L + 1) // 2, max(L // 2, 1)
        d = self.din
        d("xT0", [SPC, D, T]); d("cT", [128, 8, NV]); d("ada_w", [L, D, 6 * D]); d("ada_bT", [128, L, 48])
        d("n1w", [128, L, 8]); d("n2w", [128, L, 8]); d("fnw", [128, 8])
        d("att_w_in", [NEV, D, cfg.ATT_IN]); d("att_w_out", [NEV, D, D])
        d("qnw", [128, NEV]); d("knw", [128, NEV]); d("dlam", [128, NEV, 256]); d("dnw", [128, NEV])
        d("rec_w_in", [NOD, D, cfg.REC_IN]); d("rec_w_out", [NOD, D, D])
        d("rdl", [128, NOD, 8]); d("cwT", [128, NOD, 8, 3]); d("cbT", [128, NOD, 8])
        d("dtb", [16, NOD]); d("alog", [16, NOD]); d("alogr", [128, NOD, 16]); d("dsk", [128, NOD, 4]); d("snw", [128, NOD, 4])
        d("router_w", [L, D, cfg.NE]); d("wg", [L, cfg.NE, D, cfg.FF]); d("wu", [L, cfg.NE, D, cfg.FF]); d("wd", [L, cfg.NE, cfg.FF, D])
        for k, v in _consts(cfg).items():
            d(k, v.shape)
        self.out = self.nc.dram_tensor("outT", [SPC, D, cfg.NL], F32, kind="ExternalOutput").ap()
        self.dscr("xT", [SPC, D, T], F32)
        self.dscr("qk", [SPC, 16 * 128, T], BF16)
        self.dscr("aT", [SPC, D, T], BF16)
        self.dscr("rawT", [SPC, D, T], F32)
        self.dscr("sideT", [SPC, 12 * 128, T], F32)
        self.dscr("cumT", [SPC, 16, T], F32)
        self.dscr("thr", [2, cfg.NE], F32)
        self.dscr("slotd", [SPC, 128, cfg.NB * cfg.NE], F32)
        self.dscr("gmd", [SPC, 128, cfg.NB * cfg.NE], F32)
        self.dscr("xin", [cfg.NE, D, cfg.cols], BF16)
        self.dscr("yexp", [cfg.NE, cfg.cols, D], BF16)

    def col0(self, sm, seg):
        cfg = self.cfg
        base = (sm // cfg.G) * cfg.gcols
        sl = sm % cfg.G
        return base + (sl * cfg.capl if seg == 0 else cfg.G * cfg.capl + sl * cfg.capc)

    def P(self, l, kind, k, v):
        return self.par[:, l, kind * 8 + k, v:v + 1]

    def setup(self):
        cfg, s = self.cfg, self.s
        D, L, NV = cfg.D, cfg.DEPTH, cfg.SPC + 1
        pst = self.es
        self.par = self.sb(pst, "par", [128, L, 48, NV], F32)
        self.ones_bf = self.sb(pst, "ones_bf", [128, 128], BF16)
        self.ident_bf = self.sb(pst, "ident_bf", [128, 128], BF16)
        self.ident_f = self.sb(pst, "ident_f", [128, 128], F32)
        self.fnw = self.sb(pst, "fnws", [128, 8], F32)
        self.epsc = self.sb(pst, "epsc", [128, 4], F32)
        self.epsidx = {}
        for ii, val in enumerate((D * EPS, 64 * EPS, 128 * EPS, 256 * EPS)):
            self.epsidx[val] = ii
            self.s.op("dve", lambda: self.nc.vector.memset(self.epsc[:, ii:ii + 1], val), r=[], w=["epsc"])
        self.ld(self.ones_bf[:], self.dr["ones"][:], r=[], w=["ones_bf"], q="pool")
        self.ld(self.ident_bf[:], self.dr["ident"][:], r=[], w=["ident_bf"], q="pool")
        self.ld(self.ident_f[:], self.dr["ident"][:], r=[], w=["ident_f"])
        self.ld(self.fnw[:], self.dr["fnw"][:], r=[], w=["fnw"])
        self.ts(self.fnw[:], self.fnw[:], math.sqrt(D), None, ALU.mult, r=["fnw"], w=["fnw"])
        with contextlib.ExitStack() as st:
            cT = self.sb(st, "cT", [128, 8, NV], F32)
            clT = self.sb(st, "clT", [128, 8, NV], BF16)
            n1s = self.sb(st, "n1s", [128, L, 8], F32)
            n2s = self.sb(st, "n2s", [128, L, 8], F32)
            abT = self.sb(st, "abT", [128, L, 48], F32)
            wts = [self.sb(st, f"adaw{i}", [128, 8, 768], BF16) for i in range(2)]
            pss = [self.ps(st, f"modps{i}", [128, 6, NV]) for i in range(2)]
            self.ld(cT[:], self.dr["cT"][:], r=[], w=["cT"])
            self.ld(n1s[:], self.dr["n1w"][:], r=[], w=["n1s"])
            self.ld(n2s[:], self.dr["n2w"][:], r=[], w=["n2s"])
            self.ld(abT[:], self.dr["ada_bT"][:], r=[], w=["abT"])
            self.act(clT[:], cT[:], AF.Silu, r=["cT"], w=["clT"])
            self.ts(n1s[:], n1s[:], math.sqrt(D), None, ALU.mult, r=["n1s"], w=["n1s"])
            self.ts(n2s[:], n2s[:], math.sqrt(D), None, ALU.mult, r=["n2s"], w=["n2s"])
            it = 0
            for l in range(L):
                src = self.dr["ada_w"][l].rearrange("(k p) n -> p k n", p=128)
                for g in range(8):
                    wt, pt = wts[it % 2], pss[it % 2]
                    wk, pk = ("adaw", it % 2), ("modps", it % 2)
                    it += 1
                    for k in range(8):
                        self.ld(wt[:, k, :], src[:, k, g * 768:(g + 1) * 768], r=[], w=[wk], q="pool")
                    for jj in range(6):
                        for k in range(8):
                            self.mm(pt[:, jj, :], wt[:, k, jj * 128:(jj + 1) * 128], clT[:, k, :], start=(k == 0), stop=(k == 7),
                                    r=[wk, "clT"], w=[pk], inc=(k == 7))
                    for v in range(NV):
                        self.tt(self.par[:, l, g * 6:(g + 1) * 6, v], pt[:, :, v], abT[:, l, g * 6:(g + 1) * 6], ALU.add,
                                r=[pk, "abT"], w=["par"])
                for v in range(NV):
                    self.stt(self.par[:, l, 8:16, v], self.par[:, l, 8:16, v], 1.0, n1s[:, l, :], ALU.add, ALU.mult, r=["par", "n1s"], w=["par"])
                    self.stt(self.par[:, l, 32:40, v], self.par[:, l, 32:40, v], 1.0, n2s[:, l, :], ALU.add, ALU.mult, r=["par", "n2s"], w=["par"])
            for sm in range(cfg.SPC):
                self.ld(self.dr["xT"][sm], self.dr["xT0"][sm], r=[], w=[("xT", sm)])
            s.barrier()

    def norm_mod(self, xt, xkey, w, bufs, A_fn, B_fn, out_fn, outkey):
        sq, ssps, rs, tmp = bufs["sq"], bufs["ssps"], bufs["rs"], bufs["tmp"]
        D = self.cfg.D
        self.act(sq[:, :, :w], xt[:, :, :w], AF.Square, r=[xkey], w=["nm_sq"])
        self.rstd([sq[:, k, :w] for k in range(8)], self.ones_bf[:], ssps[:, :w], rs[:, :w], D * EPS, ["nm_sq", "ones_bf"], ["nm_ss"], ["nm_rs"])
        for k in range(8):
            if B_fn is None:
                self.stt(out_fn(k), xt[:, k, :w], A_fn(k), rs[:, :w], ALU.mult, ALU.mult, r=[xkey, "nm_rs", "par", "fnw"], w=[outkey])
            else:
                self.stt(tmp[:, k, :w], xt[:, k, :w], A_fn(k), rs[:, :w], ALU.mult, ALU.mult, r=[xkey, "nm_rs", "par"], w=[("nm_tmp", k)])
                self.act(out_fn(k), tmp[:, k, :w], AF.Identity, bias=B_fn(k), r=[("nm_tmp", k), "par"], w=[outkey])

    def nm_bufs(self, st):
        return {"sq": self.sb(st, "nm_sq", [128, 8, 512], BF16), "ssps": self.ps(st, "nm_ss", [128, 512]),
                "rs": self.sb(st, "nm_rs", [128, 512], F32), "tmp": self.sb(st, "nm_tmp", [128, 8, 512], F32)}

    def phase_norm1(self, st, l, sm):
        cfg = self.cfg
        hT = self.sb(st, "hT", [128, 8, cfg.T], BF16)
        with contextlib.ExitStack() as st2:
            nb = self.nm_bufs(st2)
            xts = [self.sb(st2, f"xt{i}", [128, 8, 512], F32) for i in range(2)]
            src = self.dr["xT"][sm].rearrange("(k p) t -> p k t", p=128)
            for i, (c0, w, seg) in enumerate(cfg.tiles):
                xt, xk = xts[i % 2], ("xt", i % 2)
                v = cfg.SPC if seg == 1 else sm
                self.ld(xt[:, :, :w], src[:, :, c0:c0 + w], r=[("xT", sm)], w=[xk])
                self.norm_mod(xt, xk, w, nb, lambda k: self.P(l, 1, k, v), lambda k: self.P(l, 0, k, v),
                              lambda k: hT[:, k, c0:c0 + w], ("hT", i))
            self.s.barrier()
        return hT

    def tile_of_block(self, b):
        for i, (c0, w, seg) in enumerate(self.cfg.tiles):
            if c0 <= b * 128 < c0 + w:
                return i
        raise ValueError(b)

    def phase_inproj(self, st, l, sm, hT, wname, widx, nin, rowspec, vspec, vt):
        cfg = self.cfg
        T = cfg.T
        with contextlib.ExitStack() as st2:
            wi = self.sb(st2, "wi", [128, 8, nin], BF16)
            src = self.dr[wname][widx].rearrange("(k p) n -> p k n", p=128)
            for k in range(8):
                self.ld(wi[:, k, :], src[:, k, :], r=[], w=[("wi", k)], q="pool")
            wkeys = [("wi", k) for k in range(8)]
            C = self.load_const(st2, "ropeC", [128, T])
            S = self.load_const(st2, "ropeS", [128, T])
            blk = self.load_const(st2, "blk64", [128, 128], cast=True)
            perm = self.load_const(st2, "perm", [128, 128], cast=True)
            ck = [("sb", C.name), ("sb", S.name), ("sb", blk.name), ("sb", perm.name)]
            acc = [self.ps(st2, f"acc{i}", [128, 512]) for i in range(2)]
            ssp = self.ps(st2, "ssp", [128, 512])
            swp = self.ps(st2, "swp", [128, 512])
            sqb = [self.sb(st2, f"sqb{i}", [128, 512], BF16) for i in range(2)]
            xwb = [self.sb(st2, f"xwb{i}", [128, 512], BF16) for i in range(2)]
            rsb = [self.sb(st2, f"rsb{i}", [128, 512], F32) for i in range(2)]
            t1b = [self.sb(st2, f"t1b{i}", [128, 512], F32) for i in range(2)]
            t2b = [self.sb(st2, f"t2b{i}", [128, 512], F32) for i in range(2)]
            yb = [self.sb(st2, f"yb{i}", [128, 512], F32) for i in range(2)]
            orow = [self.sb(st2, f"orow{i}", [128, T], BF16) for i in range(2)]
            needf = any(k_ not in ("normrope", "rope") for (k_, _, _, _) in rowspec)
            frow = [self.sb(st2, "frow0", [128, T] if needf else [128, 8], F32)] * 2
            grow = [self.sb(st2, "grow0", [128, T] if needf else [128, 8], F32)] * 2
            it = 0
            import os
            ksub = int(os.environ.get("KSUB", "999"))
            for ci, (kind, j, arg, dst) in enumerate(rowspec):
                if ci >= ksub:
                    break
                M = 16 if kind == "dt" else 128
                o, ok = orow[ci % 2], ("orow", ci % 2)
                fr, fk = frow[0], ("frow", 0)
                gr, gk = grow[0], ("grow", 0)
                for ti, (c0, w, seg) in enumerate(cfg.tiles):
                    a, ak = acc[it % 2], ("acc", it % 2)
                    q = it % 2
                    it += 1
                    for k in range(8):
                        self.mm(a[:M, :w], wi[:, k, j * 128:j * 128 + M], hT[:, k, c0:c0 + w], start=(k == 0), stop=(k == 7),
                                r=[wkeys[k], ("hT", ti)], w=[ak], inc=(k == 7))
                    if kind in ("normrope", "rope"):
                        if kind == "normrope":
                            self.act(sqb[q][:, :w], a[:, :w], AF.Square, r=[ak], w=[("sqb", q)])
                            self.ts(xwb[q][:, :w], a[:, :w], arg, None, ALU.mult, r=[ak, "pcols"], w=[("xwb", q)])
                            self.rstd([sqb[q][:, :w]], blk[:], ssp[:, :w], rsb[q][:, :w], 64 * EPS, [("sqb", q), ck[2]], ["ssp"], [("rsb", q)])
                        else:
                            self.ts(xwb[q][:, :w], a[:, :w], float(arg), None, ALU.mult, r=[ak], w=[("xwb", q)])
                        self.mm(swp[:, :w], perm[:], xwb[q][:, :w], start=True, stop=True, r=[("xwb", q), ck[3]], w=["swp"])
                        self.tt(t1b[q][:, :w], xwb[q][:, :w], C[:, c0:c0 + w], ALU.mult, r=[("xwb", q), ck[0]], w=[("t1b", q)])
                        self.tt(t2b[q][:, :w], swp[:, :w], S[:, c0:c0 + w], ALU.mult, r=["swp", ck[1]], w=[("t2b", q)])
                        if kind == "normrope":
                            self.tt(yb[q][:, :w], t1b[q][:, :w], t2b[q][:, :w], ALU.add, r=[("t1b", q), ("t2b", q)], w=[("yb", q)], eng="dve")
                            self.stt(o[:, c0:c0 + w], yb[q][:, :w], 8.0, rsb[q][:, :w], ALU.mult, ALU.mult, r=[("yb", q), ("rsb", q)], w=[ok])
                        else:
                            self.tt(o[:, c0:c0 + w], t1b[q][:, :w], t2b[q][:, :w], ALU.add, r=[("t1b", q), ("t2b", q)], w=[ok], eng="dve")
                    else:
                        self.cp(fr[:M, c0:c0 + w], a[:M, :w], r=[ak], w=[fk], eng="act")
                if kind in ("normrope", "rope"):
                    self.ld(self.dr["qk"][sm, dst * 128:(dst + 1) * 128, :], o[:], r=[ok], w=[("qk", sm, dst)])
                elif kind == "side":
                    self.ld(self.dr["sideT"][sm, dst * 128:(dst + 1) * 128, :], fr[:], r=[fk], w=[("sideT", sm, dst)])
                elif kind == "conv":
                    cw, cb, cidx = arg
                    self.act(gr[:], fr[:], AF.Identity, bias=cb[:, cidx:cidx + 1], scale=cw[:, cidx, 1:2], r=[fk, "pcols"], w=[gk])
                    for (s0, n) in ((0, cfg.NL), (cfg.NL, cfg.NC)):
                        self.stt(gr[:, s0 + 1:s0 + n], fr[:, s0:s0 + n - 1], cw[:, cidx, 0:1], gr[:, s0 + 1:s0 + n], ALU.mult, ALU.add, r=[fk, gk, "pcols"], w=[gk])
                        self.stt(gr[:, s0:s0 + n - 1], fr[:, s0 + 1:s0 + n], cw[:, cidx, 2:3], gr[:, s0:s0 + n - 1], ALU.mult, ALU.add, r=[fk, gk, "pcols"], w=[gk])
                    if dst[0] == "xs":
                        self.act(fr[:], gr[:], AF.Silu, r=[gk], w=[fk])
                        self.ld(self.dr["sideT"][sm, dst[1] * 128:(dst[1] + 1) * 128, :], fr[:], r=[fk], w=[("sideT", sm, dst[1])])
                        for b in range(cfg.NB):
                            self.tr(swp[:, 0:128], fr[:, b * 128:(b + 1) * 128], self.ident_f[:], r=[fk, "ident_f"], w=["swp"])
                            self.cp(self.recst["xstok"][:, b, cidx * 128:(cidx + 1) * 128], swp[:, 0:128], r=["swp"], w=[("xstok", b)])
                    else:
                        self.act(o[:], gr[:], AF.Silu, r=[gk], w=[ok])
                        self.ld(self.dr["qk"][sm, dst[1] * 128:(dst[1] + 1) * 128, :], o[:], r=[ok], w=[("qk", sm, dst[1])])
                elif kind == "dt":
                    self.rec_dt(st2, l, sm, fr, fk, ssp, gr, gk, C, ck[0], S, ck[1])
            it = 0
            for b in range(cfg.NB if ksub > 100 or ksub < 0 else 0):
                ti = self.tile_of_block(b)
                for (c0, n, d0) in vspec:
                    a, ak = acc[it % 2], ("acc", it % 2)
                    for k in range(8):
                        self.mm(a[:, :n], hT[:, k, b * 128:(b + 1) * 128], wi[:, k, c0:c0 + n], start=(k == 0), stop=(k == 7),
                                r=[wkeys[k], ("hT", ti)], w=[ak], inc=(k == 7))
                    self.cp(vt[:, b, d0:d0 + n], a[:, :n], r=[ak], w=[("vt", b)], eng=("act" if it % 2 else "dve"))
                    it += 1
            self.s.barrier()

    def recip(self, out, in_, r, w):
        self.s.op("dve", lambda: self.nc.vector.reciprocal(out=out, in_=in_), r=r, w=w)

    def phase_attn(self, l, sm, vt):
        cfg = self.cfg
        T, NB, NLB = cfg.T, cfg.NB, cfg.NLB
        lp = self.lp
        with contextlib.ExitStack() as st2:
            KT = [self.sb(st2, f"KT{i}", [64, T], BF16) for i in range(2)]
            QT = [self.sb(st2, f"QT{i}", [64, T], BF16) for i in range(2)]
            pt = [self.sb(st2, f"pt{i}", [128, 512], BF16) for i in range(3)]
            Sps = [self.ps(st2, f"Sps{i}", [128, 512]) for i in range(2)]
            Ops = [self.ps(st2, f"Ops{i}", [128, 512]) for i in range(2)]
            Lps = [self.ps(st2, f"Lps{i}", [128, 512]) for i in range(2)]
            ssp = self.ps(st2, "assp", [128, 512])
            rl = [self.sb(st2, f"rl{i}", [128, 512], F32) for i in range(2)]
            o1 = self.sb(st2, "o1", [128, 512], F32)
            dd = self.sb(st2, "dd", [128, 512], F32)
            sqd = self.sb(st2, "sqd", [128, 512], BF16)
            rsd = self.sb(st2, "rsd", [128, 512], F32)
            o0row = self.sb(st2, "o0row", [128, T], F32)
            ob = [self.sb(st2, f"ob{i}", [128, 512], BF16) for i in range(2)]
            heads = []
            for g in range(2):
                for jj in range(4):
                    j = 4 * g + jj
                    heads.append((j * 64, 512 + g * 64, g * 64, 64, "gqa", j, 0))
            for h in range(4):
                for m in range(2):
                    heads.append((640 + h * 128 + m * 64, 1152 + h * 128 + m * 64, 128 + h * 128, 128, "diff", h, m))
            it = 0
            et = 0
            lastk = None
            nk = 0
            for hi, (qrow, krow, vc0, dv, kind, idx, m) in enumerate(heads):
                if krow != lastk:
                    nk += 1
                    lastk = krow
                    self.ld(KT[nk % 2][:], self.dr["qk"][sm, krow:krow + 64, :], r=[("qk", sm, krow // 128)], w=[("KT", nk % 2)])
                K, Kk = KT[nk % 2], ("KT", nk % 2)
                Q, Qk = QT[hi % 2], ("QT", hi % 2)
                self.ld(Q[:], self.dr["qk"][sm, qrow:qrow + 64, :], r=[("qk", sm, qrow // 128)], w=[Qk])
                for ti, (c0, w, seg) in enumerate(cfg.tiles):
                    kbs = list(range(NB)) if seg == 0 else list(range(NLB, NB))
                    O, Ok = Ops[et % 2], ("Ops", et % 2)
                    L, Lk = Lps[et % 2], ("Lps", et % 2)
                    self.mm(Sps[it % 2][:, :w], K[:, kbs[0] * 128:(kbs[0] + 1) * 128], Q[:, c0:c0 + w], True, True, r=[Kk, Qk], w=[("Sps", it % 2)])
                    for n, kb in enumerate(kbs):
                        S, Sk = Sps[it % 2], ("Sps", it % 2)
                        p, pk = pt[it % 3], ("pt", it % 3)
                        it += 1
                        if n + 1 < len(kbs):
                            kn = kbs[n + 1]
                            self.mm(Sps[it % 2][:, :w], K[:, kn * 128:(kn + 1) * 128], Q[:, c0:c0 + w], True, True, r=[Kk, Qk], w=[("Sps", it % 2)])
                        self.act(p[:, :w], S[:, :w], AF.Exp, scale=0.125, r=[Sk], w=[pk])
                        last = (n == len(kbs) - 1)
                        self.mm(O[:dv, :w], vt[:, kb, vc0:vc0 + dv], p[:, :w], start=(n == 0), stop=last, r=[pk, ("vt", kb)], w=[Ok], inc=False)
                        self.mm(L[:dv, :w], self.ones_bf[:, :dv], p[:, :w], start=(n == 0), stop=last, r=[pk, "ones_bf"], w=[Lk], inc=True)
                    r_, rk = rl[et % 2], ("rl", et % 2)
                    o_, obk = ob[et % 2], ("ob", et % 2)
                    et += 1
                    self.recip(r_[:dv, :w], L[:dv, :w], r=[Lk], w=[rk])
                    if kind == "gqa":
                        self.tt(o_[:dv, :w], O[:dv, :w], r_[:dv, :w], ALU.mult, r=[Ok, rk], w=[obk])
                        self.ld(self.dr["aT"][sm, idx * 64:(idx + 1) * 64, c0:c0 + w], o_[:dv, :w], r=[obk], w=[("aT", sm, ti)])
                    elif m == 0:
                        self.tt(o0row[:, c0:c0 + w], O[:, :w], r_[:, :w], ALU.mult, r=[Ok, rk], w=[("o0row", ti)])
                    else:
                        self.tt(o1[:, :w], O[:, :w], r_[:, :w], ALU.mult, r=[Ok, rk], w=["o1"])
                        self.stt(dd[:, :w], o1[:, :w], lp["lamneg"], o0row[:, c0:c0 + w], ALU.mult, ALU.add, r=["o1", ("o0row", ti), "pcols"], w=["dd"])
                        self.act(sqd[:, :w], dd[:, :w], AF.Square, r=["dd"], w=["sqd"])
                        self.rstd([sqd[:, :w]], self.ones_bf[:], ssp[:, :w], rsd[:, :w], 128 * EPS, ["sqd", "ones_bf"], ["assp"], ["rsd"])
                        self.stt(o_[:, :w], dd[:, :w], lp["dwv"], rsd[:, :w], ALU.mult, ALU.mult, r=["dd", "rsd", "pcols"], w=[obk])
                        self.ld(self.dr["aT"][sm, 512 + idx * 128:512 + (idx + 1) * 128, c0:c0 + w], o_[:, :w], r=[obk], w=[("aT", sm, ti)])
            self.s.barrier()

    def layer_params_even(self, l):
        i = l // 2
        lam_init = 0.8 - 0.6 * math.exp(-0.3 * l)
        pc = self.pcol
        lp = {"qn": pc[:, 0:1], "kn": pc[:, 1:2], "lamneg": pc[:, 2:3], "dwv": pc[:, 3:4]}
        with contextlib.ExitStack() as st:
            dl = self.sb(st, "dl", [128, 256], F32)
            pr = self.sb(st, "pr", [128, 128], F32)
            self.ld(dl[:], self.dr["dlam"][:, i, :], r=[], w=["dl"])
            self.ld(pc[:, 0:1], self.dr["qnw"][:, i:i + 1], r=[], w=["pcols"])
            self.ld(pc[:, 1:2], self.dr["knw"][:, i:i + 1], r=[], w=["pcols"])
            self.ld(pc[:, 3:4], self.dr["dnw"][:, i:i + 1], r=[], w=["pcols"])
            self.tt(pr[:, 0:64], dl[:, 0:64], dl[:, 64:128], ALU.mult, r=["dl"], w=["pr"])
            self.tt(pr[:, 64:128], dl[:, 128:192], dl[:, 192:256], ALU.mult, r=["dl"], w=["pr"])
            self.s.op("dve", lambda: self.nc.vector.reduce_sum(out=pc[:, 4:5], in_=pr[:, 0:64], axis=AX.X), r=["pr"], w=["pcols"])
            self.s.op("dve", lambda: self.nc.vector.reduce_sum(out=pc[:, 5:6], in_=pr[:, 64:128], axis=AX.X), r=["pr"], w=["pcols"])
            self.act(pc[:, 4:6], pc[:, 4:6], AF.Exp, r=["pcols"], w=["pcols"])
            self.tt(pc[:, 2:3], pc[:, 5:6], pc[:, 4:5], ALU.subtract, r=["pcols"], w=["pcols"])
            self.ts(pc[:, 2:3], pc[:, 2:3], -lam_init, None, ALU.add, r=["pcols"], w=["pcols"])
            self.ts(pc[:, 3:4], pc[:, 3:4], math.sqrt(128.0) * (1 - lam_init), None, ALU.mult, r=["pcols"], w=["pcols"])
            self.s.barrier()
        self.lp = lp

    def phase_outproj(self, st, l, sm, wname, widx, h2tok, afftok, odd):
        cfg = self.cfg
        T = cfg.T
        with contextlib.ExitStack() as st2:
            wo = self.sb(st2, "wo", [128, 8, cfg.D], BF16)
            wr = self.sb(st2, "wr", [128, 8, cfg.NE], BF16)
            src = self.dr[wname][widx].rearrange("(k p) n -> p k n", p=128)
            for k in range(8):
                self.ld(wo[:, k, :], src[:, k, :], r=[], w=[("wo", k)], q="pool")
            self.ld(wr[:], self.dr["router_w"][l].rearrange("(k p) n -> p k n", p=128), r=[], w=["wr"], q="pool")
            nb = self.nm_bufs(st2)
            nbf = 1 if odd else 2
            xts = [self.sb(st2, f"dxt{i}", [128, 8, 512], F32) for i in range(nbf)]
            ats = [self.sb(st2, f"dat{i}", [128, 8, 512], BF16) for i in range(nbf)]
            h2 = [self.sb(st2, f"h2{i}", [128, 8, 512], BF16) for i in range(nbf)]
            acc = [self.ps(st2, f"dacc{i}", [128, 512]) for i in range(2)]
            lps = self.ps(st2, "lps", [128, 16])
            tps = [self.ps(st2, f"tps{i}", [128, 1024], BF16) for i in range(2)]
            ex = self.sb(st2, "ex", [128, 16], F32)
            rsum = self.sb(st2, "rsum", [128, 1], F32)
            if odd:
                ob = self.odd_bufs(st2)
            xsrc = self.dr["xT"][sm].rearrange("(k p) t -> p k t", p=128)
            asrc = self.dr["aT"][sm].rearrange("(k p) t -> p k t", p=128)
            it = 0
            bt = 0
            for ti, (c0, w, seg) in enumerate(cfg.tiles):
                v = cfg.SPC if seg == 1 else sm
                xt, xk = xts[ti % nbf], ("dxt", ti % nbf)
                at, atk = ats[ti % nbf], ("dat", ti % nbf)
                hh, hk = h2[ti % nbf], ("h2", ti % nbf)
                self.ld(xt[:, :, :w], xsrc[:, :, c0:c0 + w], r=[("xT", sm)], w=[xk])
                if odd:
                    self.odd_pre(l, sm, ob, at, atk, c0, w)
                else:
                    self.ld(at[:, :, :w], asrc[:, :, c0:c0 + w], r=[("aT", sm, ti)], w=[atk])
                for j in range(8):
                    a, ak = acc[it % 2], ("dacc", it % 2)
                    it += 1
                    for k in range(8):
                        self.mm(a[:, :w], wo[:, k, j * 128:(j + 1) * 128], at[:, k, :w], start=(k == 0), stop=(k == 7),
                                r=[("wo", k), atk], w=[ak], inc=(k == 7))
                    self.stt(xt[:, j, :w], a[:, :w], self.P(l, 2, j, v), xt[:, j, :w], ALU.mult, ALU.add, r=[ak, xk, "par"], w=[xk])
                self.ld(xsrc[:, :, c0:c0 + w], xt[:, :, :w], r=[xk], w=[("xT", sm)])
                self.norm_mod(xt, xk, w, nb, lambda k: self.P(l, 4, k, v), lambda k: self.P(l, 3, k, v), lambda k: hh[:, k, :w], hk)
                for bb in range(w // 128):
                    b = (c0 // 128) + bb
                    for k in range(8):
                        self.mm(lps[:, :], hh[:, k, bb * 128:(bb + 1) * 128], wr[:, k, :], start=(k == 0), stop=(k == 7),
                                r=[hk, "wr"], w=["lps"], inc=(k == 7))
                    self.act(ex[:], lps[:], AF.Exp, r=["lps"], w=["ex"])
                    self.s.op("dve", lambda: self.nc.vector.reduce_sum(out=rsum[:], in_=ex[:], axis=AX.X), r=["ex"], w=["rsum"])
                    self.recip(rsum[:], rsum[:], r=["rsum"], w=["rsum"])
                    self.ts(afftok[:, b, :], ex[:], rsum[:, 0:1], None, ALU.mult, r=["ex", "rsum"], w=[("aff", b)])
                    tp, tk = tps[bt % 2], ("tps", bt % 2)
                    bt += 1
                    for k in range(8):
                        self.tr(tp[:, k * 128:(k + 1) * 128], hh[:, k, bb * 128:(bb + 1) * 128], self.ident_bf[:], r=[hk, "ident_bf"], w=[tk], inc=(k == 7))
                    self.cp(h2tok[:, b, :], tp[:], r=[tk], w=[("h2tok", b)], eng=("act" if bt % 2 else "dve"))
            self.s.barrier()

    def phase_route(self, l, sm, h2tok, afftok, slot, gm):
        cfg = self.cfg
        NE, NB, NLB = cfg.NE, cfg.NB, cfg.NLB
        with contextlib.ExitStack() as st2:
            affT = self.sb(st2, "affT", [NE, cfg.T], F32)
            work = self.sb(st2, "work", [NE, cfg.NL], F32)
            mx = self.sb(st2, "mx", [NE, 8], F32)
            thr = self.sb(st2, "thr", [NE, 2], F32)
            thrb = self.sb(st2, "thrb", [128, 2, NE], F32)
            mask = self.sb(st2, "mask", [128, NB, NE], BF16)
            maskf = self.sb(st2, "maskf", [128, NB, NE], F32)
            ones = self.ones_bf
            us = self.load_const(st2, "ustrict", [128, 128], cast=True)
            iota = self.load_const(st2, "iota", [128, 256])
            tp = self.ps(st2, "rtp", [NE, 128])
            sp = self.ps(st2, "rsp", [128, NE])
            gps = [self.ps(st2, f"gps{i}", [128, 256]) for i in range(2)]
            sel = [self.sb(st2, f"sel{i}", [128, NLB, cfg.capl], BF16) for i in range(2)]
            xg = [self.sb(st2, f"xg{i}", [128, 8, cfg.capl], BF16) for i in range(2)]
            for b in range(NB):
                self.tr(tp[:, :], afftok[:, b, :], self.ident_f[:], r=[("aff", b), "ident_f"], w=["rtp"])
                self.cp(affT[:, b * 128:(b + 1) * 128], tp[:, :], r=["rtp"], w=["affT"])
            for seg, (t0, n, cap) in enumerate(((0, cfg.NL, cfg.capl), (cfg.NL, cfg.NC, cfg.capc))):
                self.cp(work[:, :n], affT[:, t0:t0 + n], r=["affT"], w=["work"])
                nit = cap // 8 + 1
                for i in range(nit):
                    self.s.op("dve", lambda: self.nc.vector.max(out=mx[:], in_=work[:, :n]), r=["work"], w=["mx"])
                    if i == nit - 2:
                        self.cp(thr[:, seg:seg + 1], mx[:, 7:8], r=["mx"], w=["thr"])
                    if i < nit - 1:
                        self.s.op("dve", lambda: self.nc.vector.match_replace(out=work[:, :n], in_to_replace=mx[:], in_values=work[:, :n], imm_value=-1.0),
                                  r=["mx", "work"], w=["work"])
                self.stt(thr[:, seg:seg + 1], thr[:, seg:seg + 1], 1.0, mx[:, 0:1], ALU.mult, ALU.add, r=["thr", "mx"], w=["thr"])
            self.ts(thr[:], thr[:], 0.5, None, ALU.mult, r=["thr"], w=["thr"])
            for seg in range(2):
                self.ld(self.dr["thr"][seg].rearrange("(e o) -> e o", o=1), thr[:, seg:seg + 1], r=["thr"], w=["thr_d"])
            for seg in range(2):
                self.ld(thrb[:, seg, :], self.dr["thr"][seg].partition_broadcast(128), r=["thr_d"], w=["thrb"])
            for b in range(NB):
                seg = 0 if b < NLB else 1
                self.tt(maskf[:, b, :], afftok[:, b, :], thrb[:, seg, :], ALU.is_gt, r=[("aff", b), "thrb"], w=[("mask", b)])
                self.cp(mask[:, b, :], maskf[:, b, :], r=[("mask", b)], w=[("maskb", b)])
                self.tt(gm[:, b, :], afftok[:, b, :], maskf[:, b, :], ALU.mult, r=[("aff", b), ("mask", b)], w=[("gm", b)], eng="dve")
            for seg, blocks in enumerate((list(range(NLB)), list(range(NLB, NB)))):
                for bi, b in enumerate(blocks):
                    for bj in range(bi + 1):
                        self.mm(sp[:, :], (us if bj == bi else ones)[:], mask[:, blocks[bj], :], start=(bj == 0), stop=(bj == bi),
                                r=[("maskb", blocks[bj]), ("sb", us.name), "ones_bf"], w=["rsp"], inc=(bj == bi))
                    self.cp(slot[:, b, :], sp[:, :], r=["rsp"], w=[("slot", b)])
            it = 0
            for e in range(NE):
                for seg, (blocks, cap, col0) in enumerate(((list(range(NLB)), cfg.capl, self.col0(sm, 0)),
                                                           (list(range(NLB, NB)), cfg.capc, self.col0(sm, 1)))):
                    sl, slk = sel[it % 2], ("sel", it % 2)
                    x_, xk = xg[it % 2], ("xg", it % 2)
                    it += 1
                    for bi, b in enumerate(blocks):
                        self.ts(sl[:, bi, :cap], iota[:, :cap], slot[:, b, e:e + 1], maskf[:, b, e:e + 1], ALU.is_equal, ALU.mult,
                                r=[("slot", b), ("mask", b), ("sb", iota.name)], w=[slk])
                    for j in range(8):
                        g, gk = gps[j % 2], ("gps", j % 2)
                        for bi, b in enumerate(blocks):
                            self.mm(g[:, :cap], h2tok[:, b, j * 128:(j + 1) * 128], sl[:, bi, :cap], start=(bi == 0), stop=(bi == len(blocks) - 1),
                                    r=[("h2tok", b), slk], w=[gk], inc=(bi == len(blocks) - 1))
                        self.cp(x_[:, j, :cap], g[:, :cap], r=[gk], w=[xk], eng=("act" if j % 2 else "dve"))
                    self.ld(self.dr["xin"][e].rearrange("(k p) c -> p k c", p=128)[:, :, col0:col0 + cap], x_[:, :, :cap], r=[xk], w=[("xin", e, sm, seg)])
            self.ld(self.dr["slotd"][sm], slot[:].rearrange("p b e -> p (b e)"), r=[("slot", b) for b in range(NB)], w=[("slotd", sm)])
            self.ld(self.dr["gmd"][sm], gm[:].rearrange("p b e -> p (b e)"), r=[("gm", b) for b in range(NB)], w=[("gmd", sm)])
            self.s.barrier()

    def phase_ffn(self, l):
        for grp in range(self.cfg.SPC // self.cfg.G):
            self.go() and self.phase_ffn_g(l, grp)

    def phase_ffn_g(self, l, grp):
        cfg = self.cfg
        NE, FC, cols, D = cfg.NE, cfg.FC, cfg.gcols, cfg.D
        gb = grp * cfg.gcols
        main = min(cols, 512)
        csplit = [(0, main)] + ([(main, cols - main)] if cols > main else [])
        cchunks = [(c, min(128, cols - c)) for c in range(0, cols, 128)]
        nq = 4 if FC >= 8 else 2
        FH = (FC + nq - 1) // nq
        units = [(f0, min(FH, FC - f0)) for f0 in range(0, FC, FH)]
        with contextlib.ExitStack() as st2:
            NU = 6
            wb = [self.sb(st2, f"wb{i}", [128, 8, FH * 128], BF16) for i in range(NU)]
            NWD = 3
            wdb = [self.sb(st2, f"wdb{i}", [128, FC, 512], BF16) for i in range(NWD)]
            xin = [self.sb(st2, f"fxin{i}", [128, 8, cols], BF16) for i in range(2)]
            actT = self.sb(st2, "actT", [128, FC, cols], BF16)
            sa = [self.sb(st2, f"sa{i}", [128, cols], F32) for i in range(2)]
            ysb = [self.sb(st2, f"ysb{i}", [128, len(cchunks), 512], BF16) for i in range(2)]
            aps = [self.ps(st2, f"aps{i}", [128, 512]) for i in range(2)]
            ups = [self.ps(st2, f"ups{i}", [128, 512]) for i in range(2)]
            cps = self.ps(st2, "cps", [128, 2, 256])
            yps = [self.ps(st2, f"yps{i}", [128, 512]) for i in range(len(cchunks))] if len(cchunks) <= 3 else None
            if yps is None:
                yps = aps + ups + [self.ps(st2, "yps4", [128, 512])]
                ypk = [("aps", 0), ("aps", 1), ("ups", 0), ("ups", 1), ("yps", 4)]
            else:
                ypk = [("yps", i) for i in range(len(cchunks))]
            ui = 0
            di = 0
            fi = 0
            for e in range(NE):
                x_, xk = xin[e % 2], ("fxin", e % 2)
                self.ld(x_[:], self.dr["xin"][e].rearrange("(k p) c -> p k c", p=128)[:, :, gb:gb + cols], r=[("xin", e, sm, sg) for sm in range(cfg.SPC) for sg in range(2)], w=[xk])
                for (f0, nf) in units:
                    wgt, wgk = wb[ui % NU], ("wb", ui % NU)
                    ui += 1
                    wut, wuk = wb[ui % NU], ("wb", ui % NU)
                    ui += 1
                    for (wt, wk, name) in ((wgt, wgk, "wg"), (wut, wuk, "wu")):
                        src = self.dr[name][l, e].rearrange("(k p) f -> p k f", p=128)
                        for k in range(8):
                            self.ld(wt[:, k, :nf * 128], src[:, k, f0 * 128:(f0 + nf) * 128], r=[], w=[wk], q="pool")
                    for f in range(nf):
                        a, ak = aps[fi % 2], ("aps", fi % 2)
                        u, uk = ups[fi % 2], ("ups", fi % 2)
                        s_, sk = sa[fi % 2], ("sa", fi % 2)
                        fi += 1
                        for (ps_, pk, wt, wk, ci) in ((a, ak, wgt, wgk, 0), (u, uk, wut, wuk, 1)):
                            for (cc0, cn) in csplit:
                                tgt = ps_[:, :cn] if cc0 == 0 else cps[:, ci, :cn]
                                tk = pk if cc0 == 0 else "cps"
                                for k in range(8):
                                    self.mm(tgt, wt[:, k, f * 128:(f + 1) * 128], x_[:, k, cc0:cc0 + cn], start=(k == 0), stop=(k == 7),
                                            r=[wk, xk], w=[tk], inc=(k == 7))
                        for (cc0, cn) in csplit:
                            asrc = a[:, :cn] if cc0 == 0 else cps[:, 0, :cn]
                            usrc = u[:, :cn] if cc0 == 0 else cps[:, 1, :cn]
                            akk = ak if cc0 == 0 else "cps"
                            ukk = uk if cc0 == 0 else "cps"
                            self.act(s_[:, cc0:cc0 + cn], asrc, AF.Silu, r=[akk], w=[sk])
                            self.tt(actT[:, f0 + f, cc0:cc0 + cn], s_[:, cc0:cc0 + cn], usrc, ALU.mult, r=[sk, ukk], w=["actT"])
                for dh in range(D // 512):
                    wd_, wdk = wdb[di % NWD], ("wdb", di % NWD)
                    y_, yk = ysb[di % 2], ("ysb", di % 2)
                    di += 1
                    src = self.dr["wd"][l, e].rearrange("(f p) d -> p f d", p=128)
                    step = 4
                    for f in range(0, FC, step):
                        nf_ = min(step, FC - f)
                        self.ld(wd_[:, f:f + nf_, :], src[:, f:f + nf_, dh * 512:(dh + 1) * 512], r=[], w=[wdk], q="pool")
                    for f in range(FC):
                        for ci, (cc0, cn) in enumerate(cchunks):
                            self.mm(yps[ci][:cn, :], actT[:, f, cc0:cc0 + cn], wd_[:, f, :], start=(f == 0), stop=(f == FC - 1),
                                    r=["actT", wdk], w=[ypk[ci]], inc=(f == FC - 1))
                    for ci, (cc0, cn) in enumerate(cchunks):
                        self.cp(y_[:cn, ci, :], yps[ci][:cn, :], r=[ypk[ci]], w=[yk], eng=("act" if ci % 2 else "dve"))
                    for ci, (cc0, cn) in enumerate(cchunks):
                        self.ld(self.dr["yexp"][e, gb + cc0:gb + cc0 + cn, dh * 512:(dh + 1) * 512], y_[:cn, ci, :], r=[yk], w=[("yexp", e)])
            self.s.barrier()

    def phase_scatter(self, l, sm, slot_unused, gm_unused):
        cfg = self.cfg
        NE, NLB, NB, D = cfg.NE, cfg.NLB, cfg.NB, cfg.D
        with contextlib.ExitStack() as st2:
            slot = self.sb(st2, "sslot", [128, NB, NE], F32)
            gm = self.sb(st2, "sgm", [128, NB, NE], F32)
            self.ld(slot[:].rearrange("p b e -> p (b e)"), self.dr["slotd"][sm], r=[], w=[("slot", sm)])
            self.ld(gm[:].rearrange("p b e -> p (b e)"), self.dr["gmd"][sm], r=[], w=[("gm", sm)])
            iota = self.load_const(st2, "iota", [128, 256])
            segs = []
            for seg, (cap, col0) in enumerate(((cfg.capl, self.col0(sm, 0)), (cfg.capc, self.col0(sm, 1)))):
                ck = min(128, cap)
                ncb = cap // ck
                y = self.sb(st2, f"sy{seg}", [ck, NE * ncb, D], BF16)
                for e in range(NE):
                    for cb in range(ncb):
                        self.ld(y[:, e * ncb + cb, :], self.dr["yexp"][e, col0 + cb * ck:col0 + (cb + 1) * ck, :], r=[("yexp", e)], w=[("sy", seg)])
                segs.append((cap, ck, ncb, y))
            mxi = max(NE * sg[2] for sg in segs)
            selT = self.sb(st2, "selT", [128, mxi, 512], BF16)
            sg_ = [self.sb(st2, f"sg{i}", [128, 256], BF16) for i in range(2)]
            tps = [self.ps(st2, f"stp{i}", [128, 8, 128], BF16) for i in range(2)]
            yT = [self.ps(st2, f"syT{i}", [128, 512]) for i in range(4)]
            xts = [self.sb(st2, f"sxt{i}", [128, 8, 512], F32) for i in range(2)]
            xsrc = self.dr["xT"][sm].rearrange("(k p) t -> p k t", p=128)
            it = 0
            gi = 0
            for ti, (c0, w, seg) in enumerate(cfg.tiles):
                cap, ck, ncb, y = segs[seg]
                v = cfg.SPC if seg == 1 else sm
                nidx = NE * ncb
                xt, xk = xts[ti % 2], ("sxt", ti % 2)
                self.ld(xt[:, :, :w], xsrc[:, :, c0:c0 + w], r=[("xT", sm)], w=[xk])
                for bb in range(w // 128):
                    b = c0 // 128 + bb
                    for g0 in range(0, nidx, 8):
                        tp, tk = tps[gi % 2], ("stp", gi % 2)
                        gi += 1
                        ng = min(8, nidx - g0)
                        for q in range(ng):
                            idx = g0 + q
                            e, cb = idx // ncb, idx % ncb
                            if cb == 0:
                                sg, sgk = sg_[it % 2], ("sg", it % 2)
                                it += 1
                                self.ts(sg[:, :cap], iota[:, :cap], slot[:, b, e:e + 1], gm[:, b, e:e + 1], ALU.is_equal, ALU.mult,
                                        r=[("slot", sm), ("gm", sm), ("sb", iota.name)], w=[sgk])
                            self.tr(tp[:ck, q, :], sg[:, cb * ck:(cb + 1) * ck], self.ident_bf[:], r=[sgk, "ident_bf"], w=[tk], inc=True)
                        self.cp(selT[:ck, g0:g0 + ng, bb * 128:(bb + 1) * 128], tp[:ck, :ng, :], r=[tk], w=["selT"], eng=("act" if gi % 2 else "dve"))
                for jg in range(2):
                    for idx in range(nidx):
                        for jj in range(4):
                            j = jg * 4 + jj
                            self.mm(yT[jj][:, :w], y[:ck, idx, j * 128:(j + 1) * 128], selT[:ck, idx, :w], start=(idx == 0), stop=(idx == nidx - 1),
                                    r=[("sy", seg), "selT"], w=[("syT", jj)], inc=(idx == nidx - 1))
                    for jj in range(4):
                        j = jg * 4 + jj
                        self.stt(xt[:, j, :w], yT[jj][:, :w], self.P(l, 5, j, v), xt[:, j, :w], ALU.mult, ALU.add, r=[("syT", jj), xk, "par"], w=[xk])
                self.ld(xsrc[:, :, c0:c0 + w], xt[:, :, :w], r=[xk], w=[("xT", sm)])
            self.s.barrier()

    def phase_final(self):
        cfg = self.cfg
        with contextlib.ExitStack() as st2:
            nb = self.nm_bufs(st2)
            xts = [self.sb(st2, f"fxt{i}", [128, 8, 512], F32) for i in range(2)]
            ots = [self.sb(st2, f"fot{i}", [128, 8, 512], F32) for i in range(2)]
            it = 0
            for sm in range(cfg.SPC):
                xsrc = self.dr["xT"][sm].rearrange("(k p) t -> p k t", p=128)
                osrc = self.out[sm].rearrange("(k p) t -> p k t", p=128)
                for ti, (c0, w, seg) in enumerate(cfg.tiles):
                    if seg == 1:
                        continue
                    xt, xk = xts[it % 2], ("fxt", it % 2)
                    ot, ok = ots[it % 2], ("fot", it % 2)
                    it += 1
                    self.ld(xt[:, :, :w], xsrc[:, :, c0:c0 + w], r=[("xT", sm)], w=[xk])
                    self.norm_mod(xt, xk, w, nb, lambda k: self.fnw[:, k:k + 1], None, lambda k: ot[:, k, :w], ok)
                    self.ld(osrc[:, :, c0:c0 + w], ot[:, :, :w], r=[ok], w=[("out", sm)])
            self.s.barrier()

    def go(self):
        self.ph += 1
        return self.ph <= self.stop

    def build(self):
        cfg = self.cfg
        self.declare()
        self.pcol = self.sb(self.es, "pcol", [128, 64], F32)
        import os
        self.stop = int(os.environ.get('KSTOP', '999'))
        self.ph = 0
        self.setup()
        NB, NE = cfg.NB, cfg.NE
        slot = [None] * cfg.SPC
        gm = [None] * cfg.SPC
        for l in range(cfg.DEPTH):
            odd = (l % 2 == 1)
            i = l // 2
            if odd:
                self.go() and self.layer_params_odd(l)
            else:
                self.go() and self.layer_params_even(l)
            for sm in range(cfg.SPC):
                with contextlib.ExitStack() as st:
                    vt = self.sb(st, "vt", [128, NB, 512 if odd else 640], BF16)
                    if odd:
                        self.recst = {"xstok": self.sb(st, "xstok", [128, NB, 512], BF16), "dttok": self.sb(st, "dttok", [128, NB, 16], F32),
                                      "ncum": self.sb(st, "ncum", [128, NB, 16], F32)}
                    with contextlib.ExitStack() as st1:
                        hT = self.phase_norm1(st1, l, sm) if self.go() else None
                        if odd:
                            self.go() and self.rec_inproj(st1, l, sm, hT, vt)
                        else:
                            lp = self.lp
                            rows = [("normrope", j, lp["qn"], j) for j in range(4)] + [("normrope", 4, lp["kn"], 4)]
                            rows += [("rope", 6 + j, 1.0, 5 + j) for j in range(4)] + [("rope", 10 + j, 1.0, 9 + j) for j in range(4)]
                            self.go() and self.phase_inproj(st1, l, sm, hT, "att_w_in", i, cfg.ATT_IN, rows, [(5 * 128, 128, 0), (14 * 128, 512, 128)], vt)
                    if odd:
                        self.go() and self.phase_rec(l, sm, vt)
                    else:
                        self.go() and self.phase_attn(l, sm, vt)
                with contextlib.ExitStack() as st:
                    h2tok = self.sb(st, "h2tok", [128, NB, cfg.D], BF16)
                    afftok = self.sb(st, "afftok", [128, NB, NE], F32)
                    slot[sm] = self.sb(st, "slot", [128, NB, NE], F32)
                    gm[sm] = self.sb(st, "gm", [128, NB, NE], F32)
                    self.go() and self.phase_outproj(st, l, sm, "rec_w_out" if odd else "att_w_out", i, h2tok, afftok, odd)
                    self.go() and self.phase_route(l, sm, h2tok, afftok, slot[sm], gm[sm])
            self.phase_ffn(l)
            for sm in range(cfg.SPC):
                self.go() and self.phase_scatter(l, sm, slot[sm], gm[sm])
        self.go() and self.phase_final()
        self.es.close()
        return self.nc


def _prep(cfg, inp):
    f = np.float32
    L, SPC = cfg.DEPTH, cfg.SPC

    def colT(a):
        a = np.asarray(a, f)
        sh = a.shape
        a = a.reshape(sh[:-1] + (sh[-1] // 128, 128))
        return np.ascontiguousarray(np.moveaxis(a, -1, 0))

    def rep(a, n=128):
        a = np.asarray(a, f)
        return np.ascontiguousarray(np.broadcast_to(a[None], (n,) + a.shape))
    shared = {}
    shared["ada_w"] = np.asarray(inp["ada_w"], f)
    shared["ada_bT"] = colT(inp["ada_b"])
    shared["n1w"] = colT(inp["norm1_w"]); shared["n2w"] = colT(inp["norm2_w"]); shared["fnw"] = colT(inp["final_norm_w"])
    shared["att_w_in"] = np.asarray(inp["att_w_in"], f); shared["att_w_out"] = np.asarray(inp["att_w_out"], f)
    shared["qnw"] = np.ascontiguousarray(np.tile(np.asarray(inp["att_q_norm_w"], f), (1, 2)).T)
    shared["knw"] = np.ascontiguousarray(np.tile(np.asarray(inp["att_k_norm_w"], f), (1, 2)).T)
    shared["dlam"] = rep(np.asarray(inp["diff_lambda"], f).reshape(-1, 256))
    shared["dnw"] = np.ascontiguousarray(np.asarray(inp["diff_norm_w"], f).T)
    if L >= 2:
        shared["rec_w_in"] = np.asarray(inp["rec_w_in"], f); shared["rec_w_out"] = np.asarray(inp["rec_w_out"], f)
        shared["rdl"] = rep(np.asarray(inp["ret_decay_logit"], f).reshape(-1, 8))
        shared["cwT"] = np.ascontiguousarray(np.moveaxis(colT(np.moveaxis(np.asarray(inp["ssd_conv_w"], f), 1, 0)), 1, -1))
        shared["cbT"] = colT(inp["ssd_conv_b"])
        shared["dtb"] = np.ascontiguousarray(np.asarray(inp["ssd_dt_bias"], f).reshape(-1, 16).T)
        shared["alog"] = np.ascontiguousarray(np.asarray(inp["ssd_a_log"], f).reshape(-1, 16).T)
        shared["alogr"] = rep(np.asarray(inp["ssd_a_log"], f).reshape(-1, 16))
        shared["dsk"] = np.ascontiguousarray(np.moveaxis(np.repeat(np.asarray(inp["ssd_d_skip"], f), 64, axis=-1).reshape(-1, 4, 128), -1, 0))
        shared["snw"] = colT(inp["ssd_norm_w"])
    else:
        z = lambda *s: np.zeros(s, f)
        shared.update(rec_w_in=z(1, cfg.D, cfg.REC_IN), rec_w_out=z(1, cfg.D, cfg.D), rdl=z(128, 1, 8), cwT=z(128, 1, 8, 3), cbT=z(128, 1, 8),
                      dtb=z(16, 1), alog=z(16, 1), alogr=z(128, 1, 16), dsk=z(128, 1, 4), snw=z(128, 1, 4))
    shared["router_w"] = np.asarray(inp["router_w"], f)
    shared["wg"] = np.asarray(inp["expert_w_gate"], f); shared["wu"] = np.asarray(inp["expert_w_up"], f); shared["wd"] = np.asarray(inp["expert_w_down"], f)
    shared.update(_consts(cfg))
    x, ctx, c = np.asarray(inp["x"], f), np.asarray(inp["ctx"], f), np.asarray(inp["c"], f)
    cc = np.asarray(inp["c_ctx"], f)
    maps = []
    for core in range(cfg.NCORES):
        b0 = core * SPC
        m = dict(shared)
        m["xT0"] = np.ascontiguousarray(np.concatenate([x[b0:b0 + SPC], ctx[b0:b0 + SPC]], axis=1).transpose(0, 2, 1))
        cv = np.concatenate([c[b0:b0 + SPC], cc[None]], 0)
        m["cT"] = np.ascontiguousarray(cv.reshape(SPC + 1, 8, 128).transpose(2, 1, 0))
        maps.append(m)
    return maps


def run(cfg, inp):
    prog = Prog(cfg)
    nc = prog.build()
    maps = _prep(cfg, inp)
    res = run_bass_kernel_spmd(nc, maps, core_ids=list(range(cfg.NCORES)))
    outs = [np.asarray(r["outT"]).transpose(0, 2, 1) for r in res.results]
    return np.ascontiguousarray(np.concatenate(outs, 0)).astype(np.float32)


def kernel(**inputs):
    cfg = Cfg(SPC=2)
    return run(cfg, inputs)


def _layer_params_odd(self, l):
    i = l // 2
    pc = self.pcol
    lp = {"lg": pc[:, 8:16], "neglg": pc[:, 16:24], "dtb": pc[:16, 24:25], "aneg": pc[:16, 25:26], "isF": pc[:16, 26:27], "isB": pc[:16, 27:28],
          "one": pc[:, 63:64], "dsk": pc[:, 32:36], "snw": pc[:, 36:40]}
    if not hasattr(self, "cw"):
        self.cw = self.sb(self.es, "cw", [128, 8, 3], F32)
        self.cb = self.sb(self.es, "cb", [128, 8], F32)
    V = self.nc.vector
    self.s.op("dve", lambda: V.memset(pc[:, 63:64], 1.0), r=[], w=["pcols"])
    self.s.op("dve", lambda: V.memset(pc[:16, 26:27], 0.0), r=[], w=["pcols"])
    self.s.op("dve", lambda: V.memset(pc[:8, 26:27], 1.0), r=[], w=["pcols"])
    self.s.op("dve", lambda: V.memset(pc[:16, 27:28], 1.0), r=[], w=["pcols"])
    self.s.op("dve", lambda: V.memset(pc[:8, 27:28], 0.0), r=[], w=["pcols"])
    self.ld(pc[:, 8:16], self.dr["rdl"][:, i, :], r=[], w=["pcols"])
    self.ld(pc[:16, 24:25], self.dr["dtb"][:, i:i + 1], r=[], w=["pcols"])
    self.ld(pc[:16, 25:26], self.dr["alog"][:, i:i + 1], r=[], w=["pcols"])
    self.ld(pc[:, 32:36], self.dr["dsk"][:, i, :], r=[], w=["pcols"])
    self.ld(pc[:, 36:40], self.dr["snw"][:, i, :], r=[], w=["pcols"])
    self.ld(self.cw[:], self.dr["cwT"][:, i], r=[], w=["pcols"])
    self.ld(self.cb[:], self.dr["cbT"][:, i, :], r=[], w=["pcols"])
    self.act(pc[:, 16:24], pc[:, 8:16], AF.Exp, scale=-1.0, r=["pcols"], w=["pcols"])
    self.act(pc[:, 16:24], pc[:, 16:24], AF.Ln, bias=pc[:, 63:64], r=["pcols"], w=["pcols"])
    self.ts(pc[:, 8:16], pc[:, 16:24], -1.0, None, ALU.mult, r=["pcols"], w=["pcols"])
    self.act(pc[:16, 25:26], pc[:16, 25:26], AF.Exp, r=["pcols"], w=["pcols"])
    self.ts(pc[:16, 25:26], pc[:16, 25:26], -1.0, None, ALU.mult, r=["pcols"], w=["pcols"])
    self.ts(pc[:, 36:40], pc[:, 36:40], 16.0, None, ALU.mult, r=["pcols"], w=["pcols"])
    self.s.barrier()
    self.lp = lp


def _rec_inproj(self, st1, l, sm, hT, vt):
    cfg = self.cfg
    i = l // 2
    rows = [("rope", 0, 1.0, 0), ("rope", 1, 1.0, 1), ("rope", 2, 0.125, 2), ("rope", 3, 0.125, 3)]
    rows += [("side", 8 + c, None, c) for c in range(4)] + [("side", 12 + c, None, 4 + c) for c in range(4)]
    rows += [("conv", 16 + c, (self.cw, self.cb, c), ("xs", 8 + c)) for c in range(4)]
    rows += [("conv", 20 + c, (self.cw, self.cb, 4 + c), ("qk", 4 + c)) for c in range(4)]
    rows += [("dt", 24, None, None)]
    self.phase_inproj(st1, l, sm, hT, "rec_w_in", i, cfg.REC_IN, rows, [(4 * 128, 512, 0)], vt)


def _rec_dt(self, st2, l, sm, fr, fk, ssp, gr, gk, Cb, Ck, Sb, Sk):
    cfg = self.cfg
    T, NL, NC, NB = cfg.T, cfg.NL, cfg.NC, cfg.NB
    lp, rs = self.lp, self.recst
    with contextlib.ExitStack() as st:
        dtT, la, pa, pb = fr[:16, :], gr[:16, :], Cb[:16, :], Sb[:16, :]
        cF = self.sb(st, "cF", [16, T], F32)
        self.act(dtT[:], fr[:16, :], AF.Exp, bias=lp["dtb"], r=[fk, "pcols"], w=[fk])
        self.act(dtT[:], dtT[:], AF.Ln, bias=lp["one"][:16], r=[fk, "pcols"], w=[fk])
        self.ts(la[:], dtT[:], lp["aneg"], None, ALU.mult, r=[fk, "pcols"], w=[gk])
        cur, ck_ = la, gk
        bufs = [(pa, Ck), (pb, Sk)]
        bi = 0
        sh = 1
        while sh < max(NL, NC):
            nxt, nk = bufs[bi % 2]
            bi += 1
            for (s0, n) in ((0, NL), (NL, NC)):
                if sh < n:
                    self.tt(nxt[:, s0 + sh:s0 + n], cur[:, s0 + sh:s0 + n], cur[:, s0:s0 + n - sh], ALU.add, r=[ck_], w=[nk])
                    self.cp(nxt[:, s0:s0 + sh], cur[:, s0:s0 + sh], r=[ck_], w=[nk])
                else:
                    self.cp(nxt[:, s0:s0 + n], cur[:, s0:s0 + n], r=[ck_], w=[nk])
            cur, ck_ = nxt, nk
            sh *= 2
        P, Pk = cur, ck_
        oth, ok_ = bufs[bi % 2]
        totc, totl = P[:, T - 1:T], P[:, NL - 1:NL]
        self.ts(cF[:, 0:NL], P[:, 0:NL], totc, None, ALU.add, r=[Pk], w=["cF"])
        self.cp(cF[:, NL:T], P[:, NL:T], r=[Pk], w=["cF"])
        self.tt(oth[:], la[:], P[:], ALU.subtract, r=[gk, Pk], w=[ok_])
        self.ts(oth[:, 0:NL], oth[:, 0:NL], totc, totl, ALU.add, ALU.add, r=[ok_, Pk], w=[ok_])
        self.ts(oth[:, NL:T], oth[:, NL:T], totc, None, ALU.add, r=[ok_, Pk], w=[ok_])
        self.ts(cF[:], cF[:], lp["isF"], None, ALU.mult, r=["cF", "pcols"], w=["cF"])
        self.stt(cF[:], oth[:], lp["isB"], cF[:], ALU.mult, ALU.add, r=[ok_, "cF", "pcols"], w=["cF"])
        self.ld(self.dr["cumT"][sm], cF[:], r=["cF"], w=[("cumT", sm)])
        self.ts(la[:], cF[:], -1.0, None, ALU.mult, r=["cF"], w=[gk])
        for b in range(NB):
            self.tr(ssp[:, 0:16], dtT[:, b * 128:(b + 1) * 128], self.ident_f[:16, :16], r=[fk, "ident_f"], w=["ssp"])
            self.cp(rs["dttok"][:, b, :], ssp[:, 0:16], r=["ssp"], w=[("dttok", b)])
            self.tr(ssp[:, 16:32], la[:, b * 128:(b + 1) * 128], self.ident_f[:16, :16], r=[gk, "ident_f"], w=["ssp"])
            self.cp(rs["ncum"][:, b, :], ssp[:, 16:32], r=["ssp"], w=[("ncum", b)])


Prog.layer_params_odd = _layer_params_odd
Prog.rec_inproj = _rec_inproj
Prog.rec_dt = _rec_dt


def _vis(tf_s, tf_t):
    if tf_s.max() <= tf_t.min():
        return "full"
    if tf_s.min() > tf_t.max():
        return "none"
    return "part"


def _phase_rec(self, l, sm, vt):
    cfg = self.cfg
    T, NB, NL, NC = cfg.T, cfg.NB, cfg.NL, cfg.NC
    lp, rs = self.lp, self.recst
    cst = _consts(cfg)
    TF, TB = cst["tfrow"][0], cst["tbrow"][0]
    with contextlib.ExitStack() as st2:
        tfrow = self.load_const(st2, "tfrow", [128, T])
        tbrow = self.load_const(st2, "tbrow", [128, T])
        tfcol = self.load_const(st2, "tfcol", [128, NB])
        tbcol = self.load_const(st2, "tbcol", [128, NB])
        maskF = self.load_const(st2, "maskF", [128, 4, 512])
        maskB = self.load_const(st2, "maskB", [128, 4, 512])
        ckeys = [("sb", t.name) for t in (tfrow, tbrow, tfcol, tbcol, maskF, maskB)]
        vssd = self.sb(st2, "vssd", [128, NB, 16, 64], BF16)
        for b in range(NB):
            for dh in range(16):
                H = dh % 8
                self.ts(vssd[:, b, dh, :], rs["xstok"][:, b, H * 64:(H + 1) * 64], rs["dttok"][:, b, dh:dh + 1], None, ALU.mult,
                        r=[("xstok", b), ("dttok", b)], w=[("vssd", b)])
        KT = [self.sb(st2, f"rKT{i}", [128, T], BF16) for i in range(2)]
        QT = [self.sb(st2, f"rQT{i}", [128, T], BF16) for i in range(2)]
        nbF = self.sb(st2, "nbF", [128, NB], F32)
        nbB = self.sb(st2, "nbB", [128, NB], F32)
        Sps = [self.ps(st2, f"rS{i}", [128, 512]) for i in range(2)]
        Ops = [self.ps(st2, f"rO{i}", [128, 512]) for i in range(4)]
        Dt = [self.sb(st2, f"rD{i}", [128, 512], F32) for i in range(4)]
        pre = [self.sb(st2, f"rpre{i}", [128, 512], F32) for i in range(2)]
        pt = [self.sb(st2, f"rpt{i}", [128, 512], BF16) for i in range(3)]
        osb = [self.sb(st2, f"rosb{i}", [128, 512], F32) for i in range(2)]
        crow = [self.sb(st2, f"crow{i}", [128, 512], F32) for i in range(8)]
        cnt = {"s": 0, "d": 0, "p": 0, "pt": 0, "o": 0}

        def decay(row_ap, rowkey, scale, bias, mode, mask_ap):
            d, dk = Dt[cnt["d"] % 4], ("rD", cnt["d"] % 4)
            cnt["d"] += 1
            src, sk = row_ap, rowkey
            if mode == "part":
                p_, pk_ = pre[cnt["p"] % 2], ("rpre", cnt["p"] % 2)
                cnt["p"] += 1
                if scale is None:
                    self.tt(p_[:, :mask_ap.shape[1]], row_ap, mask_ap, ALU.add, r=[rowkey] + ckeys, w=[pk_])
                else:
                    self.stt(p_[:, :mask_ap.shape[1]], row_ap, scale, mask_ap, ALU.mult, ALU.add, r=[rowkey, "pcols"] + ckeys, w=[pk_])
                src, sk = p_[:, :mask_ap.shape[1]], pk_
                self.act(d[:, :mask_ap.shape[1]], src, AF.Exp, bias=bias, r=[sk, "nb", "pcols"] + [("ncum", b_) for b_ in range(NB)], w=[dk])
            else:
                wdt = row_ap.shape[1]
                if scale is None:
                    self.act(d[:, :wdt], src, AF.Exp, bias=bias, r=[sk, "nb"] + [("ncum", b_) for b_ in range(NB)], w=[dk])
                else:
                    self.act(d[:, :wdt], src, AF.Exp, bias=bias, scale=scale, r=[sk, "nb", "pcols"], w=[dk])
            return d, dk

        def classify(c0, w, kb):
            tt_f, tt_b = TF[c0:c0 + w], TB[c0:c0 + w]
            ts_f, ts_b = TF[kb * 128:(kb + 1) * 128], TB[kb * 128:(kb + 1) * 128]
            vf, vb = _vis(ts_f, tt_f), _vis(ts_b, tt_b)
            j = (kb * 128 - c0) // 128
            return vf, vb, j

        for h in range(4):
            K, Kk = KT[h % 2], ("rKT", h % 2)
            Q, Qk = QT[h % 2], ("rQT", h % 2)
            self.ld(K[:64, :], self.dr["qk"][sm, 256 + h * 64:256 + (h + 1) * 64, :], r=[("qk", sm, 2 + h // 2)], w=[Kk])
            self.ld(Q[:64, :], self.dr["qk"][sm, h * 64:(h + 1) * 64, :], r=[("qk", sm, h // 2)], w=[Qk])
            self.ts(nbF[:], tfcol[:], lp["neglg"][:, h:h + 1], None, ALU.mult, r=ckeys + ["pcols"], w=["nb"])
            self.ts(nbB[:], tbcol[:], lp["neglg"][:, 4 + h:5 + h], None, ALU.mult, r=ckeys + ["pcols"], w=["nb"])
            for ti, (c0, w, seg) in enumerate(cfg.tiles):
                O, Ok = Ops[cnt["o"] % 4], ("rO", cnt["o"] % 4)
                ob_, obk = osb[cnt["o"] % 2], ("rosb", cnt["o"] % 2)
                cnt["o"] += 1
                contrib = []
                for kb in range(NB):
                    vf, vb, j = classify(c0, w, kb)
                    if vf != "none" or vb != "none":
                        contrib.append((kb, vf, vb, j))
                kb0 = contrib[0][0]
                self.mm(Sps[cnt["s"] % 2][:, :w], K[:64, kb0 * 128:(kb0 + 1) * 128], Q[:64, c0:c0 + w], True, True, r=[Kk, Qk], w=[("rS", cnt["s"] % 2)])
                for n, (kb, vf, vb, j) in enumerate(contrib):
                    S, Sk = Sps[cnt["s"] % 2], ("rS", cnt["s"] % 2)
                    cnt["s"] += 1
                    if n + 1 < len(contrib):
                        kn = contrib[n + 1][0]
                        self.mm(Sps[cnt["s"] % 2][:, :w], K[:64, kn * 128:(kn + 1) * 128], Q[:64, c0:c0 + w], True, True, r=[Kk, Qk], w=[("rS", cnt["s"] % 2)])
                    ds = []
                    if vf != "none":
                        ds.append(decay(tfrow[:, c0:c0 + w], ckeys[0], lp["lg"][:, h:h + 1], nbF[:, kb:kb + 1], vf, maskF[:, j, :w] if vf == "part" else None))
                    if vb != "none":
                        ds.append(decay(tbrow[:, c0:c0 + w], ckeys[1], lp["lg"][:, 4 + h:5 + h], nbB[:, kb:kb + 1], vb, maskB[:, j, :w] if vb == "part" else None))
                    d, dk = ds[0]
                    if len(ds) == 2:
                        self.tt(d[:, :w], d[:, :w], ds[1][0][:, :w], ALU.add, r=[dk, ds[1][1]], w=[dk])
                    p, pk = pt[cnt["pt"] % 3], ("rpt", cnt["pt"] % 3)
                    cnt["pt"] += 1
                    self.tt(p[:, :w], S[:, :w], d[:, :w], ALU.mult, r=[Sk, dk], w=[pk])
                    self.mm(O[:, :w], vt[:, kb, h * 128:(h + 1) * 128], p[:, :w], start=(n == 0), stop=(n == len(contrib) - 1),
                            r=[pk, ("vt", kb)], w=[Ok], inc=True)
                self.cp(ob_[:, :w], O[:, :w], r=[Ok], w=[obk], eng="act")
                self.ld(self.dr["rawT"][sm, h * 128:(h + 1) * 128, c0:c0 + w], ob_[:, :w], r=[obk], w=[("rawT", sm, ti)])
        for g in range(2):
            K, Kk = KT[g % 2], ("rKT", g % 2)
            Q, Qk = QT[g % 2], ("rQT", g % 2)
            self.ld(K[:], self.dr["qk"][sm, (4 + g) * 128:(5 + g) * 128, :], r=[("qk", sm, 4 + g)], w=[Kk])
            self.ld(Q[:], self.dr["qk"][sm, (6 + g) * 128:(7 + g) * 128, :], r=[("qk", sm, 6 + g)], w=[Qk])
            for ti, (c0, w, seg) in enumerate(cfg.tiles):
                for hh in range(4):
                    for d_ in range(2):
                        dh = d_ * 8 + g * 4 + hh
                        self.ld(crow[d_ * 4 + hh][:, :w], self.dr["cumT"][sm, dh, c0:c0 + w].partition_broadcast(128), r=[("cumT", sm)], w=[("crow", d_ * 4 + hh)])
                contrib = []
                for kb in range(NB):
                    vf, vb, j = classify(c0, w, kb)
                    if vf != "none" or vb != "none":
                        contrib.append((kb, vf, vb, j))
                ncontrib = sum((vf != "none") + (vb != "none") for (_, vf, vb, _) in contrib)
                seen = [0] * 4
                kb0 = contrib[0][0]
                self.mm(Sps[cnt["s"] % 2][:, :w], K[:, kb0 * 128:(kb0 + 1) * 128], Q[:, c0:c0 + w], True, True, r=[Kk, Qk], w=[("rS", cnt["s"] % 2)])
                for n, (kb, vf, vb, j) in enumerate(contrib):
                    S, Sk = Sps[cnt["s"] % 2], ("rS", cnt["s"] % 2)
                    cnt["s"] += 1
                    if n + 1 < len(contrib):
                        kn = contrib[n + 1][0]
                        self.mm(Sps[cnt["s"] % 2][:, :w], K[:, kn * 128:(kn + 1) * 128], Q[:, c0:c0 + w], True, True, r=[Kk, Qk], w=[("rS", cnt["s"] % 2)])
                    for hh in range(4):
                        for d_, (vis, msk) in enumerate(((vf, maskF), (vb, maskB))):
                            if vis == "none":
                                continue
                            dh = d_ * 8 + g * 4 + hh
                            d, dk = decay(crow[d_ * 4 + hh][:, :w], ("crow", d_ * 4 + hh), None, rs["ncum"][:, kb, dh:dh + 1], vis,
                                          msk[:, j, :w] if vis == "part" else None)
                            p, pk = pt[cnt["pt"] % 3], ("rpt", cnt["pt"] % 3)
                            cnt["pt"] += 1
                            self.tt(p[:, :w], S[:, :w], d[:, :w], ALU.mult, r=[Sk, dk], w=[pk])
                            self.mm(Ops[hh][:64, :w], vssd[:, kb, dh, :], p[:, :w], start=(seen[hh] == 0), stop=(seen[hh] == ncontrib - 1),
                                    r=[pk, ("vssd", kb)], w=[("rO", hh)], inc=True)
                            seen[hh] += 1
                for hh in range(4):
                    H = g * 4 + hh
                    ob_, obk = osb[hh % 2], ("rosb", hh % 2)
                    self.cp(ob_[:64, :w], Ops[hh][:64, :w], r=[("rO", hh)], w=[obk], eng="act")
                    self.ld(self.dr["rawT"][sm, 512 + H * 64:512 + (H + 1) * 64, c0:c0 + w], ob_[:64, :w], r=[obk], w=[("rawT", sm, ti)])
        self.s.barrier()


def _odd_bufs(self, st2):
    return {"raw": self.sb(st2, "oraw", [128, 8, 512], F32), "side": self.sb(st2, "oside", [128, 12, 512], F32),
            "sq": self.sb(st2, "osq", [128, 4, 512], BF16), "rs": self.sb(st2, "ors", [128, 512], F32),
            "sg": self.sb(st2, "osg", [128, 512], F32), "t": self.sb(st2, "ot", [128, 512], F32),
            "s2": self.sb(st2, "os2", [128, 4, 512], F32), "ssp": self.ps(st2, "otp", [128, 512])}


def _odd_pre(self, l, sm, ob, at, atk, c0, w):
    lp = self.lp
    raw, side, sq, rs_, sg, t, s2, ssp = (ob[k] for k in ("raw", "side", "sq", "rs", "sg", "t", "s2", "ssp"))
    rsrc = self.dr["rawT"][sm].rearrange("(k p) t -> p k t", p=128)
    ssrc = self.dr["sideT"][sm].rearrange("(k p) t -> p k t", p=128)
    self.ld(raw[:, :, :w], rsrc[:, :, c0:c0 + w], r=[("rawT", sm)], w=["oraw"])
    self.ld(side[:, :, :w], ssrc[:, :, c0:c0 + w], r=[("sideT", sm)], w=["oside"])
    for c in range(4):
        self.act(sq[:, 0, :w], raw[:, c, :w], AF.Square, r=["oraw"], w=["osq"])
        self.rstd([sq[:, 0, :w]], self.ones_bf[:], ssp[:, :w], rs_[:, :w], 128 * EPS, ["osq", "ones_bf"], ["otp"], ["ors"])
        self.act(sg[:, :w], side[:, c, :w], AF.Silu, r=["oside"], w=["osg"])
        self.tt(t[:, :w], raw[:, c, :w], rs_[:, :w], ALU.mult, r=["oraw", "ors"], w=["ot"])
        self.stt(at[:, c, :w], t[:, :w], math.sqrt(128.0), sg[:, :w], ALU.mult, ALU.mult, r=["ot", "osg"], w=[atk])
    for c in range(4):
        self.stt(t[:, :w], side[:, 8 + c, :w], lp["dsk"][:, c:c + 1], raw[:, 4 + c, :w], ALU.mult, ALU.add, r=["oside", "oraw", "pcols"], w=["ot"])
        self.act(sg[:, :w], side[:, 4 + c, :w], AF.Silu, r=["oside"], w=["osg"])
        self.tt(s2[:, c, :w], t[:, :w], sg[:, :w], ALU.mult, r=["ot", "osg"], w=["os2"])
        self.act(sq[:, c, :w], s2[:, c, :w], AF.Square, r=["os2"], w=["osq"])
    for gg in range(2):
        self.rstd([sq[:, 2 * gg, :w], sq[:, 2 * gg + 1, :w]], self.ones_bf[:], ssp[:, :w], rs_[:, :w], 256 * EPS, ["osq", "ones_bf"], ["otp"], ["ors"])
        for c in (2 * gg, 2 * gg + 1):
            self.stt(at[:, 4 + c, :w], s2[:, c, :w], lp["snw"][:, c:c + 1], rs_[:, :w], ALU.mult, ALU.mult, r=["os2", "ors", "pcols"], w=[atk])


Prog.phase_rec = _phase_rec
Prog.odd_bufs = _odd_bufs
Prog.odd_pre = _odd_pre
```

```python
import math
import contextlib
import numpy as np
import concourse.bass as bass
import concourse.mybir as mybir
from concourse.bass_utils import run_bass_kernel_spmd

F32 = mybir.dt.float32
BF16 = mybir.dt.bfloat16
AF = mybir.ActivationFunctionType
ALU = mybir.AluOpType
AX = mybir.AxisListType
NEG = -30000.0
EPS = 1e-6


class Cfg:
    def __init__(self, BATCH=16, SEQ=2048, DEPTH=4, CTX=256, FF=2816, SPC=2, NE=16):
        self.D = 1024
        self.BATCH, self.NL, self.DEPTH, self.NC, self.FF, self.SPC, self.NE = BATCH, SEQ, DEPTH, CTX, FF, SPC, NE
        self.T = SEQ + CTX
        self.NCORES = BATCH // SPC
        self.GRID_W = 64
        self.KD = 8
        self.ATT_IN = 2304
        self.REC_IN = 3088
        self.capl = 2 * SEQ // NE
        self.capc = 2 * CTX // NE
        self.NLB = SEQ // 128
        self.NCB = CTX // 128
        self.NB = self.NLB + self.NCB
        self.tiles = []
        for c0 in range(0, SEQ, 512):
            self.tiles.append((c0, min(512, SEQ - c0), 0))
        for c0 in range(0, CTX, 512):
            self.tiles.append((SEQ + c0, min(512, CTX - c0), 1))
        self.FC = FF // 128
        self.G = min(SPC, 2)
        self.gcols = self.G * (self.capl + self.capc)
        self.cols = (SPC // self.G) * self.gcols


class Buf:
    __slots__ = ("w", "r")

    def __init__(self):
        self.w = None
        self.r = {}


class Sched:
    ENG = ("pe", "act", "dve", "pool", "sp")
    LIMIT = 20000
    NS = 12

    def __init__(self, nc, es):
        self.nc, self.es = nc, es
        self.E = {"pe": nc.tensor, "act": nc.scalar, "dve": nc.vector, "pool": nc.gpsimd, "sp": nc.sync}
        self.sems = {}
        self.cnt = {e: 0 for e in self.ENG}
        self.epoch = {e: 0 for e in self.ENG}
        self.pending = {e: False for e in self.ENG}
        self.seen = {e: {} for e in self.ENG}
        self.latest = {}
        self.bufs = {}
        self.dmacnt = {e: 0 for e in self.ENG}
        self.nins = 0

    def semh(self, k):
        if k not in self.sems:
            self.sems[k] = self.es.enter_context(self.nc.semaphore("s_" + "_".join(str(x) for x in k)))
        return self.sems[k]

    PSUM_NAMES = {"acc", "ssp", "swp", "nm_ss", "modps", "Sps", "Ops", "Lps", "assp", "dacc", "lps", "tps", "rtp", "rsp", "gps",
                  "aps", "ups", "cps", "yps", "stp", "syT", "rS", "rO", "otp"}

    def _split(self, r, w):
        r2, w2 = [], list(w)
        for key in r:
            name = key if isinstance(key, str) else key[0]
            if name in self.PSUM_NAMES:
                if key not in w2:
                    w2.append(key)
            else:
                r2.append(key)
        return r2, w2

    def _need(self, eng, r, w):
        need = {}

        def req(tok):
            if tok is None:
                return
            k, v = tok
            if eng == "pe" and k[0] == "pe":
                return
            if need.get(k, 0) < v:
                need[k] = v
        for key in r:
            b = self.bufs.get(key)
            if b is None:
                b = self.bufs[key] = Buf()
            req(b.w)
        for key in w:
            b = self.bufs.get(key)
            if b is None:
                b = self.bufs[key] = Buf()
            name = key if isinstance(key, str) else key[0]
            same_ok = (name not in self.PSUM_NAMES) and eng in ("act", "dve")
            if not (same_ok and b.w is not None and b.w[0][0] == eng):
                req(b.w)
            for tok in b.r.values():
                if same_ok and tok[0][0] == eng:
                    continue
                req(tok)
        seen = self.seen[eng]
        for k, v in need.items():
            if seen.get(k, 0) < v:
                self.E[eng].wait_ge(self.semh(k), v)
                seen[k] = v
                self.nins += 1

    def _mark(self, tok, r, w):
        k, v = tok
        if self.latest.get(k, 0) < v:
            self.latest[k] = v
        for key in r:
            self.bufs[key].r[k] = tok
        for key in w:
            b = self.bufs[key]
            b.w = tok
            b.r = {}

    def skip(self):
        import os
        if not hasattr(self, "maxops"):
            self.maxops = int(os.environ.get("KOPS", "100000000"))
            self.opc = 0
        self.opc += 1
        import os
        if os.environ.get("KDBG") and abs(self.opc - self.maxops) < 8:
            import traceback
            fr = traceback.extract_stack()[-4]
            print("OP", self.opc, fr.lineno, fr.line)
        if self.opc == self.maxops:
            print("LAST OP before cutoff ^^^")
        return self.opc > self.maxops

    def op(self, eng, fn, r=(), w=(), inc=True):
        if self.skip():
            return
        r, w = self._split(r, w)
        self._need(eng, r, w)
        ins = fn()
        self.nins += 1
        k = (eng, self.epoch[eng])
        if inc:
            self.cnt[eng] += 1
            ins.then_inc(self.semh(k), 1)
            self.pending[eng] = False
            tok = (k, self.cnt[eng])
        else:
            self.pending[eng] = True
            tok = (k, self.cnt[eng] + 1)
        self._mark(tok, r, w)
        if inc and self.cnt[eng] >= self.LIMIT:
            self.epoch[eng] += 1
            self.cnt[eng] = 0

    def dma(self, q, out, in_, r=(), w=()):
        if self.skip():
            return
        r, w = self._split(r, w)
        self._need(q, r, w)
        j = self.dmacnt[q]
        self.dmacnt[q] += 1
        slot, rnd = j % self.NS, j // self.NS
        k = ("dma", q, slot)
        if rnd > 0 and self.seen[q].get(k, 0) < 16 * rnd:
            self.E[q].wait_ge(self.semh(k), 16 * rnd)
            self.seen[q][k] = 16 * rnd
        self.E[q].dma_start(out=out, in_=in_, allow_slow_non_contiguous=True).then_inc(self.semh(k), 16)
        self.nins += 1
        self._mark((k, 16 * (rnd + 1)), r, w)

    def barrier(self):
        for e in self.ENG:
            assert not self.pending[e], e
        for e in self.ENG:
            seen = self.seen[e]
            for k, v in self.latest.items():
                if e == "pe" and k[0] == "pe":
                    continue
                if seen.get(k, 0) < v:
                    self.E[e].wait_ge(self.semh(k), v)
                    seen[k] = v
                    self.nins += 1
        self.bufs = {}


def _consts(cfg):
    NL, NC, T = cfg.NL, cfg.NC, cfg.T
    c = {}
    c["ones"] = np.ones((128, 128), np.float32)
    bo = np.zeros((128, 128), np.float32)
    bo[:64, :64] = 1
    bo[64:, 64:] = 1
    c["blk64"] = bo
    c["ident"] = np.eye(128, dtype=np.float32)
    t = np.arange(NL)
    row = (t // cfg.GRID_W).astype(np.float32)
    col = (t % cfg.GRID_W).astype(np.float32)
    inv = (10000.0 ** (-np.arange(16, dtype=np.float32) / 16)).astype(np.float32)
    ang = np.concatenate([row[:, None] * inv, col[:, None] * inv], -1).astype(np.float32)
    cos, sin = np.cos(ang).astype(np.float32), np.sin(ang).astype(np.float32)
    C = np.ones((128, T), np.float32)
    S = np.zeros((128, T), np.float32)
    for p in range(128):
        i = p % 32
        C[p, :NL] = cos[:, i]
        S[p, :NL] = -sin[:, i] if (p % 64) < 32 else sin[:, i]
    c["ropeC"], c["ropeS"] = C, S
    perm = np.zeros((128, 128), np.float32)
    for m in range(128):
        k = m + 32 if (m % 64) < 32 else m - 32
        perm[k, m] = 1
    c["perm"] = perm
    TF = np.concatenate([NC + np.arange(NL), np.arange(NC)]).astype(np.float32)
    TB = np.concatenate([NC + (NL - 1 - np.arange(NL)), NC - 1 - np.arange(NC)]).astype(np.float32)
    c["tfrow"] = np.broadcast_to(TF[None, :], (128, T)).copy()
    c["tbrow"] = np.broadcast_to(TB[None, :], (128, T)).copy()
    c["tfcol"] = TF.reshape(cfg.NB, 128).T.copy()
    c["tbcol"] = TB.reshape(cfg.NB, 128).T.copy()
    mF = np.zeros((128, 4, 512), np.float32)
    mB = np.zeros((128, 4, 512), np.float32)
    s = np.arange(128)[:, None]
    tt = np.arange(512)[None, :]
    for j in range(4):
        mF[:, j, :] = np.where(128 * j + s <= tt, 0.0, NEG)
        mB[:, j, :] = np.where(128 * j + s >= tt, 0.0, NEG)
    c["maskF"], c["maskB"] = mF, mB
    c["iota"] = np.broadcast_to(np.arange(256, dtype=np.float32)[None, :], (128, 256)).copy()
    us = np.zeros((128, 128), np.float32)
    for m in range(128):
        us[:m, m] = 1
    c["ustrict"] = us
    return c


class Prog:
    def __init__(self, cfg):
        self.cfg = cfg
        self.nc = bass.Bass("TRN2", target_bir_lowering=False)
        self.es = contextlib.ExitStack()
        self.s = Sched(self.nc, self.es)
        self.dr = {}
        self.uid = 0

    def din(self, name, shape, dt=F32):
        self.dr[name] = self.nc.dram_tensor(name, list(shape), dt, kind="ExternalInput").ap()
        return self.dr[name]

    def dscr(self, name, shape, dt):
        self.dr[name] = self.nc.dram_tensor(name, list(shape), dt, kind="Internal").ap()
        return self.dr[name]

    def sb(self, st, name, shape, dt):
        self.uid += 1
        return st.enter_context(self.nc.sbuf_tensor(f"{name}_{self.uid}", list(shape), dt))

    def ps(self, st, name, shape, dt=F32):
        self.uid += 1
        full = 512 if dt == F32 else 1024
        t = st.enter_context(self.nc.psum_tensor(f"{name}_{self.uid}", [128, full], dt))
        n = 1
        for d in shape[1:]:
            n *= d
        v = t[:shape[0], :n]
        if len(shape) == 3:
            v = v.rearrange("p (a b) -> p a b", b=shape[2])
        return v

    def mm(self, out, lhsT, rhs, start, stop, r, w, inc=None):
        if inc is None:
            inc = stop
        if self.s.__dict__.get("maxops", 10**9) < 10**8 and not stop:
            inc = True
        self.s.op("pe", lambda: self.nc.tensor.matmul(out, lhsT, rhs, start=start, stop=stop), r=r, w=w, inc=inc)

    def tr(self, out, in_, ident, r, w, inc=True):
        self.s.op("pe", lambda: self.nc.tensor.transpose(out, in_, ident), r=r, w=w, inc=inc)

    def act(self, out, in_, func, r, w, bias=None, scale=None, accum_out=None):
        kw = {}
        if bias is not None:
            kw["bias"] = bias
        if scale is not None:
            kw["scale"] = scale
        if accum_out is not None:
            kw["accum_out"] = accum_out
        self.s.op("act", lambda: self.nc.scalar.activation(out=out, in_=in_, func=func, **kw), r=r, w=w)

    def ts(self, out, in0, s1, s2, op0, op1=None, r=(), w=(), eng="dve"):
        e = self.nc.vector if eng == "dve" else self.nc.gpsimd
        if op1 is None:
            self.s.op(eng, lambda: e.tensor_scalar(out=out, in0=in0, scalar1=s1, scalar2=None, op0=op0), r=r, w=w)
        else:
            self.s.op(eng, lambda: e.tensor_scalar(out=out, in0=in0, scalar1=s1, scalar2=s2, op0=op0, op1=op1), r=r, w=w)

    def tt(self, out, in0, in1, op, r, w, eng="dve"):
        e = self.nc.vector if eng == "dve" else self.nc.gpsimd
        self.s.op(eng, lambda: e.tensor_tensor(out=out, in0=in0, in1=in1, op=op), r=r, w=w)

    def stt(self, out, in0, scalar, in1, op0, op1, r, w, eng="dve"):
        e = self.nc.vector if eng == "dve" else self.nc.gpsimd
        self.s.op(eng, lambda: e.scalar_tensor_tensor(out=out, in0=in0, scalar=scalar, in1=in1, op0=op0, op1=op1), r=r, w=w)

    def cp(self, out, in_, r, w, eng="dve"):
        if eng == "act":
            self.s.op("act", lambda: self.nc.scalar.copy(out=out, in_=in_), r=r, w=w)
        else:
            e = self.nc.vector if eng == "dve" else self.nc.gpsimd
            self.s.op(eng, lambda: e.tensor_copy(out=out, in_=in_), r=r, w=w)

    def ld(self, out, in_, r, w, q="sp"):
        self.s.dma(q, out, in_, r=r, w=w)

    def load_const(self, st, name, shape, dt=F32, cast=False):
        t = self.sb(st, name, shape, BF16 if cast else dt)
        self.ld(t[:], self.dr[name][:], r=[("dram", name)], w=[("sb", t.name)], q="pool" if cast else "sp")
        return t

    def rstd(self, sq_list, lhsT, ps_ap, out_ap, addc, rsq, rps, rout):
        n = len(sq_list)
        for i, a in enumerate(sq_list):
            self.mm(ps_ap, lhsT, a, start=(i == 0), stop=(i == n - 1), r=rsq, w=rps)
        self.act(out_ap, ps_ap, AF.Sqrt, bias=self.epsc[:, self.epsidx[addc]:self.epsidx[addc] + 1], r=rps + ["epsc"], w=rout)
        self.recip(out_ap, out_ap, r=rout, w=rout)

    def declare(self):
        cfg = self.cfg
        D, T, SPC, L = cfg.D, cfg.T, cfg.SPC, cfg.DEPTH
        NV = SPC + 1
        NEV, NOD = (L + 1) // 2, max(L // 2, 1)
        d = self.din
        d("xT0", [SPC, D, T]); d("cT", [128, 8, NV]); d("ada_w", [L, D, 6 * D]); d("ada_bT", [128, L, 48])
        d("n1w", [128, L, 8]); d("n2w", [128, L, 8]); d("fnw", [128, 8])
        d("att_w_in", [NEV, D, cfg.ATT_IN]); d("att_w_out", [NEV, D, D])
        d("qnw", [128, NEV]); d("knw", [128, NEV]); d("dlam", [128, NEV, 256]); d("dnw", [128, NEV])
        d("rec_w_in", [NOD, D, cfg.REC_IN]); d("rec_w_out", [NOD, D, D])
        d("rdl", [128, NOD, 8]); d("cwT", [128, NOD, 8, 3]); d("cbT", [128, NOD, 8])
        d("dtb", [16, NOD]); d("alog", [16, NOD]); d("alogr", [128, NOD, 16]); d("dsk", [128, NOD, 4]); d("snw", [128, NOD, 4])
        d("router_w", [L, D, cfg.NE]); d("wg", [L, cfg.NE, D, cfg.FF]); d("wu", [L, cfg.NE, D, cfg.FF]); d("wd", [L, cfg.NE, cfg.FF, D])
        for k, v in _consts(cfg).items():
            d(k, v.shape)
        self.out = self.nc.dram_tensor("outT", [SPC, D, cfg.NL], F32, kind="ExternalOutput").ap()
        self.dscr("xT", [SPC, D, T], F32)
        self.dscr("qk", [SPC, 16 * 128, T], BF16)
        self.dscr("aT", [SPC, D, T], BF16)
        self.dscr("rawT", [SPC, D, T], F32)
        self.dscr("sideT", [SPC, 12 * 128, T], F32)
        self.dscr("cumT", [SPC, 16, T], F32)
        self.dscr("thr", [2, cfg.NE], F32)
        self.dscr("slotd", [SPC, 128, cfg.NB * cfg.NE], F32)
        self.dscr("gmd", [SPC, 128, cfg.NB * cfg.NE], F32)
        self.dscr("xin", [cfg.NE, D, cfg.cols], BF16)
        self.dscr("yexp", [cfg.NE, cfg.cols, D], BF16)

    def col0(self, sm, seg):
        cfg = self.cfg
        base = (sm // cfg.G) * cfg.gcols
        sl = sm % cfg.G
        return base + (sl * cfg.capl if seg == 0 else cfg.G * cfg.capl + sl * cfg.capc)

    def P(self, l, kind, k, v):
        return self.par[:, l, kind * 8 + k, v:v + 1]

    def setup(self):
        cfg, s = self.cfg, self.s
        D, L, NV = cfg.D, cfg.DEPTH, cfg.SPC + 1
        pst = self.es
        self.par = self.sb(pst, "par", [128, L, 48, NV], F32)
        self.ones_bf = self.sb(pst, "ones_bf", [128, 128], BF16)
        self.ident_bf = self.sb(pst, "ident_bf", [128, 128], BF16)
        self.ident_f = self.sb(pst, "ident_f", [128, 128], F32)
        self.fnw = self.sb(pst, "fnws", [128, 8], F32)
        self.epsc = self.sb(pst, "epsc", [128, 4], F32)
        self.epsidx = {}
        for ii, val in enumerate((D * EPS, 64 * EPS, 128 * EPS, 256 * EPS)):
            self.epsidx[val] = ii
            self.s.op("dve", lambda: self.nc.vector.memset(self.epsc[:, ii:ii + 1], val), r=[], w=["epsc"])
        self.ld(self.ones_bf[:], self.dr["ones"][:], r=[], w=["ones_bf"], q="pool")
        self.ld(self.ident_bf[:], self.dr["ident"][:], r=[], w=["ident_bf"], q="pool")
        self.ld(self.ident_f[:], self.dr["ident"][:], r=[], w=["ident_f"])
        self.ld(self.fnw[:], self.dr["fnw"][:], r=[], w=["fnw"])
        self.ts(self.fnw[:], self.fnw[:], math.sqrt(D), None, ALU.mult, r=["fnw"], w=["fnw"])
        with contextlib.ExitStack() as st:
            cT = self.sb(st, "cT", [128, 8, NV], F32)
            clT = self.sb(st, "clT", [128, 8, NV], BF16)
            n1s = self.sb(st, "n1s", [128, L, 8], F32)
            n2s = self.sb(st, "n2s", [128, L, 8], F32)
            abT = self.sb(st, "abT", [128, L, 48], F32)
            wts = [self.sb(st, f"adaw{i}", [128, 8, 768], BF16) for i in range(2)]
            pss = [self.ps(st, f"modps{i}", [128, 6, NV]) for i in range(2)]
            self.ld(cT[:], self.dr["cT"][:], r=[], w=["cT"])
            self.ld(n1s[:], self.dr["n1w"][:], r=[], w=["n1s"])
            self.ld(n2s[:], self.dr["n2w"][:], r=[], w=["n2s"])
            self.ld(abT[:], self.dr["ada_bT"][:], r=[], w=["abT"])
            self.act(clT[:], cT[:], AF.Silu, r=["cT"], w=["clT"])
            self.ts(n1s[:], n1s[:], math.sqrt(D), None, ALU.mult, r=["n1s"], w=["n1s"])
            self.ts(n2s[:], n2s[:], math.sqrt(D), None, ALU.mult, r=["n2s"], w=["n2s"])
            it = 0
            for l in range(L):
                src = self.dr["ada_w"][l].rearrange("(k p) n -> p k n", p=128)
                for g in range(8):
                    wt, pt = wts[it % 2], pss[it % 2]
                    wk, pk = ("adaw", it % 2), ("modps", it % 2)
                    it += 1
                    for k in range(8):
                        self.ld(wt[:, k, :], src[:, k, g * 768:(g + 1) * 768], r=[], w=[wk], q="pool")
                    for jj in range(6):
                        for k in range(8):
                            self.mm(pt[:, jj, :], wt[:, k, jj * 128:(jj + 1) * 128], clT[:, k, :], start=(k == 0), stop=(k == 7),
                                    r=[wk, "clT"], w=[pk], inc=(k == 7))
                    for v in range(NV):
                        self.tt(self.par[:, l, g * 6:(g + 1) * 6, v], pt[:, :, v], abT[:, l, g * 6:(g + 1) * 6], ALU.add,
                                r=[pk, "abT"], w=["par"])
                for v in range(NV):
                    self.stt(self.par[:, l, 8:16, v], self.par[:, l, 8:16, v], 1.0, n1s[:, l, :], ALU.add, ALU.mult, r=["par", "n1s"], w=["par"])
                    self.stt(self.par[:, l, 32:40, v], self.par[:, l, 32:40, v], 1.0, n2s[:, l, :], ALU.add, ALU.mult, r=["par", "n2s"], w=["par"])
            for sm in range(cfg.SPC):
                self.ld(self.dr["xT"][sm], self.dr["xT0"][sm], r=[], w=[("xT", sm)])
            s.barrier()

    def norm_mod(self, xt, xkey, w, bufs, A_fn, B_fn, out_fn, outkey):
        sq, ssps, rs, tmp = bufs["sq"], bufs["ssps"], bufs["rs"], bufs["tmp"]
        D = self.cfg.D
        self.act(sq[:, :, :w], xt[:, :, :w], AF.Square, r=[xkey], w=["nm_sq"])
        self.rstd([sq[:, k, :w] for k in range(8)], self.ones_bf[:], ssps[:, :w], rs[:, :w], D * EPS, ["nm_sq", "ones_bf"], ["nm_ss"], ["nm_rs"])
        for k in range(8):
            if B_fn is None:
                self.stt(out_fn(k), xt[:, k, :w], A_fn(k), rs[:, :w], ALU.mult, ALU.mult, r=[xkey, "nm_rs", "par", "fnw"], w=[outkey])
            else:
                self.stt(tmp[:, k, :w], xt[:, k, :w], A_fn(k), rs[:, :w], ALU.mult, ALU.mult, r=[xkey, "nm_rs", "par"], w=[("nm_tmp", k)])
                self.act(out_fn(k), tmp[:, k, :w], AF.Identity, bias=B_fn(k), r=[("nm_tmp", k), "par"], w=[outkey])

    def nm_bufs(self, st):
        return {"sq": self.sb(st, "nm_sq", [128, 8, 512], BF16), "ssps": self.ps(st, "nm_ss", [128, 512]),
                "rs": self.sb(st, "nm_rs", [128, 512], F32), "tmp": self.sb(st, "nm_tmp", [128, 8, 512], F32)}

    def phase_norm1(self, st, l, sm):
        cfg = self.cfg
        hT = self.sb(st, "hT", [128, 8, cfg.T], BF16)
        with contextlib.ExitStack() as st2:
            nb = self.nm_bufs(st2)
            xts = [self.sb(st2, f"xt{i}", [128, 8, 512], F32) for i in range(2)]
            src = self.dr["xT"][sm].rearrange("(k p) t -> p k t", p=128)
            for i, (c0, w, seg) in enumerate(cfg.tiles):
                xt, xk = xts[i % 2], ("xt", i % 2)
                v = cfg.SPC if seg == 1 else sm
                self.ld(xt[:, :, :w], src[:, :, c0:c0 + w], r=[("xT", sm)], w=[xk])
                self.norm_mod(xt, xk, w, nb, lambda k: self.P(l, 1, k, v), lambda k: self.P(l, 0, k, v),
                              lambda k: hT[:, k, c0:c0 + w], ("hT", i))
            self.s.barrier()
        return hT

    def tile_of_block(self, b):
        for i, (c0, w, seg) in enumerate(self.cfg.tiles):
            if c0 <= b * 128 < c0 + w:
                return i
        raise ValueError(b)

    def phase_inproj(self, st, l, sm, hT, wname, widx, nin, rowspec, vspec, vt):
        cfg = self.cfg
        T = cfg.T
        with contextlib.ExitStack() as st2:
            wi = self.sb(st2, "wi", [128, 8, nin], BF16)
            src = self.dr[wname][widx].rearrange("(k p) n -> p k n", p=128)
            for k in range(8):
                self.ld(wi[:, k, :], src[:, k, :], r=[], w=[("wi", k)], q="pool")
            wkeys = [("wi", k) for k in range(8)]
            C = self.load_const(st2, "ropeC", [128, T])
            S = self.load_const(st2, "ropeS", [128, T])
            blk = self.load_const(st2, "blk64", [128, 128], cast=True)
            perm = self.load_const(st2, "perm", [128, 128], cast=True)
            ck = [("sb", C.name), ("sb", S.name), ("sb", blk.name), ("sb", perm.name)]
            acc = [self.ps(st2, f"acc{i}", [128, 512]) for i in range(2)]
            ssp = self.ps(st2, "ssp", [128, 512])
            swp = self.ps(st2, "swp", [128, 512])
            sqb = [self.sb(st2, f"sqb{i}", [128, 512], BF16) for i in range(2)]
            xwb = [self.sb(st2, f"xwb{i}", [128, 512], BF16) for i in range(2)]
            rsb = [self.sb(st2, f"rsb{i}", [128, 512], F32) for i in range(2)]
            t1b = [self.sb(st2, f"t1b{i}", [128, 512], F32) for i in range(2)]
            t2b = [self.sb(st2, f"t2b{i}", [128, 512], F32) for i in range(2)]
            yb = [self.sb(st2, f"yb{i}", [128, 512], F32) for i in range(2)]
            orow = [self.sb(st2, f"orow{i}", [128, T], BF16) for i in range(2)]
            needf = any(k_ not in ("normrope", "rope") for (k_, _, _, _) in rowspec)
            frow = [self.sb(st2, "frow0", [128, T] if needf else [128, 8], F32)] * 2
            grow = [self.sb(st2, "grow0", [128, T] if needf else [128, 8], F32)] * 2
            it = 0
            import os
            ksub = int(os.environ.get("KSUB", "999"))
            for ci, (kind, j, arg, dst) in enumerate(rowspec):
                if ci >= ksub:
                    break
                M = 16 if kind == "dt" else 128
                o, ok = orow[ci % 2], ("orow", ci % 2)
                fr, fk = frow[0], ("frow", 0)
                gr, gk = grow[0], ("grow", 0)
                for ti, (c0, w, seg) in enumerate(cfg.tiles):
                    a, ak = acc[it % 2], ("acc", it % 2)
                    q = it % 2
                    it += 1
                    for k in range(8):
                        self.mm(a[:M, :w], wi[:, k, j * 128:j * 128 + M], hT[:, k, c0:c0 + w], start=(k == 0), stop=(k == 7),
                                r=[wkeys[k], ("hT", ti)], w=[ak], inc=(k == 7))
                    if kind in ("normrope", "rope"):
                        if kind == "normrope":
                            self.act(sqb[q][:, :w], a[:, :w], AF.Square, r=[ak], w=[("sqb", q)])
                            self.ts(xwb[q][:, :w], a[:, :w], arg, None, ALU.mult, r=[ak, "pcols"], w=[("xwb", q)])
                            self.rstd([sqb[q][:, :w]], blk[:], ssp[:, :w], rsb[q][:, :w], 64 * EPS, [("sqb", q), ck[2]], ["ssp"], [("rsb", q)])
                        else:
                            self.ts(xwb[q][:, :w], a[:, :w], float(arg), None, ALU.mult, r=[ak], w=[("xwb", q)])
                        self.mm(swp[:, :w], perm[:], xwb[q][:, :w], start=True, stop=True, r=[("xwb", q), ck[3]], w=["swp"])
                        self.tt(t1b[q][:, :w], xwb[q][:, :w], C[:, c0:c0 + w], ALU.mult, r=[("xwb", q), ck[0]], w=[("t1b", q)])
                        self.tt(t2b[q][:, :w], swp[:, :w], S[:, c0:c0 + w], ALU.mult, r=["swp", ck[1]], w=[("t2b", q)])
                        if kind == "normrope":
                            self.tt(yb[q][:, :w], t1b[q][:, :w], t2b[q][:, :w], ALU.add, r=[("t1b", q), ("t2b", q)], w=[("yb", q)], eng="dve")
                            self.stt(o[:, c0:c0 + w], yb[q][:, :w], 8.0, rsb[q][:, :w], ALU.mult, ALU.mult, r=[("yb", q), ("rsb", q)], w=[ok])
                        else:
                            self.tt(o[:, c0:c0 + w], t1b[q][:, :w], t2b[q][:, :w], ALU.add, r=[("t1b", q), ("t2b", q)], w=[ok], eng="dve")
                    else:
                        self.cp(fr[:M, c0:c0 + w], a[:M, :w], r=[ak], w=[fk], eng="act")
                if kind in ("normrope", "rope"):
                    self.ld(self.dr["qk"][sm, dst * 128:(dst + 1) * 128, :], o[:], r=[ok], w=[("qk", sm, dst)])
                elif kind == "side":
                    self.ld(self.dr["sideT"][sm, dst * 128:(dst + 1) * 128, :], fr[:], r=[fk], w=[("sideT", sm, dst)])
                elif kind == "conv":
                    cw, cb, cidx = arg
                    self.act(gr[:], fr[:], AF.Identity, bias=cb[:, cidx:cidx + 1], scale=cw[:, cidx, 1:2], r=[fk, "pcols"], w=[gk])
                    for (s0, n) in ((0, cfg.NL), (cfg.NL, cfg.NC)):
                        self.stt(gr[:, s0 + 1:s0 + n], fr[:, s0:s0 + n - 1], cw[:, cidx, 0:1], gr[:, s0 + 1:s0 + n], ALU.mult, ALU.add, r=[fk, gk, "pcols"], w=[gk])
                        self.stt(gr[:, s0:s0 + n - 1], fr[:, s0 + 1:s0 + n], cw[:, cidx, 2:3], gr[:, s0:s0 + n - 1], ALU.mult, ALU.add, r=[fk, gk, "pcols"], w=[gk])
                    if dst[0] == "xs":
                        self.act(fr[:], gr[:], AF.Silu, r=[gk], w=[fk])
                        self.ld(self.dr["sideT"][sm, dst[1] * 128:(dst[1] + 1) * 128, :], fr[:], r=[fk], w=[("sideT", sm, dst[1])])
                        for b in range(cfg.NB):
                            self.tr(swp[:, 0:128], fr[:, b * 128:(b + 1) * 128], self.ident_f[:], r=[fk, "ident_f"], w=["swp"])
                            self.cp(self.recst["xstok"][:, b, cidx * 128:(cidx + 1) * 128], swp[:, 0:128], r=["swp"], w=[("xstok", b)])
                    else:
                        self.act(o[:], gr[:], AF.Silu, r=[gk], w=[ok])
                        self.ld(self.dr["qk"][sm, dst[1] * 128:(dst[1] + 1) * 128, :], o[:], r=[ok], w=[("qk", sm, dst[1])])
                elif kind == "dt":
                    self.rec_dt(st2, l, sm, fr, fk, ssp, gr, gk, C, ck[0], S, ck[1])
            it = 0
            for b in range(cfg.NB if ksub > 100 or ksub < 0 else 0):
                ti = self.tile_of_block(b)
                for (c0, n, d0) in vspec:
                    a, ak = acc[it % 2], ("acc", it % 2)
                    for k in range(8):
                        self.mm(a[:, :n], hT[:, k, b * 128:(b + 1) * 128], wi[:, k, c0:c0 + n], start=(k == 0), stop=(k == 7),
                                r=[wkeys[k], ("hT", ti)], w=[ak], inc=(k == 7))
                    self.cp(vt[:, b, d0:d0 + n], a[:, :n], r=[ak], w=[("vt", b)], eng=("act" if it % 2 else "dve"))
                    it += 1
            self.s.barrier()

    def recip(self, out, in_, r, w):
        self.s.op("dve", lambda: self.nc.vector.reciprocal(out=out, in_=in_), r=r, w=w)

    def phase_attn(self, l, sm, vt):
        cfg = self.cfg
        T, NB, NLB = cfg.T, cfg.NB, cfg.NLB
        lp = self.lp
        with contextlib.ExitStack() as st2:
            KT = [self.sb(st2, f"KT{i}", [64, T], BF16) for i in range(2)]
            QT = [self.sb(st2, f"QT{i}", [64, T], BF16) for i in range(2)]
            pt = [self.sb(st2, f"pt{i}", [128, 512], BF16) for i in range(3)]
            Sps = [self.ps(st2, f"Sps{i}", [128, 512]) for i in range(2)]
            Ops = [self.ps(st2, f"Ops{i}", [128, 512]) for i in range(2)]
            Lps = [self.ps(st2, f"Lps{i}", [128, 512]) for i in range(2)]
            ssp = self.ps(st2, "assp", [128, 512])
            rl = [self.sb(st2, f"rl{i}", [128, 512], F32) for i in range(2)]
            o1 = self.sb(st2, "o1", [128, 512], F32)
            dd = self.sb(st2, "dd", [128, 512], F32)
            sqd = self.sb(st2, "sqd", [128, 512], BF16)
            rsd = self.sb(st2, "rsd", [128, 512], F32)
            o0row = self.sb(st2, "o0row", [128, T], F32)
            ob = [self.sb(st2, f"ob{i}", [128, 512], BF16) for i in range(2)]
            heads = []
            for g in range(2):
                for jj in range(4):
                    j = 4 * g + jj
                    heads.append((j * 64, 512 + g * 64, g * 64, 64, "gqa", j, 0))
            for h in range(4):
                for m in range(2):
                    heads.append((640 + h * 128 + m * 64, 1152 + h * 128 + m * 64, 128 + h * 128, 128, "diff", h, m))
            it = 0
            et = 0
            lastk = None
            nk = 0
            for hi, (qrow, krow, vc0, dv, kind, idx, m) in enumerate(heads):
                if krow != lastk:
                    nk += 1
                    lastk = krow
                    self.ld(KT[nk % 2][:], self.dr["qk"][sm, krow:krow + 64, :], r=[("qk", sm, krow // 128)], w=[("KT", nk % 2)])
                K, Kk = KT[nk % 2], ("KT", nk % 2)
                Q, Qk = QT[hi % 2], ("QT", hi % 2)
                self.ld(Q[:], self.dr["qk"][sm, qrow:qrow + 64, :], r=[("qk", sm, qrow // 128)], w=[Qk])
                for ti, (c0, w, seg) in enumerate(cfg.tiles):
                    kbs = list(range(NB)) if seg == 0 else list(range(NLB, NB))
                    O, Ok = Ops[et % 2], ("Ops", et % 2)
                    L, Lk = Lps[et % 2], ("Lps", et % 2)
                    self.mm(Sps[it % 2][:, :w], K[:, kbs[0] * 128:(kbs[0] + 1) * 128], Q[:, c0:c0 + w], True, True, r=[Kk, Qk], w=[("Sps", it % 2)])
                    for n, kb in enumerate(kbs):
                        S, Sk = Sps[it % 2], ("Sps", it % 2)
                        p, pk = pt[it % 3], ("pt", it % 3)
                        it += 1
                        if n + 1 < len(kbs):
                            kn = kbs[n + 1]
                            self.mm(Sps[it % 2][:, :w], K[:, kn * 128:(kn + 1) * 128], Q[:, c0:c0 + w], True, True, r=[Kk, Qk], w=[("Sps", it % 2)])
                        self.act(p[:, :w], S[:, :w], AF.Exp, scale=0.125, r=[Sk], w=[pk])
                        last = (n == len(kbs) - 1)
                        self.mm(O[:dv, :w], vt[:, kb, vc0:vc0 + dv], p[:, :w], start=(n == 0), stop=last, r=[pk, ("vt", kb)], w=[Ok], inc=False)
                        self.mm(L[:dv, :w], self.ones_bf[:, :dv], p[:, :w], start=(n == 0), stop=last, r=[pk, "ones_bf"], w=[Lk], inc=True)
                    r_, rk = rl[et % 2], ("rl", et % 2)
                    o_, obk = ob[et % 2], ("ob", et % 2)
                    et += 1
                    self.recip(r_[:dv, :w], L[:dv, :w], r=[Lk], w=[rk])
                    if kind == "gqa":
                        self.tt(o_[:dv, :w], O[:dv, :w], r_[:dv, :w], ALU.mult, r=[Ok, rk], w=[obk])
                        self.ld(self.dr["aT"][sm, idx * 64:(idx + 1) * 64, c0:c0 + w], o_[:dv, :w], r=[obk], w=[("aT", sm, ti)])
                    elif m == 0:
                        self.tt(o0row[:, c0:c0 + w], O[:, :w], r_[:, :w], ALU.mult, r=[Ok, rk], w=[("o0row", ti)])
                    else:
                        self.tt(o1[:, :w], O[:, :w], r_[:, :w], ALU.mult, r=[Ok, rk], w=["o1"])
                        self.stt(dd[:, :w], o1[:, :w], lp["lamneg"], o0row[:, c0:c0 + w], ALU.mult, ALU.add, r=["o1", ("o0row", ti), "pcols"], w=["dd"])
                        self.act(sqd[:, :w], dd[:, :w], AF.Square, r=["dd"], w=["sqd"])
                        self.rstd([sqd[:, :w]], self.ones_bf[:], ssp[:, :w], rsd[:, :w], 128 * EPS, ["sqd", "ones_bf"], ["assp"], ["rsd"])
                        self.stt(o_[:, :w], dd[:, :w], lp["dwv"], rsd[:, :w], ALU.mult, ALU.mult, r=["dd", "rsd", "pcols"], w=[obk])
                        self.ld(self.dr["aT"][sm, 512 + idx * 128:512 + (idx + 1) * 128, c0:c0 + w], o_[:, :w], r=[obk], w=[("aT", sm, ti)])
            self.s.barrier()

    def layer_params_even(self, l):
        i = l // 2
        lam_init = 0.8 - 0.6 * math.exp(-0.3 * l)
        pc = self.pcol
        lp = {"qn": pc[:, 0:1], "kn": pc[:, 1:2], "lamneg": pc[:, 2:3], "dwv": pc[:, 3:4]}
        with contextlib.ExitStack() as st:
            dl = self.sb(st, "dl", [128, 256], F32)
            pr = self.sb(st, "pr", [128, 128], F32)
            self.ld(dl[:], self.dr["dlam"][:, i, :], r=[], w=["dl"])
            self.ld(pc[:, 0:1], self.dr["qnw"][:, i:i + 1], r=[], w=["pcols"])
            self.ld(pc[:, 1:2], self.dr["knw"][:, i:i + 1], r=[], w=["pcols"])
            self.ld(pc[:, 3:4], self.dr["dnw"][:, i:i + 1], r=[], w=["pcols"])
            self.tt(pr[:, 0:64], dl[:, 0:64], dl[:, 64:128], ALU.mult, r=["dl"], w=["pr"])
            self.tt(pr[:, 64:128], dl[:, 128:192], dl[:, 192:256], ALU.mult, r=["dl"], w=["pr"])
            self.s.op("dve", lambda: self.nc.vector.reduce_sum(out=pc[:, 4:5], in_=pr[:, 0:64], axis=AX.X), r=["pr"], w=["pcols"])
            self.s.op("dve", lambda: self.nc.vector.reduce_sum(out=pc[:, 5:6], in_=pr[:, 64:128], axis=AX.X), r=["pr"], w=["pcols"])
            self.act(pc[:, 4:6], pc[:, 4:6], AF.Exp, r=["pcols"], w=["pcols"])
            self.tt(pc[:, 2:3], pc[:, 5:6], pc[:, 4:5], ALU.subtract, r=["pcols"], w=["pcols"])
            self.ts(pc[:, 2:3], pc[:, 2:3], -lam_init, None, ALU.add, r=["pcols"], w=["pcols"])
            self.ts(pc[:, 3:4], pc[:, 3:4], math.sqrt(128.0) * (1 - lam_init), None, ALU.mult, r=["pcols"], w=["pcols"])
            self.s.barrier()
        self.lp = lp

    def phase_outproj(self, st, l, sm, wname, widx, h2tok, afftok, odd):
        cfg = self.cfg
        T = cfg.T
        with contextlib.ExitStack() as st2:
            wo = self.sb(st2, "wo", [128, 8, cfg.D], BF16)
            wr = self.sb(st2, "wr", [128, 8, cfg.NE], BF16)
            src = self.dr[wname][widx].rearrange("(k p) n -> p k n", p=128)
            for k in range(8):
                self.ld(wo[:, k, :], src[:, k, :], r=[], w=[("wo", k)], q="pool")
            self.ld(wr[:], self.dr["router_w"][l].rearrange("(k p) n -> p k n", p=128), r=[], w=["wr"], q="pool")
            nb = self.nm_bufs(st2)
            nbf = 1 if odd else 2
            xts = [self.sb(st2, f"dxt{i}", [128, 8, 512], F32) for i in range(nbf)]
            ats = [self.sb(st2, f"dat{i}", [128, 8, 512], BF16) for i in range(nbf)]
            h2 = [self.sb(st2, f"h2{i}", [128, 8, 512], BF16) for i in range(nbf)]
            acc = [self.ps(st2, f"dacc{i}", [128, 512]) for i in range(2)]
            lps = self.ps(st2, "lps", [128, 16])
            tps = [self.ps(st2, f"tps{i}", [128, 1024], BF16) for i in range(2)]
            ex = self.sb(st2, "ex", [128, 16], F32)
            rsum = self.sb(st2, "rsum", [128, 1], F32)
            if odd:
                ob = self.odd_bufs(st2)
            xsrc = self.dr["xT"][sm].rearrange("(k p) t -> p k t", p=128)
            asrc = self.dr["aT"][sm].rearrange("(k p) t -> p k t", p=128)
            it = 0
            bt = 0
            for ti, (c0, w, seg) in enumerate(cfg.tiles):
                v = cfg.SPC if seg == 1 else sm
                xt, xk = xts[ti % nbf], ("dxt", ti % nbf)
                at, atk = ats[ti % nbf], ("dat", ti % nbf)
                hh, hk = h2[ti % nbf], ("h2", ti % nbf)
                self.ld(xt[:, :, :w], xsrc[:, :, c0:c0 + w], r=[("xT", sm)], w=[xk])
                if odd:
                    self.odd_pre(l, sm, ob, at, atk, c0, w)
                else:
                    self.ld(at[:, :, :w], asrc[:, :, c0:c0 + w], r=[("aT", sm, ti)], w=[atk])
                for j in range(8):
                    a, ak = acc[it % 2], ("dacc", it % 2)
                    it += 1
                    for k in range(8):
                        self.mm(a[:, :w], wo[:, k, j * 128:(j + 1) * 128], at[:, k, :w], start=(k == 0), stop=(k == 7),
                                r=[("wo", k), atk], w=[ak], inc=(k == 7))
                    self.stt(xt[:, j, :w], a[:, :w], self.P(l, 2, j, v), xt[:, j, :w], ALU.mult, ALU.add, r=[ak, xk, "par"], w=[xk])
                self.ld(xsrc[:, :, c0:c0 + w], xt[:, :, :w], r=[xk], w=[("xT", sm)])
                self.norm_mod(xt, xk, w, nb, lambda k: self.P(l, 4, k, v), lambda k: self.P(l, 3, k, v), lambda k: hh[:, k, :w], hk)
                for bb in range(w // 128):
                    b = (c0 // 128) + bb
                    for k in range(8):
                        self.mm(lps[:, :], hh[:, k, bb * 128:(bb + 1) * 128], wr[:, k, :], start=(k == 0), stop=(k == 7),
                                r=[hk, "wr"], w=["lps"], inc=(k == 7))
                    self.act(ex[:], lps[:], AF.Exp, r=["lps"], w=["ex"])
                    self.s.op("dve", lambda: self.nc.vector.reduce_sum(out=rsum[:], in_=ex[:], axis=AX.X), r=["ex"], w=["rsum"])
                    self.recip(rsum[:], rsum[:], r=["rsum"], w=["rsum"])
                    self.ts(afftok[:, b, :], ex[:], rsum[:, 0:1], None, ALU.mult, r=["ex", "rsum"], w=[("aff", b)])
                    tp, tk = tps[bt % 2], ("tps", bt % 2)
                    bt += 1
                    for k in range(8):
                        self.tr(tp[:, k * 128:(k + 1) * 128], hh[:, k, bb * 128:(bb + 1) * 128], self.ident_bf[:], r=[hk, "ident_bf"], w=[tk], inc=(k == 7))
                    self.cp(h2tok[:, b, :], tp[:], r=[tk], w=[("h2tok", b)], eng=("act" if bt % 2 else "dve"))
            self.s.barrier()

    def phase_route(self, l, sm, h2tok, afftok, slot, gm):
        cfg = self.cfg
        NE, NB, NLB = cfg.NE, cfg.NB, cfg.NLB
        with contextlib.ExitStack() as st2:
            affT = self.sb(st2, "affT", [NE, cfg.T], F32)
            work = self.sb(st2, "work", [NE, cfg.NL], F32)
            mx = self.sb(st2, "mx", [NE, 8], F32)
            thr = self.sb(st2, "thr", [NE, 2], F32)
            thrb = self.sb(st2, "thrb", [128, 2, NE], F32)
            mask = self.sb(st2, "mask", [128, NB, NE], BF16)
            maskf = self.sb(st2, "maskf", [128, NB, NE], F32)
            ones = self.ones_bf
            us = self.load_const(st2, "ustrict", [128, 128], cast=True)
            iota = self.load_const(st2, "iota", [128, 256])
            tp = self.ps(st2, "rtp", [NE, 128])
            sp = self.ps(st2, "rsp", [128, NE])
            gps = [self.ps(st2, f"gps{i}", [128, 256]) for i in range(2)]
            sel = [self.sb(st2, f"sel{i}", [128, NLB, cfg.capl], BF16) for i in range(2)]
            xg = [self.sb(st2, f"xg{i}", [128, 8, cfg.capl], BF16) for i in range(2)]
            for b in range(NB):
                self.tr(tp[:, :], afftok[:, b, :], self.ident_f[:], r=[("aff", b), "ident_f"], w=["rtp"])
                self.cp(affT[:, b * 128:(b + 1) * 128], tp[:, :], r=["rtp"], w=["affT"])
            for seg, (t0, n, cap) in enumerate(((0, cfg.NL, cfg.capl), (cfg.NL, cfg.NC, cfg.capc))):
                self.cp(work[:, :n], affT[:, t0:t0 + n], r=["affT"], w=["work"])
                nit = cap // 8 + 1
                for i in range(nit):
                    self.s.op("dve", lambda: self.nc.vector.max(out=mx[:], in_=work[:, :n]), r=["work"], w=["mx"])
                    if i == nit - 2:
                        self.cp(thr[:, seg:seg + 1], mx[:, 7:8], r=["mx"], w=["thr"])
                    if i < nit - 1:
                        self.s.op("dve", lambda: self.nc.vector.match_replace(out=work[:, :n], in_to_replace=mx[:], in_values=work[:, :n], imm_value=-1.0),
                                  r=["mx", "work"], w=["work"])
                self.stt(thr[:, seg:seg + 1], thr[:, seg:seg + 1], 1.0, mx[:, 0:1], ALU.mult, ALU.add, r=["thr", "mx"], w=["thr"])
            self.ts(thr[:], thr[:], 0.5, None, ALU.mult, r=["thr"], w=["thr"])
            for seg in range(2):
                self.ld(self.dr["thr"][seg].rearrange("(e o) -> e o", o=1), thr[:, seg:seg + 1], r=["thr"], w=["thr_d"])
            for seg in range(2):
                self.ld(thrb[:, seg, :], self.dr["thr"][seg].partition_broadcast(128), r=["thr_d"], w=["thrb"])
            for b in range(NB):
                seg = 0 if b < NLB else 1
                self.tt(maskf[:, b, :], afftok[:, b, :], thrb[:, seg, :], ALU.is_gt, r=[("aff", b), "thrb"], w=[("mask", b)])
                self.cp(mask[:, b, :], maskf[:, b, :], r=[("mask", b)], w=[("maskb", b)])
                self.tt(gm[:, b, :], afftok[:, b, :], maskf[:, b, :], ALU.mult, r=[("aff", b), ("mask", b)], w=[("gm", b)], eng="dve")
            for seg, blocks in enumerate((list(range(NLB)), list(range(NLB, NB)))):
                for bi, b in enumerate(blocks):
                    for bj in range(bi + 1):
                        self.mm(sp[:, :], (us if bj == bi else ones)[:], mask[:, blocks[bj], :], start=(bj == 0), stop=(bj == bi),
                                r=[("maskb", blocks[bj]), ("sb", us.name), "ones_bf"], w=["rsp"], inc=(bj == bi))
                    self.cp(slot[:, b, :], sp[:, :], r=["rsp"], w=[("slot", b)])
            it = 0
            for e in range(NE):
                for seg, (blocks, cap, col0) in enumerate(((list(range(NLB)), cfg.capl, self.col0(sm, 0)),
                                                           (list(range(NLB, NB)), cfg.capc, self.col0(sm, 1)))):
                    sl, slk = sel[it % 2], ("sel", it % 2)
                    x_, xk = xg[it % 2], ("xg", it % 2)
                    it += 1
                    for bi, b in enumerate(blocks):
                        self.ts(sl[:, bi, :cap], iota[:, :cap], slot[:, b, e:e + 1], maskf[:, b, e:e + 1], ALU.is_equal, ALU.mult,
                                r=[("slot", b), ("mask", b), ("sb", iota.name)], w=[slk])
                    for j in range(8):
                        g, gk = gps[j % 2], ("gps", j % 2)
                        for bi, b in enumerate(blocks):
                            self.mm(g[:, :cap], h2tok[:, b, j * 128:(j + 1) * 128], sl[:, bi, :cap], start=(bi == 0), stop=(bi == len(blocks) - 1),
                                    r=[("h2tok", b), slk], w=[gk], inc=(bi == len(blocks) - 1))
                        self.cp(x_[:, j, :cap], g[:, :cap], r=[gk], w=[xk], eng=("act" if j % 2 else "dve"))
                    self.ld(self.dr["xin"][e].rearrange("(k p) c -> p k c", p=128)[:, :, col0:col0 + cap], x_[:, :, :cap], r=[xk], w=[("xin", e, sm, seg)])
            self.ld(self.dr["slotd"][sm], slot[:].rearrange("p b e -> p (b e)"), r=[("slot", b) for b in range(NB)], w=[("slotd", sm)])
            self.ld(self.dr["gmd"][sm], gm[:].rearrange("p b e -> p (b e)"), r=[("gm", b) for b in range(NB)], w=[("gmd", sm)])
            self.s.barrier()

    def phase_ffn(self, l):
        for grp in range(self.cfg.SPC // self.cfg.G):
            self.go() and self.phase_ffn_g(l, grp)

    def phase_ffn_g(self, l, grp):
        cfg = self.cfg
        NE, FC, cols, D = cfg.NE, cfg.FC, cfg.gcols, cfg.D
        gb = grp * cfg.gcols
        main = min(cols, 512)
        csplit = [(0, main)] + ([(main, cols - main)] if cols > main else [])
        cchunks = [(c, min(128, cols - c)) for c in range(0, cols, 128)]
        nq = 4 if FC >= 8 else 2
        FH = (FC + nq - 1) // nq
        units = [(f0, min(FH, FC - f0)) for f0 in range(0, FC, FH)]
        with contextlib.ExitStack() as st2:
            NU = 6
            wb = [self.sb(st2, f"wb{i}", [128, 8, FH * 128], BF16) for i in range(NU)]
            NWD = 3
            wdb = [self.sb(st2, f"wdb{i}", [128, FC, 512], BF16) for i in range(NWD)]
            xin = [self.sb(st2, f"fxin{i}", [128, 8, cols], BF16) for i in range(2)]
            actT = self.sb(st2, "actT", [128, FC, cols], BF16)
            sa = [self.sb(st2, f"sa{i}", [128, cols], F32) for i in range(2)]
            ysb = [self.sb(st2, f"ysb{i}", [128, len(cchunks), 512], BF16) for i in range(2)]
            aps = [self.ps(st2, f"aps{i}", [128, 512]) for i in range(2)]
            ups = [self.ps(st2, f"ups{i}", [128, 512]) for i in range(2)]
            cps = self.ps(st2, "cps", [128, 2, 256])
            yps = [self.ps(st2, f"yps{i}", [128, 512]) for i in range(len(cchunks))] if len(cchunks) <= 3 else None
            if yps is None:
                yps = aps + ups + [self.ps(st2, "yps4", [128, 512])]
                ypk = [("aps", 0), ("aps", 1), ("ups", 0), ("ups", 1), ("yps", 4)]
            else:
                ypk = [("yps", i) for i in range(len(cchunks))]
            ui = 0
            di = 0
            fi = 0
            for e in range(NE):
                x_, xk = xin[e % 2], ("fxin", e % 2)
                self.ld(x_[:], self.dr["xin"][e].rearrange("(k p) c -> p k c", p=128)[:, :, gb:gb + cols], r=[("xin", e, sm, sg) for sm in range(cfg.SPC) for sg in range(2)], w=[xk])
                for (f0, nf) in units:
                    wgt, wgk = wb[ui % NU], ("wb", ui % NU)
                    ui += 1
                    wut, wuk = wb[ui % NU], ("wb", ui % NU)
                    ui += 1
                    for (wt, wk, name) in ((wgt, wgk, "wg"), (wut, wuk, "wu")):
                        src = self.dr[name][l, e].rearrange("(k p) f -> p k f", p=128)
                        for k in range(8):
                            self.ld(wt[:, k, :nf * 128], src[:, k, f0 * 128:(f0 + nf) * 128], r=[], w=[wk], q="pool")
                    for f in range(nf):
                        a, ak = aps[fi % 2], ("aps", fi % 2)
                        u, uk = ups[fi % 2], ("ups", fi % 2)
                        s_, sk = sa[fi % 2], ("sa", fi % 2)
                        fi += 1
                        for (ps_, pk, wt, wk, ci) in ((a, ak, wgt, wgk, 0), (u, uk, wut, wuk, 1)):
                            for (cc0, cn) in csplit:
                                tgt = ps_[:, :cn] if cc0 == 0 else cps[:, ci, :cn]
                                tk = pk if cc0 == 0 else "cps"
                                for k in range(8):
                                    self.mm(tgt, wt[:, k, f * 128:(f + 1) * 128], x_[:, k, cc0:cc0 + cn], start=(k == 0), stop=(k == 7),
                                            r=[wk, xk], w=[tk], inc=(k == 7))
                        for (cc0, cn) in csplit:
                            asrc = a[:, :cn] if cc0 == 0 else cps[:, 0, :cn]
                            usrc = u[:, :cn] if cc0 == 0 else cps[:, 1, :cn]
                            akk = ak if cc0 == 0 else "cps"
                            ukk = uk if cc0 == 0 else "cps"
                            self.act(s_[:, cc0:cc0 + cn], asrc, AF.Silu, r=[akk], w=[sk])
                            self.tt(actT[:, f0 + f, cc0:cc0 + cn], s_[:, cc0:cc0 + cn], usrc, ALU.mult, r=[sk, ukk], w=["actT"])
                for dh in range(D // 512):
                    wd_, wdk = wdb[di % NWD], ("wdb", di % NWD)
                    y_, yk = ysb[di % 2], ("ysb", di % 2)
                    di += 1
                    src = self.dr["wd"][l, e].rearrange("(f p) d -> p f d", p=128)
                    step = 4
                    for f in range(0, FC, step):
                        nf_ = min(step, FC - f)
                        self.ld(wd_[:, f:f + nf_, :], src[:, f:f + nf_, dh * 512:(dh + 1) * 512], r=[], w=[wdk], q="pool")
                    for f in range(FC):
                        for ci, (cc0, cn) in enumerate(cchunks):
                            self.mm(yps[ci][:cn, :], actT[:, f, cc0:cc0 + cn], wd_[:, f, :], start=(f == 0), stop=(f == FC - 1),
                                    r=["actT", wdk], w=[ypk[ci]], inc=(f == FC - 1))
                    for ci, (cc0, cn) in enumerate(cchunks):
                        self.cp(y_[:cn, ci, :], yps[ci][:cn, :], r=[ypk[ci]], w=[yk], eng=("act" if ci % 2 else "dve"))
                    for ci, (cc0, cn) in enumerate(cchunks):
                        self.ld(self.dr["yexp"][e, gb + cc0:gb + cc0 + cn, dh * 512:(dh + 1) * 512], y_[:cn, ci, :], r=[yk], w=[("yexp", e)])
            self.s.barrier()

    def phase_scatter(self, l, sm, slot_unused, gm_unused):
        cfg = self.cfg
        NE, NLB, NB, D = cfg.NE, cfg.NLB, cfg.NB, cfg.D
        with contextlib.ExitStack() as st2:
            slot = self.sb(st2, "sslot", [128, NB, NE], F32)
            gm = self.sb(st2, "sgm", [128, NB, NE], F32)
            self.ld(slot[:].rearrange("p b e -> p (b e)"), self.dr["slotd"][sm], r=[], w=[("slot", sm)])
            self.ld(gm[:].rearrange("p b e -> p (b e)"), self.dr["gmd"][sm], r=[], w=[("gm", sm)])
            iota = self.load_const(st2, "iota", [128, 256])
            segs = []
            for seg, (cap, col0) in enumerate(((cfg.capl, self.col0(sm, 0)), (cfg.capc, self.col0(sm, 1)))):
                ck = min(128, cap)
                ncb = cap // ck
                y = self.sb(st2, f"sy{seg}", [ck, NE * ncb, D], BF16)
                for e in range(NE):
                    for cb in range(ncb):
                        self.ld(y[:, e * ncb + cb, :], self.dr["yexp"][e, col0 + cb * ck:col0 + (cb + 1) * ck, :], r=[("yexp", e)], w=[("sy", seg)])
                segs.append((cap, ck, ncb, y))
            mxi = max(NE * sg[2] for sg in segs)
            selT = self.sb(st2, "selT", [128, mxi, 512], BF16)
            sg_ = [self.sb(st2, f"sg{i}", [128, 256], BF16) for i in range(2)]
            tps = [self.ps(st2, f"stp{i}", [128, 8, 128], BF16) for i in range(2)]
            yT = [self.ps(st2, f"syT{i}", [128, 512]) for i in range(4)]
            xts = [self.sb(st2, f"sxt{i}", [128, 8, 512], F32) for i in range(2)]
            xsrc = self.dr["xT"][sm].rearrange("(k p) t -> p k t", p=128)
            it = 0
            gi = 0
            for ti, (c0, w, seg) in enumerate(cfg.tiles):
                cap, ck, ncb, y = segs[seg]
                v = cfg.SPC if seg == 1 else sm
                nidx = NE * ncb
                xt, xk = xts[ti % 2], ("sxt", ti % 2)
                self.ld(xt[:, :, :w], xsrc[:, :, c0:c0 + w], r=[("xT", sm)], w=[xk])
                for bb in range(w // 128):
                    b = c0 // 128 + bb
                    for g0 in range(0, nidx, 8):
                        tp, tk = tps[gi % 2], ("stp", gi % 2)
                        gi += 1
                        ng = min(8, nidx - g0)
                        for q in range(ng):
                            idx = g0 + q
                            e, cb = idx // ncb, idx % ncb
                            if cb == 0:
                                sg, sgk = sg_[it % 2], ("sg", it % 2)
                                it += 1
                                self.ts(sg[:, :cap], iota[:, :cap], slot[:, b, e:e + 1], gm[:, b, e:e + 1], ALU.is_equal, ALU.mult,
                                        r=[("slot", sm), ("gm", sm), ("sb", iota.name)], w=[sgk])
                            self.tr(tp[:ck, q, :], sg[:, cb * ck:(cb + 1) * ck], self.ident_bf[:], r=[sgk, "ident_bf"], w=[tk], inc=True)
                        self.cp(selT[:ck, g0:g0 + ng, bb * 128:(bb + 1) * 128], tp[:ck, :ng, :], r=[tk], w=["selT"], eng=("act" if gi % 2 else "dve"))
                for jg in range(2):
                    for idx in range(nidx):
                        for jj in range(4):
                            j = jg * 4 + jj
                            self.mm(yT[jj][:, :w], y[:ck, idx, j * 128:(j + 1) * 128], selT[:ck, idx, :w], start=(idx == 0), stop=(idx == nidx - 1),
                                    r=[("sy", seg), "selT"], w=[("syT", jj)], inc=(idx == nidx - 1))
                    for jj in range(4):
                        j = jg * 4 + jj
                        self.stt(xt[:, j, :w], yT[jj][:, :w], self.P(l, 5, j, v), xt[:, j, :w], ALU.mult, ALU.add, r=[("syT", jj), xk, "par"], w=[xk])
                self.ld(xsrc[:, :, c0:c0 + w], xt[:, :, :w], r=[xk], w=[("xT", sm)])
            self.s.barrier()

    def phase_final(self):
        cfg = self.cfg
        with contextlib.ExitStack() as st2:
            nb = self.nm_bufs(st2)
            xts = [self.sb(st2, f"fxt{i}", [128, 8, 512], F32) for i in range(2)]
            ots = [self.sb(st2, f"fot{i}", [128, 8, 512], F32) for i in range(2)]
            it = 0
            for sm in range(cfg.SPC):
                xsrc = self.dr["xT"][sm].rearrange("(k p) t -> p k t", p=128)
                osrc = self.out[sm].rearrange("(k p) t -> p k t", p=128)
                for ti, (c0, w, seg) in enumerate(cfg.tiles):
                    if seg == 1:
                        continue
                    xt, xk = xts[it % 2], ("fxt", it % 2)
                    ot, ok = ots[it % 2], ("fot", it % 2)
                    it += 1
                    self.ld(xt[:, :, :w], xsrc[:, :, c0:c0 + w], r=[("xT", sm)], w=[xk])
                    self.norm_mod(xt, xk, w, nb, lambda k: self.fnw[:, k:k + 1], None, lambda k: ot[:, k, :w], ok)
                    self.ld(osrc[:, :, c0:c0 + w], ot[:, :, :w], r=[ok], w=[("out", sm)])
            self.s.barrier()

    def go(self):
        self.ph += 1
        return self.ph <= self.stop

    def build(self):
        cfg = self.cfg
        self.declare()
        self.pcol = self.sb(self.es, "pcol", [128, 64], F32)
        import os
        self.stop = int(os.environ.get('KSTOP', '999'))
        self.ph = 0
        self.setup()
        NB, NE = cfg.NB, cfg.NE
        slot = [None] * cfg.SPC
        gm = [None] * cfg.SPC
        for l in range(cfg.DEPTH):
            odd = (l % 2 == 1)
            i = l // 2
            if odd:
                self.go() and self.layer_params_odd(l)
            else:
                self.go() and self.layer_params_even(l)
            for sm in range(cfg.SPC):
                with contextlib.ExitStack() as st:
                    vt = self.sb(st, "vt", [128, NB, 512 if odd else 640], BF16)
                    if odd:
                        self.recst = {"xstok": self.sb(st, "xstok", [128, NB, 512], BF16), "dttok": self.sb(st, "dttok", [128, NB, 16], F32),
                                      "ncum": self.sb(st, "ncum", [128, NB, 16], F32)}
                    with contextlib.ExitStack() as st1:
                        hT = self.phase_norm1(st1, l, sm) if self.go() else None
                        if odd:
                            self.go() and self.rec_inproj(st1, l, sm, hT, vt)
                        else:
                            lp = self.lp
                            rows = [("normrope", j, lp["qn"], j) for j in range(4)] + [("normrope", 4, lp["kn"], 4)]
                            rows += [("rope", 6 + j, 1.0, 5 + j) for j in range(4)] + [("rope", 10 + j, 1.0, 9 + j) for j in range(4)]
                            self.go() and self.phase_inproj(st1, l, sm, hT, "att_w_in", i, cfg.ATT_IN, rows, [(5 * 128, 128, 0), (14 * 128, 512, 128)], vt)
                    if odd:
                        self.go() and self.phase_rec(l, sm, vt)
                    else:
                        self.go() and self.phase_attn(l, sm, vt)
                with contextlib.ExitStack() as st:
                    h2tok = self.sb(st, "h2tok", [128, NB, cfg.D], BF16)
                    afftok = self.sb(st, "afftok", [128, NB, NE], F32)
                    slot[sm] = self.sb(st, "slot", [128, NB, NE], F32)
                    gm[sm] = self.sb(st, "gm", [128, NB, NE], F32)
                    self.go() and self.phase_outproj(st, l, sm, "rec_w_out" if odd else "att_w_out", i, h2tok, afftok, odd)
                    self.go() and self.phase_route(l, sm, h2tok, afftok, slot[sm], gm[sm])
            self.phase_ffn(l)
            for sm in range(cfg.SPC):
                self.go() and self.phase_scatter(l, sm, slot[sm], gm[sm])
        self.go() and self.phase_final()
        self.es.close()
        return self.nc


def _prep(cfg, inp):
    f = np.float32
    L, SPC = cfg.DEPTH, cfg.SPC

    def colT(a):
        a = np.asarray(a, f)
        sh = a.shape
        a = a.reshape(sh[:-1] + (sh[-1] // 128, 128))
        return np.ascontiguousarray(np.moveaxis(a, -1, 0))

    def rep(a, n=128):
        a = np.asarray(a, f)
        return np.ascontiguousarray(np.broadcast_to(a[None], (n,) + a.shape))
    shared = {}
    shared["ada_w"] = np.asarray(inp["ada_w"], f)
    shared["ada_bT"] = colT(inp["ada_b"])
    shared["n1w"] = colT(inp["norm1_w"]); shared["n2w"] = colT(inp["norm2_w"]); shared["fnw"] = colT(inp["final_norm_w"])
    shared["att_w_in"] = np.asarray(inp["att_w_in"], f); shared["att_w_out"] = np.asarray(inp["att_w_out"], f)
    shared["qnw"] = np.ascontiguousarray(np.tile(np.asarray(inp["att_q_norm_w"], f), (1, 2)).T)
    shared["knw"] = np.ascontiguousarray(np.tile(np.asarray(inp["att_k_norm_w"], f), (1, 2)).T)
    shared["dlam"] = rep(np.asarray(inp["diff_lambda"], f).reshape(-1, 256))
    shared["dnw"] = np.ascontiguousarray(np.asarray(inp["diff_norm_w"], f).T)
    if L >= 2:
        shared["rec_w_in"] = np.asarray(inp["rec_w_in"], f); shared["rec_w_out"] = np.asarray(inp["rec_w_out"], f)
        shared["rdl"] = rep(np.asarray(inp["ret_decay_logit"], f).reshape(-1, 8))
        shared["cwT"] = np.ascontiguousarray(np.moveaxis(colT(np.moveaxis(np.asarray(inp["ssd_conv_w"], f), 1, 0)), 1, -1))
        shared["cbT"] = colT(inp["ssd_conv_b"])
        shared["dtb"] = np.ascontiguousarray(np.asarray(inp["ssd_dt_bias"], f).reshape(-1, 16).T)
        shared["alog"] = np.ascontiguousarray(np.asarray(inp["ssd_a_log"], f).reshape(-1, 16).T)
        shared["alogr"] = rep(np.asarray(inp["ssd_a_log"], f).reshape(-1, 16))
        shared["dsk"] = np.ascontiguousarray(np.moveaxis(np.repeat(np.asarray(inp["ssd_d_skip"], f), 64, axis=-1).reshape(-1, 4, 128), -1, 0))
        shared["snw"] = colT(inp["ssd_norm_w"])
    else:
        z = lambda *s: np.zeros(s, f)
        shared.update(rec_w_in=z(1, cfg.D, cfg.REC_IN), rec_w_out=z(1, cfg.D, cfg.D), rdl=z(128, 1, 8), cwT=z(128, 1, 8, 3), cbT=z(128, 1, 8),
                      dtb=z(16, 1), alog=z(16, 1), alogr=z(128, 1, 16), dsk=z(128, 1, 4), snw=z(128, 1, 4))
    shared["router_w"] = np.asarray(inp["router_w"], f)
    shared["wg"] = np.asarray(inp["expert_w_gate"], f); shared["wu"] = np.asarray(inp["expert_w_up"], f); shared["wd"] = np.asarray(inp["expert_w_down"], f)
    shared.update(_consts(cfg))
    x, ctx, c = np.asarray(inp["x"], f), np.asarray(inp["ctx"], f), np.asarray(inp["c"], f)
    cc = np.asarray(inp["c_ctx"], f)
    maps = []
    for core in range(cfg.NCORES):
        b0 = core * SPC
        m = dict(shared)
        m["xT0"] = np.ascontiguousarray(np.concatenate([x[b0:b0 + SPC], ctx[b0:b0 + SPC]], axis=1).transpose(0, 2, 1))
        cv = np.concatenate([c[b0:b0 + SPC], cc[None]], 0)
        m["cT"] = np.ascontiguousarray(cv.reshape(SPC + 1, 8, 128).transpose(2, 1, 0))
        maps.append(m)
    return maps


def run(cfg, inp):
    prog = Prog(cfg)
    nc = prog.build()
    maps = _prep(cfg, inp)
    res = run_bass_kernel_spmd(nc, maps, core_ids=list(range(cfg.NCORES)))
    outs = [np.asarray(r["outT"]).transpose(0, 2, 1) for r in res.results]
    return np.ascontiguousarray(np.concatenate(outs, 0)).astype(np.float32)


def kernel(**inputs):
    cfg = Cfg(SPC=2)
    return run(cfg, inputs)


def _layer_params_odd(self, l):
    i = l // 2
    pc = self.pcol
    lp = {"lg": pc[:, 8:16], "neglg": pc[:, 16:24], "dtb": pc[:16, 24:25], "aneg": pc[:16, 25:26], "isF": pc[:16, 26:27], "isB": pc[:16, 27:28],
          "one": pc[:, 63:64], "dsk": pc[:, 32:36], "snw": pc[:, 36:40]}
    if not hasattr(self, "cw"):
        self.cw = self.sb(self.es, "cw", [128, 8, 3], F32)
        self.cb = self.sb(self.es, "cb", [128, 8], F32)
    V = self.nc.vector
    self.s.op("dve", lambda: V.memset(pc[:, 63:64], 1.0), r=[], w=["pcols"])
    self.s.op("dve", lambda: V.memset(pc[:16, 26:27], 0.0), r=[], w=["pcols"])
    self.s.op("dve", lambda: V.memset(pc[:8, 26:27], 1.0), r=[], w=["pcols"])
    self.s.op("dve", lambda: V.memset(pc[:16, 27:28], 1.0), r=[], w=["pcols"])
    self.s.op("dve", lambda: V.memset(pc[:8, 27:28], 0.0), r=[], w=["pcols"])
    self.ld(pc[:, 8:16], self.dr["rdl"][:, i, :], r=[], w=["pcols"])
    self.ld(pc[:16, 24:25], self.dr["dtb"][:, i:i + 1], r=[], w=["pcols"])
    self.ld(pc[:16, 25:26], self.dr["alog"][:, i:i + 1], r=[], w=["pcols"])
    self.ld(pc[:, 32:36], self.dr["dsk"][:, i, :], r=[], w=["pcols"])
    self.ld(pc[:, 36:40], self.dr["snw"][:, i, :], r=[], w=["pcols"])
    self.ld(self.cw[:], self.dr["cwT"][:, i], r=[], w=["pcols"])
    self.ld(self.cb[:], self.dr["cbT"][:, i, :], r=[], w=["pcols"])
    self.act(pc[:, 16:24], pc[:, 8:16], AF.Exp, scale=-1.0, r=["pcols"], w=["pcols"])
    self.act(pc[:, 16:24], pc[:, 16:24], AF.Ln, bias=pc[:, 63:64], r=["pcols"], w=["pcols"])
    self.ts(pc[:, 8:16], pc[:, 16:24], -1.0, None, ALU.mult, r=["pcols"], w=["pcols"])
    self.act(pc[:16, 25:26], pc[:16, 25:26], AF.Exp, r=["pcols"], w=["pcols"])
    self.ts(pc[:16, 25:26], pc[:16, 25:26], -1.0, None, ALU.mult, r=["pcols"], w=["pcols"])
    self.ts(pc[:, 36:40], pc[:, 36:40], 16.0, None, ALU.mult, r=["pcols"], w=["pcols"])
    self.s.barrier()
    self.lp = lp


def _rec_inproj(self, st1, l, sm, hT, vt):
    cfg = self.cfg
    i = l // 2
    rows = [("rope", 0, 1.0, 0), ("rope", 1, 1.0, 1), ("rope", 2, 0.125, 2), ("rope", 3, 0.125, 3)]
    rows += [("side", 8 + c, None, c) for c in range(4)] + [("side", 12 + c, None, 4 + c) for c in range(4)]
    rows += [("conv", 16 + c, (self.cw, self.cb, c), ("xs", 8 + c)) for c in range(4)]
    rows += [("conv", 20 + c, (self.cw, self.cb, 4 + c), ("qk", 4 + c)) for c in range(4)]
    rows += [("dt", 24, None, None)]
    self.phase_inproj(st1, l, sm, hT, "rec_w_in", i, cfg.REC_IN, rows, [(4 * 128, 512, 0)], vt)


def _rec_dt(self, st2, l, sm, fr, fk, ssp, gr, gk, Cb, Ck, Sb, Sk):
    cfg = self.cfg
    T, NL, NC, NB = cfg.T, cfg.NL, cfg.NC, cfg.NB
    lp, rs = self.lp, self.recst
    with contextlib.ExitStack() as st:
        dtT, la, pa, pb = fr[:16, :], gr[:16, :], Cb[:16, :], Sb[:16, :]
        cF = self.sb(st, "cF", [16, T], F32)
        self.act(dtT[:], fr[:16, :], AF.Exp, bias=lp["dtb"], r=[fk, "pcols"], w=[fk])
        self.act(dtT[:], dtT[:], AF.Ln, bias=lp["one"][:16], r=[fk, "pcols"], w=[fk])
        self.ts(la[:], dtT[:], lp["aneg"], None, ALU.mult, r=[fk, "pcols"], w=[gk])
        cur, ck_ = la, gk
        bufs = [(pa, Ck), (pb, Sk)]
        bi = 0
        sh = 1
        while sh < max(NL, NC):
            nxt, nk = bufs[bi % 2]
            bi += 1
            for (s0, n) in ((0, NL), (NL, NC)):
                if sh < n:
                    self.tt(nxt[:, s0 + sh:s0 + n], cur[:, s0 + sh:s0 + n], cur[:, s0:s0 + n - sh], ALU.add, r=[ck_], w=[nk])
                    self.cp(nxt[:, s0:s0 + sh], cur[:, s0:s0 + sh], r=[ck_], w=[nk])
                else:
                    self.cp(nxt[:, s0:s0 + n], cur[:, s0:s0 + n], r=[ck_], w=[nk])
            cur, ck_ = nxt, nk
            sh *= 2
        P, Pk = cur, ck_
        oth, ok_ = bufs[bi % 2]
        totc, totl = P[:, T - 1:T], P[:, NL - 1:NL]
        self.ts(cF[:, 0:NL], P[:, 0:NL], totc, None, ALU.add, r=[Pk], w=["cF"])
        self.cp(cF[:, NL:T], P[:, NL:T], r=[Pk], w=["cF"])
        self.tt(oth[:], la[:], P[:], ALU.subtract, r=[gk, Pk], w=[ok_])
        self.ts(oth[:, 0:NL], oth[:, 0:NL], totc, totl, ALU.add, ALU.add, r=[ok_, Pk], w=[ok_])
        self.ts(oth[:, NL:T], oth[:, NL:T], totc, None, ALU.add, r=[ok_, Pk], w=[ok_])
        self.ts(cF[:], cF[:], lp["isF"], None, ALU.mult, r=["cF", "pcols"], w=["cF"])
        self.stt(cF[:], oth[:], lp["isB"], cF[:], ALU.mult, ALU.add, r=[ok_, "cF", "pcols"], w=["cF"])
        self.ld(self.dr["cumT"][sm], cF[:], r=["cF"], w=[("cumT", sm)])
        self.ts(la[:], cF[:], -1.0, None, ALU.mult, r=["cF"], w=[gk])
        for b in range(NB):
            self.tr(ssp[:, 0:16], dtT[:, b * 128:(b + 1) * 128], self.ident_f[:16, :16], r=[fk, "ident_f"], w=["ssp"])
            self.cp(rs["dttok"][:, b, :], ssp[:, 0:16], r=["ssp"], w=[("dttok", b)])
            self.tr(ssp[:, 16:32], la[:, b * 128:(b + 1) * 128], self.ident_f[:16, :16], r=[gk, "ident_f"], w=["ssp"])
            self.cp(rs["ncum"][:, b, :], ssp[:, 16:32], r=["ssp"], w=[("ncum", b)])


Prog.layer_params_odd = _layer_params_odd
Prog.rec_inproj = _rec_inproj
Prog.rec_dt = _rec_dt


def _vis(tf_s, tf_t):
    if tf_s.max() <= tf_t.min():
        return "full"
    if tf_s.min() > tf_t.max():
        return "none"
    return "part"


def _phase_rec(self, l, sm, vt):
    cfg = self.cfg
    T, NB, NL, NC = cfg.T, cfg.NB, cfg.NL, cfg.NC
    lp, rs = self.lp, self.recst
    cst = _consts(cfg)
    TF, TB = cst["tfrow"][0], cst["tbrow"][0]
    with contextlib.ExitStack() as st2:
        tfrow = self.load_const(st2, "tfrow", [128, T])
        tbrow = self.load_const(st2, "tbrow", [128, T])
        tfcol = self.load_const(st2, "tfcol", [128, NB])
        tbcol = self.load_const(st2, "tbcol", [128, NB])
        maskF = self.load_const(st2, "maskF", [128, 4, 512])
        maskB = self.load_const(st2, "maskB", [128, 4, 512])
        ckeys = [("sb", t.name) for t in (tfrow, tbrow, tfcol, tbcol, maskF, maskB)]
        vssd = self.sb(st2, "vssd", [128, NB, 16, 64], BF16)
        for b in range(NB):
            for dh in range(16):
                H = dh % 8
                self.ts(vssd[:, b, dh, :], rs["xstok"][:, b, H * 64:(H + 1) * 64], rs["dttok"][:, b, dh:dh + 1], None, ALU.mult,
                        r=[("xstok", b), ("dttok", b)], w=[("vssd", b)])
        KT = [self.sb(st2, f"rKT{i}", [128, T], BF16) for i in range(2)]
        QT = [self.sb(st2, f"rQT{i}", [128, T], BF16) for i in range(2)]
        nbF = self.sb(st2, "nbF", [128, NB], F32)
        nbB = self.sb(st2, "nbB", [128, NB], F32)
        Sps = [self.ps(st2, f"rS{i}", [128, 512]) for i in range(2)]
        Ops = [self.ps(st2, f"rO{i}", [128, 512]) for i in range(4)]
        Dt = [self.sb(st2, f"rD{i}", [128, 512], F32) for i in range(4)]
        pre = [self.sb(st2, f"rpre{i}", [128, 512], F32) for i in range(2)]
        pt = [self.sb(st2, f"rpt{i}", [128, 512], BF16) for i in range(3)]
        osb = [self.sb(st2, f"rosb{i}", [128, 512], F32) for i in range(2)]
        crow = [self.sb(st2, f"crow{i}", [128, 512], F32) for i in range(8)]
        cnt = {"s": 0, "d": 0, "p": 0, "pt": 0, "o": 0}

        def decay(row_ap, rowkey, scale, bias, mode, mask_ap):
            d, dk = Dt[cnt["d"] % 4], ("rD", cnt["d"] % 4)
            cnt["d"] += 1
            src, sk = row_ap, rowkey
            if mode == "part":
                p_, pk_ = pre[cnt["p"] % 2], ("rpre", cnt["p"] % 2)
                cnt["p"] += 1
                if scale is None:
                    self.tt(p_[:, :mask_ap.shape[1]], row_ap, mask_ap, ALU.add, r=[rowkey] + ckeys, w=[pk_])
                else:
                    self.stt(p_[:, :mask_ap.shape[1]], row_ap, scale, mask_ap, ALU.mult, ALU.add, r=[rowkey, "pcols"] + ckeys, w=[pk_])
                src, sk = p_[:, :mask_ap.shape[1]], pk_
                self.act(d[:, :mask_ap.shape[1]], src, AF.Exp, bias=bias, r=[sk, "nb", "pcols"] + [("ncum", b_) for b_ in range(NB)], w=[dk])
            else:
                wdt = row_ap.shape[1]
                if scale is None:
                    self.act(d[:, :wdt], src, AF.Exp, bias=bias, r=[sk, "nb"] + [("ncum", b_) for b_ in range(NB)], w=[dk])
                else:
                    self.act(d[:, :wdt], src, AF.Exp, bias=bias, scale=scale, r=[sk, "nb", "pcols"], w=[dk])
            return d, dk

        def classify(c0, w, kb):
            tt_f, tt_b = TF[c0:c0 + w], TB[c0:c0 + w]
            ts_f, ts_b = TF[kb * 128:(kb + 1) * 128], TB[kb * 128:(kb + 1) * 128]
            vf, vb = _vis(ts_f, tt_f), _vis(ts_b, tt_b)
            j = (kb * 128 - c0) // 128
            return vf, vb, j

        for h in range(4):
            K, Kk = KT[h % 2], ("rKT", h % 2)
            Q, Qk = QT[h % 2], ("rQT", h % 2)
            self.ld(K[:64, :], self.dr["qk"][sm, 256 + h * 64:256 + (h + 1) * 64, :], r=[("qk", sm, 2 + h // 2)], w=[Kk])
            self.ld(Q[:64, :], self.dr["qk"][sm, h * 64:(h + 1) * 64, :], r=[("qk", sm, h // 2)], w=[Qk])
            self.ts(nbF[:], tfcol[:], lp["neglg"][:, h:h + 1], None, ALU.mult, r=ckeys + ["pcols"], w=["nb"])
            self.ts(nbB[:], tbcol[:], lp["neglg"][:, 4 + h:5 + h], None, ALU.mult, r=ckeys + ["pcols"], w=["nb"])
            for ti, (c0, w, seg) in enumerate(cfg.tiles):
                O, Ok = Ops[cnt["o"] % 4], ("rO", cnt["o"] % 4)
                ob_, obk = osb[cnt["o"] % 2], ("rosb", cnt["o"] % 2)
                cnt["o"] += 1
                contrib = []
                for kb in range(NB):
                    vf, vb, j = classify(c0, w, kb)
                    if vf != "none" or vb != "none":
                        contrib.append((kb, vf, vb, j))
                kb0 = contrib[0][0]
                self.mm(Sps[cnt["s"] % 2][:, :w], K[:64, kb0 * 128:(kb0 + 1) * 128], Q[:64, c0:c0 + w], True, True, r=[Kk, Qk], w=[("rS", cnt["s"] % 2)])
                for n, (kb, vf, vb, j) in enumerate(contrib):
                    S, Sk = Sps[cnt["s"] % 2], ("rS", cnt["s"] % 2)
                    cnt["s"] += 1
                    if n + 1 < len(contrib):
                        kn = contrib[n + 1][0]
                        self.mm(Sps[cnt["s"] % 2][:, :w], K[:64, kn * 128:(kn + 1) * 128], Q[:64, c0:c0 + w], True, True, r=[Kk, Qk], w=[("rS", cnt["s"] % 2)])
                    ds = []
                    if vf != "none":
                        ds.append(decay(tfrow[:, c0:c0 + w], ckeys[0], lp["lg"][:, h:h + 1], nbF[:, kb:kb + 1], vf, maskF[:, j, :w] if vf == "part" else None))
                    if vb != "none":
                        ds.append(decay(tbrow[:, c0:c0 + w], ckeys[1], lp["lg"][:, 4 + h:5 + h], nbB[:, kb:kb + 1], vb, maskB[:, j, :w] if vb == "part" else None))
                    d, dk = ds[0]
                    if len(ds) == 2:
                        self.tt(d[:, :w], d[:, :w], ds[1][0][:, :w], ALU.add, r=[dk, ds[1][1]], w=[dk])
                    p, pk = pt[cnt["pt"] % 3], ("rpt", cnt["pt"] % 3)
                    cnt["pt"] += 1
                    self.tt(p[:, :w], S[:, :w], d[:, :w], ALU.mult, r=[Sk, dk], w=[pk])
                    self.mm(O[:, :w], vt[:, kb, h * 128:(h + 1) * 128], p[:, :w], start=(n == 0), stop=(n == len(contrib) - 1),
                            r=[pk, ("vt", kb)], w=[Ok], inc=True)
                self.cp(ob_[:, :w], O[:, :w], r=[Ok], w=[obk], eng="act")
                self.ld(self.dr["rawT"][sm, h * 128:(h + 1) * 128, c0:c0 + w], ob_[:, :w], r=[obk], w=[("rawT", sm, ti)])
        for g in range(2):
            K, Kk = KT[g % 2], ("rKT", g % 2)
            Q, Qk = QT[g % 2], ("rQT", g % 2)
            self.ld(K[:], self.dr["qk"][sm, (4 + g) * 128:(5 + g) * 128, :], r=[("qk", sm, 4 + g)], w=[Kk])
            self.ld(Q[:], self.dr["qk"][sm, (6 + g) * 128:(7 + g) * 128, :], r=[("qk", sm, 6 + g)], w=[Qk])
            for ti, (c0, w, seg) in enumerate(cfg.tiles):
                for hh in range(4):
                    for d_ in range(2):
                        dh = d_ * 8 + g * 4 + hh
                        self.ld(crow[d_ * 4 + hh][:, :w], self.dr["cumT"][sm, dh, c0:c0 + w].partition_broadcast(128), r=[("cumT", sm)], w=[("crow", d_ * 4 + hh)])
                contrib = []
                for kb in range(NB):
                    vf, vb, j = classify(c0, w, kb)
                    if vf != "none" or vb != "none":
                        contrib.append((kb, vf, vb, j))
                ncontrib = sum((vf != "none") + (vb != "none") for (_, vf, vb, _) in contrib)
                seen = [0] * 4
                kb0 = contrib[0][0]
                self.mm(Sps[cnt["s"] % 2][:, :w], K[:, kb0 * 128:(kb0 + 1) * 128], Q[:, c0:c0 + w], True, True, r=[Kk, Qk], w=[("rS", cnt["s"] % 2)])
                for n, (kb, vf, vb, j) in enumerate(contrib):
                    S, Sk = Sps[cnt["s"] % 2], ("rS", cnt["s"] % 2)
                    cnt["s"] += 1
                    if n + 1 < len(contrib):
                        kn = contrib[n + 1][0]
                        self.mm(Sps[cnt["s"] % 2][:, :w], K[:, kn * 128:(kn + 1) * 128], Q[:, c0:c0 + w], True, True, r=[Kk, Qk], w=[("rS", cnt["s"] % 2)])
                    for hh in range(4):
                        for d_, (vis, msk) in enumerate(((vf, maskF), (vb, maskB))):
                            if vis == "none":
                                continue
                            dh = d_ * 8 + g * 4 + hh
                            d, dk = decay(crow[d_ * 4 + hh][:, :w], ("crow", d_ * 4 + hh), None, rs["ncum"][:, kb, dh:dh + 1], vis,
                                          msk[:, j, :w] if vis == "part" else None)
                            p, pk = pt[cnt["pt"] % 3], ("rpt", cnt["pt"] % 3)
                            cnt["pt"] += 1
                            self.tt(p[:, :w], S[:, :w], d[:, :w], ALU.mult, r=[Sk, dk], w=[pk])
                            self.mm(Ops[hh][:64, :w], vssd[:, kb, dh, :], p[:, :w], start=(seen[hh] == 0), stop=(seen[hh] == ncontrib - 1),
                                    r=[pk, ("vssd", kb)], w=[("rO", hh)], inc=True)
                            seen[hh] += 1
                for hh in range(4):
                    H = g * 4 + hh
                    ob_, obk = osb[hh % 2], ("rosb", hh % 2)
                    self.cp(ob_[:64, :w], Ops[hh][:64, :w], r=[("rO", hh)], w=[obk], eng="act")
                    self.ld(self.dr["rawT"][sm, 512 + H * 64:512 + (H + 1) * 64, c0:c0 + w], ob_[:64, :w], r=[obk], w=[("rawT", sm, ti)])
        self.s.barrier()


def _odd_bufs(self, st2):
    return {"raw": self.sb(st2, "oraw", [128, 8, 512], F32), "side": self.sb(st2, "oside", [128, 12, 512], F32),
            "sq": self.sb(st2, "osq", [128, 4, 512], BF16), "rs": self.sb(st2, "ors", [128, 512], F32),
            "sg": self.sb(st2, "osg", [128, 512], F32), "t": self.sb(st2, "ot", [128, 512], F32),
            "s2": self.sb(st2, "os2", [128, 4, 512], F32), "ssp": self.ps(st2, "otp", [128, 512])}


def _odd_pre(self, l, sm, ob, at, atk, c0, w):
    lp = self.lp
    raw, side, sq, rs_, sg, t, s2, ssp = (ob[k] for k in ("raw", "side", "sq", "rs", "sg", "t", "s2", "ssp"))
    rsrc = self.dr["rawT"][sm].rearrange("(k p) t -> p k t", p=128)
    ssrc = self.dr["sideT"][sm].rearrange("(k p) t -> p k t", p=128)
    self.ld(raw[:, :, :w], rsrc[:, :, c0:c0 + w], r=[("rawT", sm)], w=["oraw"])
    self.ld(side[:, :, :w], ssrc[:, :, c0:c0 + w], r=[("sideT", sm)], w=["oside"])
    for c in range(4):
        self.act(sq[:, 0, :w], raw[:, c, :w], AF.Square, r=["oraw"], w=["osq"])
        self.rstd([sq[:, 0, :w]], self.ones_bf[:], ssp[:, :w], rs_[:, :w], 128 * EPS, ["osq", "ones_bf"], ["otp"], ["ors"])
        self.act(sg[:, :w], side[:, c, :w], AF.Silu, r=["oside"], w=["osg"])
        self.tt(t[:, :w], raw[:, c, :w], rs_[:, :w], ALU.mult, r=["oraw", "ors"], w=["ot"])
        self.stt(at[:, c, :w], t[:, :w], math.sqrt(128.0), sg[:, :w], ALU.mult, ALU.mult, r=["ot", "osg"], w=[atk])
    for c in range(4):
        self.stt(t[:, :w], side[:, 8 + c, :w], lp["dsk"][:, c:c + 1], raw[:, 4 + c, :w], ALU.mult, ALU.add, r=["oside", "oraw", "pcols"], w=["ot"])
        self.act(sg[:, :w], side[:, 4 + c, :w], AF.Silu, r=["oside"], w=["osg"])
        self.tt(s2[:, c, :w], t[:, :w], sg[:, :w], ALU.mult, r=["ot", "osg"], w=["os2"])
        self.act(sq[:, c, :w], s2[:, c, :w], AF.Square, r=["os2"], w=["osq"])
    for gg in range(2):
        self.rstd([sq[:, 2 * gg, :w], sq[:, 2 * gg + 1, :w]], self.ones_bf[:], ssp[:, :w], rs_[:, :w], 256 * EPS, ["osq", "ones_bf"], ["otp"], ["ors"])
        for c in (2 * gg, 2 * gg + 1):
            self.stt(at[:, 4 + c, :w], s2[:, c, :w], lp["snw"][:, c:c + 1], rs_[:, :w], ALU.mult, ALU.mult, r=["os2", "ors", "pcols"], w=[atk])


Prog.phase_rec = _phase_rec
Prog.odd_bufs = _odd_bufs
Prog.odd_pre = _odd_pre
```

```python
import math
import contextlib
import numpy as np
import concourse.bass as bass
import concourse.mybir as mybir
from concourse.bass_utils import run_bass_kernel_spmd

F32 = mybir.dt.float32
BF16 = mybir.dt.bfloat16
AF = mybir.ActivationFunctionType
ALU = mybir.AluOpType
AX = mybir.AxisListType
NEG = -30000.0
EPS = 1e-6


class Cfg:
    def __init__(self, BATCH=16, SEQ=2048, DEPTH=4, CTX=256, FF=2816, SPC=2, NE=16):
        self.D = 1024
        self.BATCH, self.NL, self.DEPTH, self.NC, self.FF, self.SPC, self.NE = BATCH, SEQ, DEPTH, CTX, FF, SPC, NE
        self.T = SEQ + CTX
        self.NCORES = BATCH // SPC
        self.GRID_W = 64
        self.KD = 8
        self.ATT_IN = 2304
        self.REC_IN = 3088
        self.capl = 2 * SEQ // NE
        self.capc = 2 * CTX // NE
        self.NLB = SEQ // 128
        self.NCB = CTX // 128
        self.NB = self.NLB + self.NCB
        self.tiles = []
        for c0 in range(0, SEQ, 512):
            self.tiles.append((c0, min(512, SEQ - c0), 0))
        for c0 in range(0, CTX, 512):
            self.tiles.append((SEQ + c0, min(512, CTX - c0), 1))
        self.FC = FF // 128
        self.G = min(SPC, 2)
        self.gcols = self.G * (self.capl + self.capc)
        self.cols = (SPC // self.G) * self.gcols


class Buf:
    __slots__ = ("w", "r")

    def __init__(self):
        self.w = None
        self.r = {}


class Sched:
    ENG = ("pe", "act", "dve", "pool", "sp")
    LIMIT = 20000
    NS = 16

    def __init__(self, nc, es):
        self.nc, self.es = nc, es
        self.E = {"pe": nc.tensor, "act": nc.scalar, "dve": nc.vector, "pool": nc.gpsimd, "sp": nc.sync}
        self.sems = {}
        self.cnt = {e: 0 for e in self.ENG}
        self.epoch = {e: 0 for e in self.ENG}
        self.pending = {e: False for e in self.ENG}
        self.seen = {e: {} for e in self.ENG}
        self.latest = {}
        self.bufs = {}
        self.dmacnt = {e: 0 for e in self.ENG}
        self.nins = 0

    def semh(self, k):
        if k not in self.sems:
            self.sems[k] = self.es.enter_context(self.nc.semaphore("s_" + "_".join(str(x) for x in k)))
        return self.sems[k]

    PSUM_NAMES = {"acc", "ssp", "swp", "nm_ss", "modps", "Sps", "Ops", "Lps", "assp", "dacc", "lps", "tps", "rtp", "rsp", "gps",
                  "aps", "ups", "cps", "yps", "stp", "syT", "rS", "rO", "otp"}

    def _split(self, r, w):
        r2, w2 = [], list(w)
        for key in r:
            name = key if isinstance(key, str) else key[0]
            if name in self.PSUM_NAMES:
                if key not in w2:
                    w2.append(key)
            else:
                r2.append(key)
        return r2, w2

    def _need(self, eng, r, w):
        need = {}

        def req(tok):
            if tok is None:
                return
            k, v = tok
            if eng == "pe" and k[0] == "pe":
                return
            if need.get(k, 0) < v:
                need[k] = v
        for key in r:
            b = self.bufs.get(key)
            if b is None:
                b = self.bufs[key] = Buf()
            req(b.w)
        for key in w:
            b = self.bufs.get(key)
            if b is None:
                b = self.bufs[key] = Buf()
            name = key if isinstance(key, str) else key[0]
            same_ok = (name not in self.PSUM_NAMES) and eng in ("act", "dve")
            if not (same_ok and b.w is not None and b.w[0][0] == eng):
                req(b.w)
            for tok in b.r.values():
                if same_ok and tok[0][0] == eng:
                    continue
                req(tok)
        seen = self.seen[eng]
        for k, v in need.items():
            if seen.get(k, 0) < v:
                self.E[eng].wait_ge(self.semh(k), v)
                seen[k] = v
                self.nins += 1

    def _mark(self, tok, r, w):
        k, v = tok
        if self.latest.get(k, 0) < v:
            self.latest[k] = v
        for key in r:
            self.bufs[key].r[k] = tok
        for key in w:
            b = self.bufs[key]
            b.w = tok
            b.r = {}

    def skip(self):
        import os
        if not hasattr(self, "maxops"):
            self.maxops = int(os.environ.get("KOPS", "100000000"))
            self.opc = 0
        self.opc += 1
        import os
        if os.environ.get("KDBG") and abs(self.opc - self.maxops) < 8:
            import traceback
            fr = traceback.extract_stack()[-4]
            print("OP", self.opc, fr.lineno, fr.line)
        if self.opc == self.maxops:
            print("LAST OP before cutoff ^^^")
        return self.opc > self.maxops

    def op(self, eng, fn, r=(), w=(), inc=True):
        if self.skip():
            return
        r, w = self._split(r, w)
        self._need(eng, r, w)
        ins = fn()
        self.nins += 1
        k = (eng, self.epoch[eng])
        if inc:
            self.cnt[eng] += 1
            ins.then_inc(self.semh(k), 1)
            self.pending[eng] = False
            tok = (k, self.cnt[eng])
        else:
            self.pending[eng] = True
            tok = (k, self.cnt[eng] + 1)
        self._mark(tok, r, w)
        if inc and self.cnt[eng] >= self.LIMIT:
            self.epoch[eng] += 1
            self.cnt[eng] = 0

    def dma(self, q, out, in_, r=(), w=()):
        if self.skip():
            return
        r, w = self._split(r, w)
        self._need(q, r, w)
        j = self.dmacnt[q]
        self.dmacnt[q] += 1
        slot, rnd = j % self.NS, j // self.NS
        k = ("dma", q, slot)
        if rnd > 0 and self.seen[q].get(k, 0) < 16 * rnd:
            self.E[q].wait_ge(self.semh(k), 16 * rnd)
            self.seen[q][k] = 16 * rnd
        self.E[q].dma_start(out=out, in_=in_, allow_slow_non_contiguous=True).then_inc(self.semh(k), 16)
        self.nins += 1
        self._mark((k, 16 * (rnd + 1)), r, w)

    def barrier(self):
        for e in self.ENG:
            assert not self.pending[e], e
        for e in self.ENG:
            seen = self.seen[e]
            for k, v in self.latest.items():
                if e == "pe" and k[0] == "pe":
                    continue
                if seen.get(k, 0) < v:
                    self.E[e].wait_ge(self.semh(k), v)
                    seen[k] = v
                    self.nins += 1
        self.bufs = {}


def _consts(cfg):
    NL, NC, T = cfg.NL, cfg.NC, cfg.T
    c = {}
    c["ones"] = np.ones((128, 128), np.float32)
    bo = np.zeros((128, 128), np.float32)
    bo[:64, :64] = 1
    bo[64:, 64:] = 1
    c["blk64"] = bo
    c["ident"] = np.eye(128, dtype=np.float32)
    t = np.arange(NL)
    row = (t // cfg.GRID_W).astype(np.float32)
    col = (t % cfg.GRID_W).astype(np.float32)
    inv = (10000.0 ** (-np.arange(16, dtype=np.float32) / 16)).astype(np.float32)
    ang = np.concatenate([row[:, None] * inv, col[:, None] * inv], -1).astype(np.float32)
    cos, sin = np.cos(ang).astype(np.float32), np.sin(ang).astype(np.float32)
    C = np.ones((128, T), np.float32)
    S = np.zeros((128, T), np.float32)
    for p in range(128):
        i = p % 32
        C[p, :NL] = cos[:, i]
        S[p, :NL] = -sin[:, i] if (p % 64) < 32 else sin[:, i]
    c["ropeC"], c["ropeS"] = C, S
    perm = np.zeros((128, 128), np.float32)
    for m in range(128):
        k = m + 32 if (m % 64) < 32 else m - 32
        perm[k, m] = 1
    c["perm"] = perm
    TF = np.concatenate([NC + np.arange(NL), np.arange(NC)]).astype(np.float32)
    TB = np.concatenate([NC + (NL - 1 - np.arange(NL)), NC - 1 - np.arange(NC)]).astype(np.float32)
    c["tfrow"] = np.broadcast_to(TF[None, :], (128, T)).copy()
    c["tbrow"] = np.broadcast_to(TB[None, :], (128, T)).copy()
    c["tfcol"] = TF.reshape(cfg.NB, 128).T.copy()
    c["tbcol"] = TB.reshape(cfg.NB, 128).T.copy()
    mF = np.zeros((128, 4, 512), np.float32)
    mB = np.zeros((128, 4, 512), np.float32)
    s = np.arange(128)[:, None]
    tt = np.arange(512)[None, :]
    for j in range(4):
        mF[:, j, :] = np.where(128 * j + s <= tt, 0.0, NEG)
        mB[:, j, :] = np.where(128 * j + s >= tt, 0.0, NEG)
    c["maskF"], c["maskB"] = mF, mB
    c["iota"] = np.broadcast_to(np.arange(256, dtype=np.float32)[None, :], (128, 256)).copy()
    us = np.zeros((128, 128), np.float32)
    for m in range(128):
        us[:m, m] = 1
    c["ustrict"] = us
    return c


class Prog:
    def __init__(self, cfg):
        self.cfg = cfg
        self.nc = bass.Bass("TRN2", target_bir_lowering=False)
        self.es = contextlib.ExitStack()
        self.s = Sched(self.nc, self.es)
        self.dr = {}
        self.uid = 0

    def din(self, name, shape, dt=F32):
        self.dr[name] = self.nc.dram_tensor(name, list(shape), dt, kind="ExternalInput").ap()
        return self.dr[name]

    def dscr(self, name, shape, dt):
        self.dr[name] = self.nc.dram_tensor(name, list(shape), dt, kind="Internal").ap()
        return self.dr[name]

    def sb(self, st, name, shape, dt):
        self.uid += 1
        return st.enter_context(self.nc.sbuf_tensor(f"{name}_{self.uid}", list(shape), dt))

    def ps(self, st, name, shape, dt=F32):
        self.uid += 1
        full = 512 if dt == F32 else 1024
        t = st.enter_context(self.nc.psum_tensor(f"{name}_{self.uid}", [128, full], dt))
        n = 1
        for d in shape[1:]:
            n *= d
        v = t[:shape[0], :n]
        if len(shape) == 3:
            v = v.rearrange("p (a b) -> p a b", b=shape[2])
        return v

    def mm(self, out, lhsT, rhs, start, stop, r, w, inc=None):
        if inc is None:
            inc = stop
        if self.s.__dict__.get("maxops", 10**9) < 10**8 and not stop:
            inc = True
        self.s.op("pe", lambda: self.nc.tensor.matmul(out, lhsT, rhs, start=start, stop=stop), r=r, w=w, inc=inc)

    def tr(self, out, in_, ident, r, w, inc=True):
        self.s.op("pe", lambda: self.nc.tensor.transpose(out, in_, ident), r=r, w=w, inc=inc)

    def act(self, out, in_, func, r, w, bias=None, scale=None, accum_out=None):
        kw = {}
        if bias is not None:
            kw["bias"] = bias
        if scale is not None:
            kw["scale"] = scale
        if accum_out is not None:
            kw["accum_out"] = accum_out
        self.s.op("act", lambda: self.nc.scalar.activation(out=out, in_=in_, func=func, **kw), r=r, w=w)

    def ts(self, out, in0, s1, s2, op0, op1=None, r=(), w=(), eng="dve"):
        e = self.nc.vector if eng == "dve" else self.nc.gpsimd
        if op1 is None:
            self.s.op(eng, lambda: e.tensor_scalar(out=out, in0=in0, scalar1=s1, scalar2=None, op0=op0), r=r, w=w)
        else:
            self.s.op(eng, lambda: e.tensor_scalar(out=out, in0=in0, scalar1=s1, scalar2=s2, op0=op0, op1=op1), r=r, w=w)

    def tt(self, out, in0, in1, op, r, w, eng="dve"):
        e = self.nc.vector if eng == "dve" else self.nc.gpsimd
        self.s.op(eng, lambda: e.tensor_tensor(out=out, in0=in0, in1=in1, op=op), r=r, w=w)

    def stt(self, out, in0, scalar, in1, op0, op1, r, w, eng="dve"):
        e = self.nc.vector if eng == "dve" else self.nc.gpsimd
        self.s.op(eng, lambda: e.scalar_tensor_tensor(out=out, in0=in0, scalar=scalar, in1=in1, op0=op0, op1=op1), r=r, w=w)

    def cp(self, out, in_, r, w, eng="dve"):
        if eng == "act":
            self.s.op("act", lambda: self.nc.scalar.copy(out=out, in_=in_), r=r, w=w)
        else:
            e = self.nc.vector if eng == "dve" else self.nc.gpsimd
            self.s.op(eng, lambda: e.tensor_copy(out=out, in_=in_), r=r, w=w)

    def ld(self, out, in_, r, w, q="sp"):
        self.s.dma(q, out, in_, r=r, w=w)

    def load_const(self, st, name, shape, dt=F32, cast=False):
        t = self.sb(st, name, shape, BF16 if cast else dt)
        self.ld(t[:], self.dr[name][:], r=[("dram", name)], w=[("sb", t.name)], q="pool" if cast else "sp")
        return t

    def rstd(self, sq_list, lhsT, ps_ap, out_ap, addc, rsq, rps, rout):
        n = len(sq_list)
        for i, a in enumerate(sq_list):
            self.mm(ps_ap, lhsT, a, start=(i == 0), stop=(i == n - 1), r=rsq, w=rps)
        self.act(out_ap, ps_ap, AF.Sqrt, bias=self.epsc[:, self.epsidx[addc]:self.epsidx[addc] + 1], r=rps + ["epsc"], w=rout)
        self.recip(out_ap, out_ap, r=rout, w=rout)

    def declare(self):
        cfg = self.cfg
        D, T, SPC, L = cfg.D, cfg.T, cfg.SPC, cfg.DEPTH
        NV = SPC + 1
        NEV, NOD = (L + 1) // 2, max(L // 2, 1)
        d = self.din
        d("xT0", [SPC, D, T]); d("cT", [128, 8, NV]); d("ada_w", [L, D, 6 * D]); d("ada_bT", [128, L, 48])
        d("n1w", [128, L, 8]); d("n2w", [128, L, 8]); d("fnw", [128, 8])
        d("att_w_in", [NEV, D, cfg.ATT_IN]); d("att_w_out", [NEV, D, D])
        d("qnw", [128, NEV]); d("knw", [128, NEV]); d("dlam", [128, NEV, 256]); d("dnw", [128, NEV])
        d("rec_w_in", [NOD, D, cfg.REC_IN]); d("rec_w_out", [NOD, D, D])
        d("rdl", [128, NOD, 8]); d("cwT", [128, NOD, 8, 3]); d("cbT", [128, NOD, 8])
        d("dtb", [16, NOD]); d("alog", [16, NOD]); d("alogr", [128, NOD, 16]); d("dsk", [128, NOD, 4]); d("snw", [128, NOD, 4])
        d("router_w", [L, D, cfg.NE]); d("wg", [L, cfg.NE, D, cfg.FF]); d("wu", [L, cfg.NE, D, cfg.FF]); d("wd", [L, cfg.NE, cfg.FF, D])
        for k, v in _consts(cfg).items():
            d(k, v.shape)
        self.out = self.nc.dram_tensor("outT", [SPC, D, cfg.NL], F32, kind="ExternalOutput").ap()
        self.dscr("xT", [SPC, D, T], F32)
        self.dscr("qk", [SPC, 16 * 128, T], BF16)
        self.dscr("aT", [SPC, D, T], BF16)
        self.dscr("rawT", [SPC, D, T], F32)
        self.dscr("sideT", [SPC, 12 * 128, T], F32)
        self.dscr("cumT", [SPC, 16, T], F32)
        self.dscr("thr", [2, cfg.NE], F32)
        self.dscr("slotd", [SPC, 128, cfg.NB * cfg.NE], F32)
        self.dscr("gmd", [SPC, 128, cfg.NB * cfg.NE], F32)
        self.dscr("xin", [cfg.NE, D, cfg.cols], BF16)
        self.dscr("yexp", [cfg.NE, cfg.cols, D], BF16)

    def col0(self, sm, seg):
        cfg = self.cfg
        base = (sm // cfg.G) * cfg.gcols
        sl = sm % cfg.G
        return base + (sl * cfg.capl if seg == 0 else cfg.G * cfg.capl + sl * cfg.capc)

    def P(self, l, kind, k, v):
        return self.par[:, l, kind * 8 + k, v:v + 1]

    def setup(self):
        cfg, s = self.cfg, self.s
        D, L, NV = cfg.D, cfg.DEPTH, cfg.SPC + 1
        pst = self.es
        self.par = self.sb(pst, "par", [128, L, 48, NV], F32)
        self.ones_bf = self.sb(pst, "ones_bf", [128, 128], BF16)
        self.ident_bf = self.sb(pst, "ident_bf", [128, 128], BF16)
        self.ident_f = self.sb(pst, "ident_f", [128, 128], F32)
        self.fnw = self.sb(pst, "fnws", [128, 8], F32)
        self.epsc = self.sb(pst, "epsc", [128, 4], F32)
        self.epsidx = {}
        for ii, val in enumerate((D * EPS, 64 * EPS, 128 * EPS, 256 * EPS)):
            self.epsidx[val] = ii
            self.s.op("dve", lambda: self.nc.vector.memset(self.epsc[:, ii:ii + 1], val), r=[], w=["epsc"])
        self.ld(self.ones_bf[:], self.dr["ones"][:], r=[], w=["ones_bf"], q="pool")
        self.ld(self.ident_bf[:], self.dr["ident"][:], r=[], w=["ident_bf"], q="pool")
        self.ld(self.ident_f[:], self.dr["ident"][:], r=[], w=["ident_f"])
        self.ld(self.fnw[:], self.dr["fnw"][:], r=[], w=["fnw"])
        self.ts(self.fnw[:], self.fnw[:], math.sqrt(D), None, ALU.mult, r=["fnw"], w=["fnw"])
        with contextlib.ExitStack() as st:
            cT = self.sb(st, "cT", [128, 8, NV], F32)
            clT = self.sb(st, "clT", [128, 8, NV], BF16)
            n1s = self.sb(st, "n1s", [128, L, 8], F32)
            n2s = self.sb(st, "n2s", [128, L, 8], F32)
            abT = self.sb(st, "abT", [128, L, 48], F32)
            wts = [self.sb(st, f"adaw{i}", [128, 8, 768], BF16) for i in range(2)]
            pss = [self.ps(st, f"modps{i}", [128, 6, NV]) for i in range(2)]
            self.ld(cT[:], self.dr["cT"][:], r=[], w=["cT"])
            self.ld(n1s[:], self.dr["n1w"][:], r=[], w=["n1s"])
            self.ld(n2s[:], self.dr["n2w"][:], r=[], w=["n2s"])
            self.ld(abT[:], self.dr["ada_bT"][:], r=[], w=["abT"])
            self.act(clT[:], cT[:], AF.Silu, r=["cT"], w=["clT"])
            self.ts(n1s[:], n1s[:], math.sqrt(D), None, ALU.mult, r=["n1s"], w=["n1s"])
            self.ts(n2s[:], n2s[:], math.sqrt(D), None, ALU.mult, r=["n2s"], w=["n2s"])
            it = 0
            for l in range(L):
                src = self.dr["ada_w"][l].rearrange("(k p) n -> p k n", p=128)
                for g in range(8):
                    wt, pt = wts[it % 2], pss[it % 2]
                    wk, pk = ("adaw", it % 2), ("modps", it % 2)
                    it += 1
                    for k in range(8):
                        self.ld(wt[:, k, :], src[:, k, g * 768:(g + 1) * 768], r=[], w=[wk], q="pool")
                    for jj in range(6):
                        for k in range(8):
                            self.mm(pt[:, jj, :], wt[:, k, jj * 128:(jj + 1) * 128], clT[:, k, :], start=(k == 0), stop=(k == 7),
                                    r=[wk, "clT"], w=[pk], inc=(k == 7))
                    for v in range(NV):
                        self.tt(self.par[:, l, g * 6:(g + 1) * 6, v], pt[:, :, v], abT[:, l, g * 6:(g + 1) * 6], ALU.add,
                                r=[pk, "abT"], w=["par"])
                for v in range(NV):
                    self.stt(self.par[:, l, 8:16, v], self.par[:, l, 8:16, v], 1.0, n1s[:, l, :], ALU.add, ALU.mult, r=["par", "n1s"], w=["par"])
                    self.stt(self.par[:, l, 32:40, v], self.par[:, l, 32:40, v], 1.0, n2s[:, l, :], ALU.add, ALU.mult, r=["par", "n2s"], w=["par"])
            for sm in range(cfg.SPC):
                self.ld(self.dr["xT"][sm], self.dr["xT0"][sm], r=[], w=[("xT", sm)])
            s.barrier()

    def norm_mod(self, xt, xkey, w, bufs, A_fn, B_fn, out_fn, outkey):
        sq, ssps, rs, tmp = bufs["sq"], bufs["ssps"], bufs["rs"], bufs["tmp"]
        D = self.cfg.D
        self.act(sq[:, :, :w], xt[:, :, :w], AF.Square, r=[xkey], w=["nm_sq"])
        self.rstd([sq[:, k, :w] for k in range(8)], self.ones_bf[:], ssps[:, :w], rs[:, :w], D * EPS, ["nm_sq", "ones_bf"], ["nm_ss"], ["nm_rs"])
        for k in range(8):
            if B_fn is None:
                self.stt(out_fn(k), xt[:, k, :w], A_fn(k), rs[:, :w], ALU.mult, ALU.mult, r=[xkey, "nm_rs", "par", "fnw"], w=[outkey])
            else:
                self.stt(tmp[:, k, :w], xt[:, k, :w], A_fn(k), rs[:, :w], ALU.mult, ALU.mult, r=[xkey, "nm_rs", "par"], w=[("nm_tmp", k)])
                self.act(out_fn(k), tmp[:, k, :w], AF.Identity, bias=B_fn(k), r=[("nm_tmp", k), "par"], w=[outkey])

    def nm_bufs(self, st):
        return {"sq": self.sb(st, "nm_sq", [128, 8, 512], BF16), "ssps": self.ps(st, "nm_ss", [128, 512]),
                "rs": self.sb(st, "nm_rs", [128, 512], F32), "tmp": self.sb(st, "nm_tmp", [128, 8, 512], F32)}

    def phase_norm1(self, st, l, sm):
        cfg = self.cfg
        hT = self.sb(st, "hT", [128, 8, cfg.T], BF16)
        with contextlib.ExitStack() as st2:
            nb = self.nm_bufs(st2)
            xts = [self.sb(st2, f"xt{i}", [128, 8, 512], F32) for i in range(2)]
            src = self.dr["xT"][sm].rearrange("(k p) t -> p k t", p=128)
            for i, (c0, w, seg) in enumerate(cfg.tiles):
                xt, xk = xts[i % 2], ("xt", i % 2)
                v = cfg.SPC if seg == 1 else sm
                self.ld(xt[:, :, :w], src[:, :, c0:c0 + w], r=[("xT", sm)], w=[xk])
                self.norm_mod(xt, xk, w, nb, lambda k: self.P(l, 1, k, v), lambda k: self.P(l, 0, k, v),
                              lambda k: hT[:, k, c0:c0 + w], ("hT", i))
            self.s.barrier()
        return hT

    def tile_of_block(self, b):
        for i, (c0, w, seg) in enumerate(self.cfg.tiles):
            if c0 <= b * 128 < c0 + w:
                return i
        raise ValueError(b)

    def phase_inproj(self, st, l, sm, hT, wname, widx, nin, rowspec, vspec, vt):
        cfg = self.cfg
        T = cfg.T
        with contextlib.ExitStack() as st2:
            wi = self.sb(st2, "wi", [128, 8, nin], BF16)
            src = self.dr[wname][widx].rearrange("(k p) n -> p k n", p=128)
            for k in range(8):
                self.ld(wi[:, k, :], src[:, k, :], r=[], w=[("wi", k)], q="pool")
            wkeys = [("wi", k) for k in range(8)]
            C = self.load_const(st2, "ropeC", [128, T])
            S = self.load_const(st2, "ropeS", [128, T])
            blk = self.load_const(st2, "blk64", [128, 128], cast=True)
            perm = self.load_const(st2, "perm", [128, 128], cast=True)
            ck = [("sb", C.name), ("sb", S.name), ("sb", blk.name), ("sb", perm.name)]
            acc = [self.ps(st2, f"acc{i}", [128, 512]) for i in range(2)]
            ssp = self.ps(st2, "ssp", [128, 512])
            swp = self.ps(st2, "swp", [128, 512])
            sqb = [self.sb(st2, f"sqb{i}", [128, 512], BF16) for i in range(2)]
            xwb = [self.sb(st2, f"xwb{i}", [128, 512], BF16) for i in range(2)]
            rsb = [self.sb(st2, f"rsb{i}", [128, 512], F32) for i in range(2)]
            t1b = [self.sb(st2, f"t1b{i}", [128, 512], F32) for i in range(2)]
            t2b = [self.sb(st2, f"t2b{i}", [128, 512], F32) for i in range(2)]
            yb = [self.sb(st2, f"yb{i}", [128, 512], F32) for i in range(2)]
            orow = [self.sb(st2, f"orow{i}", [128, T], BF16) for i in range(2)]
            needf = any(k_ not in ("normrope", "rope") for (k_, _, _, _) in rowspec)
            frow = [self.sb(st2, "frow0", [128, T] if needf else [128, 8], F32)] * 2
            grow = [self.sb(st2, "grow0", [128, T] if needf else [128, 8], F32)] * 2
            it = 0
            import os
            ksub = int(os.environ.get("KSUB", "999"))
            for ci, (kind, j, arg, dst) in enumerate(rowspec):
                if ci >= ksub:
                    break
                M = 16 if kind == "dt" else 128
                o, ok = orow[ci % 2], ("orow", ci % 2)
                fr, fk = frow[0], ("frow", 0)
                gr, gk = grow[0], ("grow", 0)
                for ti, (c0, w, seg) in enumerate(cfg.tiles):
                    a, ak = acc[it % 2], ("acc", it % 2)
                    q = it % 2
                    it += 1
                    for k in range(8):
                        self.mm(a[:M, :w], wi[:, k, j * 128:j * 128 + M], hT[:, k, c0:c0 + w], start=(k == 0), stop=(k == 7),
                                r=[wkeys[k], ("hT", ti)], w=[ak], inc=(k == 7))
                    if kind in ("normrope", "rope"):
                        if kind == "normrope":
                            self.act(sqb[q][:, :w], a[:, :w], AF.Square, r=[ak], w=[("sqb", q)])
                            self.ts(xwb[q][:, :w], a[:, :w], arg, None, ALU.mult, r=[ak, "pcols"], w=[("xwb", q)])
                            self.rstd([sqb[q][:, :w]], blk[:], ssp[:, :w], rsb[q][:, :w], 64 * EPS, [("sqb", q), ck[2]], ["ssp"], [("rsb", q)])
                        else:
                            self.ts(xwb[q][:, :w], a[:, :w], float(arg), None, ALU.mult, r=[ak], w=[("xwb", q)])
                        self.mm(swp[:, :w], perm[:], xwb[q][:, :w], start=True, stop=True, r=[("xwb", q), ck[3]], w=["swp"])
                        self.tt(t1b[q][:, :w], xwb[q][:, :w], C[:, c0:c0 + w], ALU.mult, r=[("xwb", q), ck[0]], w=[("t1b", q)])
                        self.tt(t2b[q][:, :w], swp[:, :w], S[:, c0:c0 + w], ALU.mult, r=["swp", ck[1]], w=[("t2b", q)])
                        if kind == "normrope":
                            self.tt(yb[q][:, :w], t1b[q][:, :w], t2b[q][:, :w], ALU.add, r=[("t1b", q), ("t2b", q)], w=[("yb", q)], eng="dve")
                            self.stt(o[:, c0:c0 + w], yb[q][:, :w], 8.0, rsb[q][:, :w], ALU.mult, ALU.mult, r=[("yb", q), ("rsb", q)], w=[ok])
                        else:
                            self.tt(o[:, c0:c0 + w], t1b[q][:, :w], t2b[q][:, :w], ALU.add, r=[("t1b", q), ("t2b", q)], w=[ok], eng="dve")
                    else:
                        self.cp(fr[:M, c0:c0 + w], a[:M, :w], r=[ak], w=[fk], eng="act")
                if kind in ("normrope", "rope"):
                    self.ld(self.dr["qk"][sm, dst * 128:(dst + 1) * 128, :], o[:], r=[ok], w=[("qk", sm, dst)])
                elif kind == "side":
                    self.ld(self.dr["sideT"][sm, dst * 128:(dst + 1) * 128, :], fr[:], r=[fk], w=[("sideT", sm, dst)])
                elif kind == "conv":
                    cw, cb, cidx = arg
                    self.act(gr[:], fr[:], AF.Identity, bias=cb[:, cidx:cidx + 1], scale=cw[:, cidx, 1:2], r=[fk, "pcols"], w=[gk])
                    for (s0, n) in ((0, cfg.NL), (cfg.NL, cfg.NC)):
                        self.stt(gr[:, s0 + 1:s0 + n], fr[:, s0:s0 + n - 1], cw[:, cidx, 0:1], gr[:, s0 + 1:s0 + n], ALU.mult, ALU.add, r=[fk, gk, "pcols"], w=[gk])
                        self.stt(gr[:, s0:s0 + n - 1], fr[:, s0 + 1:s0 + n], cw[:, cidx, 2:3], gr[:, s0:s0 + n - 1], ALU.mult, ALU.add, r=[fk, gk, "pcols"], w=[gk])
                    if dst[0] == "xs":
                        self.act(fr[:], gr[:], AF.Silu, r=[gk], w=[fk])
                        self.ld(self.dr["sideT"][sm, dst[1] * 128:(dst[1] + 1) * 128, :], fr[:], r=[fk], w=[("sideT", sm, dst[1])])
                        for b in range(cfg.NB):
                            self.tr(swp[:, 0:128], fr[:, b * 128:(b + 1) * 128], self.ident_f[:], r=[fk, "ident_f"], w=["swp"])
                            self.cp(self.recst["xstok"][:, b, cidx * 128:(cidx + 1) * 128], swp[:, 0:128], r=["swp"], w=[("xstok", b)])
                    else:
                        self.act(o[:], gr[:], AF.Silu, r=[gk], w=[ok])
                        self.ld(self.dr["qk"][sm, dst[1] * 128:(dst[1] + 1) * 128, :], o[:], r=[ok], w=[("qk", sm, dst[1])])
                elif kind == "dt":
                    self.rec_dt(st2, l, sm, fr, fk, ssp, gr, gk, C, ck[0], S, ck[1])
            it = 0
            for b in range(cfg.NB if ksub > 100 or ksub < 0 else 0):
                ti = self.tile_of_block(b)
                for (c0, n, d0) in vspec:
                    a, ak = acc[it % 2], ("acc", it % 2)
                    for k in range(8):
                        self.mm(a[:, :n], hT[:, k, b * 128:(b + 1) * 128], wi[:, k, c0:c0 + n], start=(k == 0), stop=(k == 7),
                                r=[wkeys[k], ("hT", ti)], w=[ak], inc=(k == 7))
                    self.cp(vt[:, b, d0:d0 + n], a[:, :n], r=[ak], w=[("vt", b)], eng=("act" if it % 2 else "dve"))
                    it += 1
            self.s.barrier()

    def recip(self, out, in_, r, w):
        self.s.op("dve", lambda: self.nc.vector.reciprocal(out=out, in_=in_), r=r, w=w)

    def phase_attn(self, l, sm, vt):
        cfg = self.cfg
        T, NB, NLB = cfg.T, cfg.NB, cfg.NLB
        lp = self.lp
        with contextlib.ExitStack() as st2:
            KT = [self.sb(st2, f"KT{i}", [64, T], BF16) for i in range(2)]
            QT = [self.sb(st2, f"QT{i}", [64, T], BF16) for i in range(2)]
            pt = [self.sb(st2, f"pt{i}", [128, 512], BF16) for i in range(3)]
            Sps = [self.ps(st2, f"Sps{i}", [128, 512]) for i in range(2)]
            Ops = [self.ps(st2, f"Ops{i}", [128, 512]) for i in range(2)]
            Lps = [self.ps(st2, f"Lps{i}", [128, 512]) for i in range(2)]
            ssp = self.ps(st2, "assp", [128, 512])
            rl = [self.sb(st2, f"rl{i}", [128, 512], F32) for i in range(2)]
            o1 = self.sb(st2, "o1", [128, 512], F32)
            dd = self.sb(st2, "dd", [128, 512], F32)
            sqd = self.sb(st2, "sqd", [128, 512], BF16)
            rsd = self.sb(st2, "rsd", [128, 512], F32)
            o0row = self.sb(st2, "o0row", [128, T], F32)
            ob = [self.sb(st2, f"ob{i}", [128, 512], BF16) for i in range(2)]
            heads = []
            for g in range(2):
                for jj in range(4):
                    j = 4 * g + jj
                    heads.append((j * 64, 512 + g * 64, g * 64, 64, "gqa", j, 0))
            for h in range(4):
                for m in range(2):
                    heads.append((640 + h * 128 + m * 64, 1152 + h * 128 + m * 64, 128 + h * 128, 128, "diff", h, m))
            it = 0
            et = 0
            lastk = None
            nk = 0
            for hi, (qrow, krow, vc0, dv, kind, idx, m) in enumerate(heads):
                if krow != lastk:
                    nk += 1
                    lastk = krow
                    self.ld(KT[nk % 2][:], self.dr["qk"][sm, krow:krow + 64, :], r=[("qk", sm, krow // 128)], w=[("KT", nk % 2)])
                K, Kk = KT[nk % 2], ("KT", nk % 2)
                Q, Qk = QT[hi % 2], ("QT", hi % 2)
                self.ld(Q[:], self.dr["qk"][sm, qrow:qrow + 64, :], r=[("qk", sm, qrow // 128)], w=[Qk])
                for ti, (c0, w, seg) in enumerate(cfg.tiles):
                    kbs = list(range(NB)) if seg == 0 else list(range(NLB, NB))
                    O, Ok = Ops[et % 2], ("Ops", et % 2)
                    L, Lk = Lps[et % 2], ("Lps", et % 2)
                    self.mm(Sps[it % 2][:, :w], K[:, kbs[0] * 128:(kbs[0] + 1) * 128], Q[:, c0:c0 + w], True, True, r=[Kk, Qk], w=[("Sps", it % 2)])
                    for n, kb in enumerate(kbs):
                        S, Sk = Sps[it % 2], ("Sps", it % 2)
                        p, pk = pt[it % 3], ("pt", it % 3)
                        it += 1
                        if n + 1 < len(kbs):
                            kn = kbs[n + 1]
                            self.mm(Sps[it % 2][:, :w], K[:, kn * 128:(kn + 1) * 128], Q[:, c0:c0 + w], True, True, r=[Kk, Qk], w=[("Sps", it % 2)])
                        self.act(p[:, :w], S[:, :w], AF.Exp, scale=0.125, r=[Sk], w=[pk])
                        last = (n == len(kbs) - 1)
                        self.mm(O[:dv, :w], vt[:, kb, vc0:vc0 + dv], p[:, :w], start=(n == 0), stop=last, r=[pk, ("vt", kb)], w=[Ok], inc=False)
                        self.mm(L[:dv, :w], self.ones_bf[:, :dv], p[:, :w], start=(n == 0), stop=last, r=[pk, "ones_bf"], w=[Lk], inc=True)
                    r_, rk = rl[et % 2], ("rl", et % 2)
                    o_, obk = ob[et % 2], ("ob", et % 2)
                    et += 1
                    self.recip(r_[:dv, :w], L[:dv, :w], r=[Lk], w=[rk])
                    if kind == "gqa":
                        self.tt(o_[:dv, :w], O[:dv, :w], r_[:dv, :w], ALU.mult, r=[Ok, rk], w=[obk])
                        self.ld(self.dr["aT"][sm, idx * 64:(idx + 1) * 64, c0:c0 + w], o_[:dv, :w], r=[obk], w=[("aT", sm, ti)])
                    elif m == 0:
                        self.tt(o0row[:, c0:c0 + w], O[:, :w], r_[:, :w], ALU.mult, r=[Ok, rk], w=[("o0row", ti)])
                    else:
                        self.tt(o1[:, :w], O[:, :w], r_[:, :w], ALU.mult, r=[Ok, rk], w=["o1"])
                        self.stt(dd[:, :w], o1[:, :w], lp["lamneg"], o0row[:, c0:c0 + w], ALU.mult, ALU.add, r=["o1", ("o0row", ti), "pcols"], w=["dd"])
                        self.act(sqd[:, :w], dd[:, :w], AF.Square, r=["dd"], w=["sqd"])
                        self.rstd([sqd[:, :w]], self.ones_bf[:], ssp[:, :w], rsd[:, :w], 128 * EPS, ["sqd", "ones_bf"], ["assp"], ["rsd"])
                        self.stt(o_[:, :w], dd[:, :w], lp["dwv"], rsd[:, :w], ALU.mult, ALU.mult, r=["dd", "rsd", "pcols"], w=[obk])
                        self.ld(self.dr["aT"][sm, 512 + idx * 128:512 + (idx + 1) * 128, c0:c0 + w], o_[:, :w], r=[obk], w=[("aT", sm, ti)])
            self.s.barrier()

    def layer_params_even(self, l):
        i = l // 2
        lam_init = 0.8 - 0.6 * math.exp(-0.3 * l)
        pc = self.pcol
        lp = {"qn": pc[:, 0:1], "kn": pc[:, 1:2], "lamneg": pc[:, 2:3], "dwv": pc[:, 3:4]}
        with contextlib.ExitStack() as st:
            dl = self.sb(st, "dl", [128, 256], F32)
            pr = self.sb(st, "pr", [128, 128], F32)
            self.ld(dl[:], self.dr["dlam"][:, i, :], r=[], w=["dl"])
            self.ld(pc[:, 0:1], self.dr["qnw"][:, i:i + 1], r=[], w=["pcols"])
            self.ld(pc[:, 1:2], self.dr["knw"][:, i:i + 1], r=[], w=["pcols"])
            self.ld(pc[:, 3:4], self.dr["dnw"][:, i:i + 1], r=[], w=["pcols"])
            self.tt(pr[:, 0:64], dl[:, 0:64], dl[:, 64:128], ALU.mult, r=["dl"], w=["pr"])
            self.tt(pr[:, 64:128], dl[:, 128:192], dl[:, 192:256], ALU.mult, r=["dl"], w=["pr"])
            self.s.op("dve", lambda: self.nc.vector.reduce_sum(out=pc[:, 4:5], in_=pr[:, 0:64], axis=AX.X), r=["pr"], w=["pcols"])
            self.s.op("dve", lambda: self.nc.vector.reduce_sum(out=pc[:, 5:6], in_=pr[:, 64:128], axis=AX.X), r=["pr"], w=["pcols"])
            self.act(pc[:, 4:6], pc[:, 4:6], AF.Exp, r=["pcols"], w=["pcols"])
            self.tt(pc[:, 2:3], pc[:, 5:6], pc[:, 4:5], ALU.subtract, r=["pcols"], w=["pcols"])
            self.ts(pc[:, 2:3], pc[:, 2:3], -lam_init, None, ALU.add, r=["pcols"], w=["pcols"])
            self.ts(pc[:, 3:4], pc[:, 3:4], math.sqrt(128.0) * (1 - lam_init), None, ALU.mult, r=["pcols"], w=["pcols"])
            self.s.barrier()
        self.lp = lp

    def phase_outproj(self, st, l, sm, wname, widx, h2tok, afftok, odd):
        cfg = self.cfg
        T = cfg.T
        with contextlib.ExitStack() as st2:
            wo = self.sb(st2, "wo", [128, 8, cfg.D], BF16)
            wr = self.sb(st2, "wr", [128, 8, cfg.NE], BF16)
            src = self.dr[wname][widx].rearrange("(k p) n -> p k n", p=128)
            for k in range(8):
                self.ld(wo[:, k, :], src[:, k, :], r=[], w=[("wo", k)], q="pool")
            self.ld(wr[:], self.dr["router_w"][l].rearrange("(k p) n -> p k n", p=128), r=[], w=["wr"], q="pool")
            nb = self.nm_bufs(st2)
            nbf = 1 if odd else 2
            xts = [self.sb(st2, f"dxt{i}", [128, 8, 512], F32) for i in range(nbf)]
            ats = [self.sb(st2, f"dat{i}", [128, 8, 512], BF16) for i in range(nbf)]
            h2 = [self.sb(st2, f"h2{i}", [128, 8, 512], BF16) for i in range(nbf)]
            acc = [self.ps(st2, f"dacc{i}", [128, 512]) for i in range(2)]
            lps = self.ps(st2, "lps", [128, 16])
            tps = [self.ps(st2, f"tps{i}", [128, 1024], BF16) for i in range(2)]
            ex = self.sb(st2, "ex", [128, 16], F32)
            rsum = self.sb(st2, "rsum", [128, 1], F32)
            if odd:
                ob = self.odd_bufs(st2)
            xsrc = self.dr["xT"][sm].rearrange("(k p) t -> p k t", p=128)
            asrc = self.dr["aT"][sm].rearrange("(k p) t -> p k t", p=128)
            it = 0
            bt = 0
            for ti, (c0, w, seg) in enumerate(cfg.tiles):
                v = cfg.SPC if seg == 1 else sm
                xt, xk = xts[ti % nbf], ("dxt", ti % nbf)
                at, atk = ats[ti % nbf], ("dat", ti % nbf)
                hh, hk = h2[ti % nbf], ("h2", ti % nbf)
                self.ld(xt[:, :, :w], xsrc[:, :, c0:c0 + w], r=[("xT", sm)], w=[xk])
                if odd:
                    self.odd_pre(l, sm, ob, at, atk, c0, w)
                else:
                    self.ld(at[:, :, :w], asrc[:, :, c0:c0 + w], r=[("aT", sm, ti)], w=[atk])
                for j in range(8):
                    a, ak = acc[it % 2], ("dacc", it % 2)
                    it += 1
                    for k in range(8):
                        self.mm(a[:, :w], wo[:, k, j * 128:(j + 1) * 128], at[:, k, :w], start=(k == 0), stop=(k == 7),
                                r=[("wo", k), atk], w=[ak], inc=(k == 7))
                    self.stt(xt[:, j, :w], a[:, :w], self.P(l, 2, j, v), xt[:, j, :w], ALU.mult, ALU.add, r=[ak, xk, "par"], w=[xk])
                self.ld(xsrc[:, :, c0:c0 + w], xt[:, :, :w], r=[xk], w=[("xT", sm)])
                self.norm_mod(xt, xk, w, nb, lambda k: self.P(l, 4, k, v), lambda k: self.P(l, 3, k, v), lambda k: hh[:, k, :w], hk)
                for bb in range(w // 128):
                    b = (c0 // 128) + bb
                    for k in range(8):
                        self.mm(lps[:, :], hh[:, k, bb * 128:(bb + 1) * 128], wr[:, k, :], start=(k == 0), stop=(k == 7),
                                r=[hk, "wr"], w=["lps"], inc=(k == 7))
                    self.act(ex[:], lps[:], AF.Exp, r=["lps"], w=["ex"])
                    self.s.op("dve", lambda: self.nc.vector.reduce_sum(out=rsum[:], in_=ex[:], axis=AX.X), r=["ex"], w=["rsum"])
                    self.recip(rsum[:], rsum[:], r=["rsum"], w=["rsum"])
                    self.ts(afftok[:, b, :], ex[:], rsum[:, 0:1], None, ALU.mult, r=["ex", "rsum"], w=[("aff", b)])
                    tp, tk = tps[bt % 2], ("tps", bt % 2)
                    bt += 1
                    for k in range(8):
                        self.tr(tp[:, k * 128:(k + 1) * 128], hh[:, k, bb * 128:(bb + 1) * 128], self.ident_bf[:], r=[hk, "ident_bf"], w=[tk], inc=(k == 7))
                    self.cp(h2tok[:, b, :], tp[:], r=[tk], w=[("h2tok", b)], eng=("act" if bt % 2 else "dve"))
            self.s.barrier()

    def phase_route(self, l, sm, h2tok, afftok, slot, gm):
        cfg = self.cfg
        NE, NB, NLB = cfg.NE, cfg.NB, cfg.NLB
        with contextlib.ExitStack() as st2:
            affT = self.sb(st2, "affT", [NE, cfg.T], F32)
            work = self.sb(st2, "work", [NE, cfg.NL], F32)
            mx = self.sb(st2, "mx", [NE, 8], F32)
            thr = self.sb(st2, "thr", [NE, 2], F32)
            thrb = self.sb(st2, "thrb", [128, 2, NE], F32)
            mask = self.sb(st2, "mask", [128, NB, NE], BF16)
            maskf = self.sb(st2, "maskf", [128, NB, NE], F32)
            ones = self.ones_bf
            us = self.load_const(st2, "ustrict", [128, 128], cast=True)
            iota = self.load_const(st2, "iota", [128, 256])
            tp = self.ps(st2, "rtp", [NE, 128])
            sp = self.ps(st2, "rsp", [128, NE])
            gps = [self.ps(st2, f"gps{i}", [128, 256]) for i in range(2)]
            sel = [self.sb(st2, f"sel{i}", [128, NLB, cfg.capl], BF16) for i in range(2)]
            xg = [self.sb(st2, f"xg{i}", [128, 8, cfg.capl], BF16) for i in range(2)]
            for b in range(NB):
                self.tr(tp[:, :], afftok[:, b, :], self.ident_f[:], r=[("aff", b), "ident_f"], w=["rtp"])
                self.cp(affT[:, b * 128:(b + 1) * 128], tp[:, :], r=["rtp"], w=["affT"])
            for seg, (t0, n, cap) in enumerate(((0, cfg.NL, cfg.capl), (cfg.NL, cfg.NC, cfg.capc))):
                self.cp(work[:, :n], affT[:, t0:t0 + n], r=["affT"], w=["work"])
                nit = cap // 8 + 1
                for i in range(nit):
                    self.s.op("dve", lambda: self.nc.vector.max(out=mx[:], in_=work[:, :n]), r=["work"], w=["mx"])
                    if i == nit - 2:
                        self.cp(thr[:, seg:seg + 1], mx[:, 7:8], r=["mx"], w=["thr"])
                    if i < nit - 1:
                        self.s.op("dve", lambda: self.nc.vector.match_replace(out=work[:, :n], in_to_replace=mx[:], in_values=work[:, :n], imm_value=-1.0),
                                  r=["mx", "work"], w=["work"])
                self.stt(thr[:, seg:seg + 1], thr[:, seg:seg + 1], 1.0, mx[:, 0:1], ALU.mult, ALU.add, r=["thr", "mx"], w=["thr"])
            self.ts(thr[:], thr[:], 0.5, None, ALU.mult, r=["thr"], w=["thr"])
            for seg in range(2):
                self.ld(self.dr["thr"][seg].rearrange("(e o) -> e o", o=1), thr[:, seg:seg + 1], r=["thr"], w=["thr_d"])
            for seg in range(2):
                self.ld(thrb[:, seg, :], self.dr["thr"][seg].partition_broadcast(128), r=["thr_d"], w=["thrb"])
            for b in range(NB):
                seg = 0 if b < NLB else 1
                self.tt(maskf[:, b, :], afftok[:, b, :], thrb[:, seg, :], ALU.is_gt, r=[("aff", b), "thrb"], w=[("mask", b)])
                self.cp(mask[:, b, :], maskf[:, b, :], r=[("mask", b)], w=[("maskb", b)])
                self.tt(gm[:, b, :], afftok[:, b, :], maskf[:, b, :], ALU.mult, r=[("aff", b), ("mask", b)], w=[("gm", b)], eng="dve")
            for seg, blocks in enumerate((list(range(NLB)), list(range(NLB, NB)))):
                for bi, b in enumerate(blocks):
                    for bj in range(bi + 1):
                        self.mm(sp[:, :], (us if bj == bi else ones)[:], mask[:, blocks[bj], :], start=(bj == 0), stop=(bj == bi),
                                r=[("maskb", blocks[bj]), ("sb", us.name), "ones_bf"], w=["rsp"], inc=(bj == bi))
                    self.cp(slot[:, b, :], sp[:, :], r=["rsp"], w=[("slot", b)])
            it = 0
            for e in range(NE):
                for seg, (blocks, cap, col0) in enumerate(((list(range(NLB)), cfg.capl, self.col0(sm, 0)),
                                                           (list(range(NLB, NB)), cfg.capc, self.col0(sm, 1)))):
                    sl, slk = sel[it % 2], ("sel", it % 2)
                    x_, xk = xg[it % 2], ("xg", it % 2)
                    it += 1
                    for bi, b in enumerate(blocks):
                        self.ts(sl[:, bi, :cap], iota[:, :cap], slot[:, b, e:e + 1], maskf[:, b, e:e + 1], ALU.is_equal, ALU.mult,
                                r=[("slot", b), ("mask", b), ("sb", iota.name)], w=[slk])
                    for j in range(8):
                        g, gk = gps[j % 2], ("gps", j % 2)
                        for bi, b in enumerate(blocks):
                            self.mm(g[:, :cap], h2tok[:, b, j * 128:(j + 1) * 128], sl[:, bi, :cap], start=(bi == 0), stop=(bi == len(blocks) - 1),
                                    r=[("h2tok", b), slk], w=[gk], inc=(bi == len(blocks) - 1))
                        self.cp(x_[:, j, :cap], g[:, :cap], r=[gk], w=[xk], eng=("act" if j % 2 else "dve"))
                    self.ld(self.dr["xin"][e].rearrange("(k p) c -> p k c", p=128)[:, :, col0:col0 + cap], x_[:, :, :cap], r=[xk], w=[("xin", e, sm, seg)])
            self.ld(self.dr["slotd"][sm], slot[:].rearrange("p b e -> p (b e)"), r=[("slot", b) for b in range(NB)], w=[("slotd", sm)])
            self.ld(self.dr["gmd"][sm], gm[:].rearrange("p b e -> p (b e)"), r=[("gm", b) for b in range(NB)], w=[("gmd", sm)])
            self.s.barrier()

    def phase_ffn(self, l):
        for grp in range(self.cfg.SPC // self.cfg.G):
            self.go() and self.phase_ffn_g(l, grp)

    def phase_ffn_g(self, l, grp):
        cfg = self.cfg
        NE, FC, cols, D = cfg.NE, cfg.FC, cfg.gcols, cfg.D
        gb = grp * cfg.gcols
        main = min(cols, 512)
        csplit = [(0, main)] + ([(main, cols - main)] if cols > main else [])
        cchunks = [(c, min(128, cols - c)) for c in range(0, cols, 128)]
        nq = 4 if FC >= 8 else 2
        FH = (FC + nq - 1) // nq
        units = [(f0, min(FH, FC - f0)) for f0 in range(0, FC, FH)]
        with contextlib.ExitStack() as st2:
            NU = 6
            wb = [self.sb(st2, f"wb{i}", [128, 8, FH * 128], BF16) for i in range(NU)]
            NWD = 3
            wdb = [self.sb(st2, f"wdb{i}", [128, FC, 512], BF16) for i in range(NWD)]
            xin = [self.sb(st2, f"fxin{i}", [128, 8, cols], BF16) for i in range(2)]
            actT = self.sb(st2, "actT", [128, FC, cols], BF16)
            sa = [self.sb(st2, f"sa{i}", [128, cols], F32) for i in range(2)]
            ysb = [self.sb(st2, f"ysb{i}", [128, len(cchunks), 512], BF16) for i in range(2)]
            aps = [self.ps(st2, f"aps{i}", [128, 512]) for i in range(2)]
            ups = [self.ps(st2, f"ups{i}", [128, 512]) for i in range(2)]
            cps = self.ps(st2, "cps", [128, 2, 256])
            yps = [self.ps(st2, f"yps{i}", [128, 512]) for i in range(len(cchunks))] if len(cchunks) <= 3 else None
            if yps is None:
                yps = aps + ups + [self.ps(st2, "yps4", [128, 512])]
                ypk = [("aps", 0), ("aps", 1), ("ups", 0), ("ups", 1), ("yps", 4)]
            else:
                ypk = [("yps", i) for i in range(len(cchunks))]
            ui = 0
            di = 0
            fi = 0
            for e in range(NE):
                x_, xk = xin[e % 2], ("fxin", e % 2)
                self.ld(x_[:], self.dr["xin"][e].rearrange("(k p) c -> p k c", p=128)[:, :, gb:gb + cols], r=[("xin", e, sm, sg) for sm in range(cfg.SPC) for sg in range(2)], w=[xk])
                for (f0, nf) in units:
                    wgt, wgk = wb[ui % NU], ("wb", ui % NU)
                    ui += 1
                    wut, wuk = wb[ui % NU], ("wb", ui % NU)
                    ui += 1
                    for (wt, wk, name) in ((wgt, wgk, "wg"), (wut, wuk, "wu")):
                        src = self.dr[name][l, e].rearrange("(k p) f -> p k f", p=128)
                        for k in range(0, 8, 4):
                            self.ld(wt[:, k:k + 4, :nf * 128], src[:, k:k + 4, f0 * 128:(f0 + nf) * 128], r=[], w=[wk], q="pool")
                    for f in range(nf):
                        a, ak = aps[fi % 2], ("aps", fi % 2)
                        u, uk = ups[fi % 2], ("ups", fi % 2)
                        s_, sk = sa[fi % 2], ("sa", fi % 2)
                        fi += 1
                        for (ps_, pk, wt, wk, ci) in ((a, ak, wgt, wgk, 0), (u, uk, wut, wuk, 1)):
                            for (cc0, cn) in csplit:
                                tgt = ps_[:, :cn] if cc0 == 0 else cps[:, ci, :cn]
                                tk = pk if cc0 == 0 else "cps"
                                for k in range(8):
                                    self.mm(tgt, wt[:, k, f * 128:(f + 1) * 128], x_[:, k, cc0:cc0 + cn], start=(k == 0), stop=(k == 7),
                                            r=[wk, xk], w=[tk], inc=(k == 7))
                        for (cc0, cn) in csplit:
                            asrc = a[:, :cn] if cc0 == 0 else cps[:, 0, :cn]
                            usrc = u[:, :cn] if cc0 == 0 else cps[:, 1, :cn]
                            akk = ak if cc0 == 0 else "cps"
                            ukk = uk if cc0 == 0 else "cps"
                            self.act(s_[:, cc0:cc0 + cn], asrc, AF.Silu, r=[akk], w=[sk])
                            self.tt(actT[:, f0 + f, cc0:cc0 + cn], s_[:, cc0:cc0 + cn], usrc, ALU.mult, r=[sk, ukk], w=["actT"])
                for dh in range(D // 512):
                    wd_, wdk = wdb[di % NWD], ("wdb", di % NWD)
                    y_, yk = ysb[di % 2], ("ysb", di % 2)
                    di += 1
                    src = self.dr["wd"][l, e].rearrange("(f p) d -> p f d", p=128)
                    step = 4
                    for f in range(0, FC, step):
                        nf_ = min(step, FC - f)
                        self.ld(wd_[:, f:f + nf_, :], src[:, f:f + nf_, dh * 512:(dh + 1) * 512], r=[], w=[wdk], q="pool")
                    for f in range(FC):
                        for ci, (cc0, cn) in enumerate(cchunks):
                            self.mm(yps[ci][:cn, :], actT[:, f, cc0:cc0 + cn], wd_[:, f, :], start=(f == 0), stop=(f == FC - 1),
                                    r=["actT", wdk], w=[ypk[ci]], inc=(f == FC - 1))
                    for ci, (cc0, cn) in enumerate(cchunks):
                        self.cp(y_[:cn, ci, :], yps[ci][:cn, :], r=[ypk[ci]], w=[yk], eng=("act" if ci % 2 else "dve"))
                    for ci, (cc0, cn) in enumerate(cchunks):
                        self.ld(self.dr["yexp"][e, gb + cc0:gb + cc0 + cn, dh * 512:(dh + 1) * 512], y_[:cn, ci, :], r=[yk], w=[("yexp", e)])
            self.s.barrier()

    def phase_scatter(self, l, sm, slot_unused, gm_unused):
        cfg = self.cfg
        NE, NLB, NB, D = cfg.NE, cfg.NLB, cfg.NB, cfg.D
        with contextlib.ExitStack() as st2:
            slot = self.sb(st2, "sslot", [128, NB, NE], F32)
            gm = self.sb(st2, "sgm", [128, NB, NE], F32)
            self.ld(slot[:].rearrange("p b e -> p (b e)"), self.dr["slotd"][sm], r=[], w=[("slot", sm)])
            self.ld(gm[:].rearrange("p b e -> p (b e)"), self.dr["gmd"][sm], r=[], w=[("gm", sm)])
            iota = self.load_const(st2, "iota", [128, 256])
            segs = []
            for seg, (cap, col0) in enumerate(((cfg.capl, self.col0(sm, 0)), (cfg.capc, self.col0(sm, 1)))):
                ck = min(128, cap)
                ncb = cap // ck
                y = self.sb(st2, f"sy{seg}", [ck, NE * ncb, D], BF16)
                for e in range(NE):
                    for cb in range(ncb):
                        self.ld(y[:, e * ncb + cb, :], self.dr["yexp"][e, col0 + cb * ck:col0 + (cb + 1) * ck, :], r=[("yexp", e)], w=[("sy", seg)])
                segs.append((cap, ck, ncb, y))
            mxi = max(NE * sg[2] for sg in segs)
            selT = self.sb(st2, "selT", [128, mxi, 512], BF16)
            sg_ = [self.sb(st2, f"sg{i}", [128, 256], BF16) for i in range(2)]
            tps = [self.ps(st2, f"stp{i}", [128, 8, 128], BF16) for i in range(2)]
            yT = [self.ps(st2, f"syT{i}", [128, 512]) for i in range(4)]
            xts = [self.sb(st2, f"sxt{i}", [128, 8, 512], F32) for i in range(2)]
            xsrc = self.dr["xT"][sm].rearrange("(k p) t -> p k t", p=128)
            it = 0
            gi = 0
            for ti, (c0, w, seg) in enumerate(cfg.tiles):
                cap, ck, ncb, y = segs[seg]
                v = cfg.SPC if seg == 1 else sm
                nidx = NE * ncb
                xt, xk = xts[ti % 2], ("sxt", ti % 2)
                self.ld(xt[:, :, :w], xsrc[:, :, c0:c0 + w], r=[("xT", sm)], w=[xk])
                for bb in range(w // 128):
                    b = c0 // 128 + bb
                    for g0 in range(0, nidx, 8):
                        tp, tk = tps[gi % 2], ("stp", gi % 2)
                        gi += 1
                        ng = min(8, nidx - g0)
                        for q in range(ng):
                            idx = g0 + q
                            e, cb = idx // ncb, idx % ncb
                            if cb == 0:
                                sg, sgk = sg_[it % 2], ("sg", it % 2)
                                it += 1
                                self.ts(sg[:, :cap], iota[:, :cap], slot[:, b, e:e + 1], gm[:, b, e:e + 1], ALU.is_equal, ALU.mult,
                                        r=[("slot", sm), ("gm", sm), ("sb", iota.name)], w=[sgk])
                            self.tr(tp[:ck, q, :], sg[:, cb * ck:(cb + 1) * ck], self.ident_bf[:], r=[sgk, "ident_bf"], w=[tk], inc=True)
                        self.cp(selT[:ck, g0:g0 + ng, bb * 128:(bb + 1) * 128], tp[:ck, :ng, :], r=[tk], w=["selT"], eng=("act" if gi % 2 else "dve"))
                for jg in range(2):
                    for idx in range(nidx):
                        for jj in range(4):
                            j = jg * 4 + jj
                            self.mm(yT[jj][:, :w], y[:ck, idx, j * 128:(j + 1) * 128], selT[:ck, idx, :w], start=(idx == 0), stop=(idx == nidx - 1),
                                    r=[("sy", seg), "selT"], w=[("syT", jj)], inc=(idx == nidx - 1))
                    for jj in range(4):
                        j = jg * 4 + jj
                        self.stt(xt[:, j, :w], yT[jj][:, :w], self.P(l, 5, j, v), xt[:, j, :w], ALU.mult, ALU.add, r=[("syT", jj), xk, "par"], w=[xk])
                self.ld(xsrc[:, :, c0:c0 + w], xt[:, :, :w], r=[xk], w=[("xT", sm)])
            self.s.barrier()

    def phase_final(self):
        cfg = self.cfg
        with contextlib.ExitStack() as st2:
            nb = self.nm_bufs(st2)
            xts = [self.sb(st2, f"fxt{i}", [128, 8, 512], F32) for i in range(2)]
            ots = [self.sb(st2, f"fot{i}", [128, 8, 512], F32) for i in range(2)]
            it = 0
            for sm in range(cfg.SPC):
                xsrc = self.dr["xT"][sm].rearrange("(k p) t -> p k t", p=128)
                osrc = self.out[sm].rearrange("(k p) t -> p k t", p=128)
                for ti, (c0, w, seg) in enumerate(cfg.tiles):
                    if seg == 1:
                        continue
                    xt, xk = xts[it % 2], ("fxt", it % 2)
                    ot, ok = ots[it % 2], ("fot", it % 2)
                    it += 1
                    self.ld(xt[:, :, :w], xsrc[:, :, c0:c0 + w], r=[("xT", sm)], w=[xk])
                    self.norm_mod(xt, xk, w, nb, lambda k: self.fnw[:, k:k + 1], None, lambda k: ot[:, k, :w], ok)
                    self.ld(osrc[:, :, c0:c0 + w], ot[:, :, :w], r=[ok], w=[("out", sm)])
            self.s.barrier()

    def go(self):
        self.ph += 1
        return self.ph <= self.stop

    def build(self):
        cfg = self.cfg
        self.declare()
        self.pcol = self.sb(self.es, "pcol", [128, 64], F32)
        import os
        self.stop = int(os.environ.get('KSTOP', '999'))
        self.ph = 0
        self.setup()
        NB, NE = cfg.NB, cfg.NE
        slot = [None] * cfg.SPC
        gm = [None] * cfg.SPC
        for l in range(cfg.DEPTH):
            odd = (l % 2 == 1)
            i = l // 2
            if odd:
                self.go() and self.layer_params_odd(l)
            else:
                self.go() and self.layer_params_even(l)
            for sm in range(cfg.SPC):
                with contextlib.ExitStack() as st:
                    vt = self.sb(st, "vt", [128, NB, 512 if odd else 640], BF16)
                    if odd:
                        self.recst = {"xstok": self.sb(st, "xstok", [128, NB, 512], BF16), "dttok": self.sb(st, "dttok", [128, NB, 16], F32),
                                      "ncum": self.sb(st, "ncum", [128, NB, 16], F32)}
                    with contextlib.ExitStack() as st1:
                        hT = self.phase_norm1(st1, l, sm) if self.go() else None
                        if odd:
                            self.go() and self.rec_inproj(st1, l, sm, hT, vt)
                        else:
                            lp = self.lp
                            rows = [("normrope", j, lp["qn"], j) for j in range(4)] + [("normrope", 4, lp["kn"], 4)]
                            rows += [("rope", 6 + j, 1.0, 5 + j) for j in range(4)] + [("rope", 10 + j, 1.0, 9 + j) for j in range(4)]
                            self.go() and self.phase_inproj(st1, l, sm, hT, "att_w_in", i, cfg.ATT_IN, rows, [(5 * 128, 128, 0), (14 * 128, 512, 128)], vt)
                    if odd:
                        self.go() and self.phase_rec(l, sm, vt)
                    else:
                        self.go() and self.phase_attn(l, sm, vt)
                with contextlib.ExitStack() as st:
                    h2tok = self.sb(st, "h2tok", [128, NB, cfg.D], BF16)
                    afftok = self.sb(st, "afftok", [128, NB, NE], F32)
                    slot[sm] = self.sb(st, "slot", [128, NB, NE], F32)
                    gm[sm] = self.sb(st, "gm", [128, NB, NE], F32)
                    self.go() and self.phase_outproj(st, l, sm, "rec_w_out" if odd else "att_w_out", i, h2tok, afftok, odd)
                    self.go() and self.phase_route(l, sm, h2tok, afftok, slot[sm], gm[sm])
            self.phase_ffn(l)
            for sm in range(cfg.SPC):
                self.go() and self.phase_scatter(l, sm, slot[sm], gm[sm])
        self.go() and self.phase_final()
        self.es.close()
        return self.nc


def _prep(cfg, inp):
    f = np.float32
    L, SPC = cfg.DEPTH, cfg.SPC

    def colT(a):
        a = np.asarray(a, f)
        sh = a.shape
        a = a.reshape(sh[:-1] + (sh[-1] // 128, 128))
        return np.ascontiguousarray(np.moveaxis(a, -1, 0))

    def rep(a, n=128):
        a = np.asarray(a, f)
        return np.ascontiguousarray(np.broadcast_to(a[None], (n,) + a.shape))
    shared = {}
    shared["ada_w"] = np.asarray(inp["ada_w"], f)
    shared["ada_bT"] = colT(inp["ada_b"])
    shared["n1w"] = colT(inp["norm1_w"]); shared["n2w"] = colT(inp["norm2_w"]); shared["fnw"] = colT(inp["final_norm_w"])
    shared["att_w_in"] = np.asarray(inp["att_w_in"], f); shared["att_w_out"] = np.asarray(inp["att_w_out"], f)
    shared["qnw"] = np.ascontiguousarray(np.tile(np.asarray(inp["att_q_norm_w"], f), (1, 2)).T)
    shared["knw"] = np.ascontiguousarray(np.tile(np.asarray(inp["att_k_norm_w"], f), (1, 2)).T)
    shared["dlam"] = rep(np.asarray(inp["diff_lambda"], f).reshape(-1, 256))
    shared["dnw"] = np.ascontiguousarray(np.asarray(inp["diff_norm_w"], f).T)
    if L >= 2:
        shared["rec_w_in"] = np.asarray(inp["rec_w_in"], f); shared["rec_w_out"] = np.asarray(inp["rec_w_out"], f)
        shared["rdl"] = rep(np.asarray(inp["ret_decay_logit"], f).reshape(-1, 8))
        shared["cwT"] = np.ascontiguousarray(np.moveaxis(colT(np.moveaxis(np.asarray(inp["ssd_conv_w"], f), 1, 0)), 1, -1))
        shared["cbT"] = colT(inp["ssd_conv_b"])
        shared["dtb"] = np.ascontiguousarray(np.asarray(inp["ssd_dt_bias"], f).reshape(-1, 16).T)
        shared["alog"] = np.ascontiguousarray(np.asarray(inp["ssd_a_log"], f).reshape(-1, 16).T)
        shared["alogr"] = rep(np.asarray(inp["ssd_a_log"], f).reshape(-1, 16))
        shared["dsk"] = np.ascontiguousarray(np.moveaxis(np.repeat(np.asarray(inp["ssd_d_skip"], f), 64, axis=-1).reshape(-1, 4, 128), -1, 0))
        shared["snw"] = colT(inp["ssd_norm_w"])
    else:
        z = lambda *s: np.zeros(s, f)
        shared.update(rec_w_in=z(1, cfg.D, cfg.REC_IN), rec_w_out=z(1, cfg.D, cfg.D), rdl=z(128, 1, 8), cwT=z(128, 1, 8, 3), cbT=z(128, 1, 8),
                      dtb=z(16, 1), alog=z(16, 1), alogr=z(128, 1, 16), dsk=z(128, 1, 4), snw=z(128, 1, 4))
    shared["router_w"] = np.asarray(inp["router_w"], f)
    shared["wg"] = np.asarray(inp["expert_w_gate"], f); shared["wu"] = np.asarray(inp["expert_w_up"], f); shared["wd"] = np.asarray(inp["expert_w_down"], f)
    shared.update(_consts(cfg))
    x, ctx, c = np.asarray(inp["x"], f), np.asarray(inp["ctx"], f), np.asarray(inp["c"], f)
    cc = np.asarray(inp["c_ctx"], f)
    maps = []
    for core in range(cfg.NCORES):
        b0 = core * SPC
        m = dict(shared)
        m["xT0"] = np.ascontiguousarray(np.concatenate([x[b0:b0 + SPC], ctx[b0:b0 + SPC]], axis=1).transpose(0, 2, 1))
        cv = np.concatenate([c[b0:b0 + SPC], cc[None]], 0)
        m["cT"] = np.ascontiguousarray(cv.reshape(SPC + 1, 8, 128).transpose(2, 1, 0))
        maps.append(m)
    return maps


def run(cfg, inp):
    prog = Prog(cfg)
    nc = prog.build()
    maps = _prep(cfg, inp)
    res = run_bass_kernel_spmd(nc, maps, core_ids=list(range(cfg.NCORES)))
    outs = [np.asarray(r["outT"]).transpose(0, 2, 1) for r in res.results]
    return np.ascontiguousarray(np.concatenate(outs, 0)).astype(np.float32)


def kernel(**inputs):
    cfg = Cfg(SPC=2)
    return run(cfg, inputs)


def _layer_params_odd(self, l):
    i = l // 2
    pc = self.pcol
    lp = {"lg": pc[:, 8:16], "neglg": pc[:, 16:24], "dtb": pc[:16, 24:25], "aneg": pc[:16, 25:26], "isF": pc[:16, 26:27], "isB": pc[:16, 27:28],
          "one": pc[:, 63:64], "dsk": pc[:, 32:36], "snw": pc[:, 36:40]}
    if not hasattr(self, "cw"):
        self.cw = self.sb(self.es, "cw", [128, 8, 3], F32)
        self.cb = self.sb(self.es, "cb", [128, 8], F32)
    V = self.nc.vector
    self.s.op("dve", lambda: V.memset(pc[:, 63:64], 1.0), r=[], w=["pcols"])
    self.s.op("dve", lambda: V.memset(pc[:16, 26:27], 0.0), r=[], w=["pcols"])
    self.s.op("dve", lambda: V.memset(pc[:8, 26:27], 1.0), r=[], w=["pcols"])
    self.s.op("dve", lambda: V.memset(pc[:16, 27:28], 1.0), r=[], w=["pcols"])
    self.s.op("dve", lambda: V.memset(pc[:8, 27:28], 0.0), r=[], w=["pcols"])
    self.ld(pc[:, 8:16], self.dr["rdl"][:, i, :], r=[], w=["pcols"])
    self.ld(pc[:16, 24:25], self.dr["dtb"][:, i:i + 1], r=[], w=["pcols"])
    self.ld(pc[:16, 25:26], self.dr["alog"][:, i:i + 1], r=[], w=["pcols"])
    self.ld(pc[:, 32:36], self.dr["dsk"][:, i, :], r=[], w=["pcols"])
    self.ld(pc[:, 36:40], self.dr["snw"][:, i, :], r=[], w=["pcols"])
    self.ld(self.cw[:], self.dr["cwT"][:, i], r=[], w=["pcols"])
    self.ld(self.cb[:], self.dr["cbT"][:, i, :], r=[], w=["pcols"])
    self.act(pc[:, 16:24], pc[:, 8:16], AF.Exp, scale=-1.0, r=["pcols"], w=["pcols"])
    self.act(pc[:, 16:24], pc[:, 16:24], AF.Ln, bias=pc[:, 63:64], r=["pcols"], w=["pcols"])
    self.ts(pc[:, 8:16], pc[:, 16:24], -1.0, None, ALU.mult, r=["pcols"], w=["pcols"])
    self.act(pc[:16, 25:26], pc[:16, 25:26], AF.Exp, r=["pcols"], w=["pcols"])
    self.ts(pc[:16, 25:26], pc[:16, 25:26], -1.0, None, ALU.mult, r=["pcols"], w=["pcols"])
    self.ts(pc[:, 36:40], pc[:, 36:40], 16.0, None, ALU.mult, r=["pcols"], w=["pcols"])
    self.s.barrier()
    self.lp = lp


def _rec_inproj(self, st1, l, sm, hT, vt):
    cfg = self.cfg
    i = l // 2
    rows = [("rope", 0, 1.0, 0), ("rope", 1, 1.0, 1), ("rope", 2, 0.125, 2), ("rope", 3, 0.125, 3)]
    rows += [("side", 8 + c, None, c) for c in range(4)] + [("side", 12 + c, None, 4 + c) for c in range(4)]
    rows += [("conv", 16 + c, (self.cw, self.cb, c), ("xs", 8 + c)) for c in range(4)]
    rows += [("conv", 20 + c, (self.cw, self.cb, 4 + c), ("qk", 4 + c)) for c in range(4)]
    rows += [("dt", 24, None, None)]
    self.phase_inproj(st1, l, sm, hT, "rec_w_in", i, cfg.REC_IN, rows, [(4 * 128, 512, 0)], vt)


def _rec_dt(self, st2, l, sm, fr, fk, ssp, gr, gk, Cb, Ck, Sb, Sk):
    cfg = self.cfg
    T, NL, NC, NB = cfg.T, cfg.NL, cfg.NC, cfg.NB
    lp, rs = self.lp, self.recst
    with contextlib.ExitStack() as st:
        dtT, la, pa, pb = fr[:16, :], gr[:16, :], Cb[:16, :], Sb[:16, :]
        cF = self.sb(st, "cF", [16, T], F32)
        self.act(dtT[:], fr[:16, :], AF.Exp, bias=lp["dtb"], r=[fk, "pcols"], w=[fk])
        self.act(dtT[:], dtT[:], AF.Ln, bias=lp["one"][:16], r=[fk, "pcols"], w=[fk])
        self.ts(la[:], dtT[:], lp["aneg"], None, ALU.mult, r=[fk, "pcols"], w=[gk])
        cur, ck_ = la, gk
        bufs = [(pa, Ck), (pb, Sk)]
        bi = 0
        sh = 1
        while sh < max(NL, NC):
            nxt, nk = bufs[bi % 2]
            bi += 1
            for (s0, n) in ((0, NL), (NL, NC)):
                if sh < n:
                    self.tt(nxt[:, s0 + sh:s0 + n], cur[:, s0 + sh:s0 + n], cur[:, s0:s0 + n - sh], ALU.add, r=[ck_], w=[nk])
                    self.cp(nxt[:, s0:s0 + sh], cur[:, s0:s0 + sh], r=[ck_], w=[nk])
                else:
                    self.cp(nxt[:, s0:s0 + n], cur[:, s0:s0 + n], r=[ck_], w=[nk])
            cur, ck_ = nxt, nk
            sh *= 2
        P, Pk = cur, ck_
        oth, ok_ = bufs[bi % 2]
        totc, totl = P[:, T - 1:T], P[:, NL - 1:NL]
        self.ts(cF[:, 0:NL], P[:, 0:NL], totc, None, ALU.add, r=[Pk], w=["cF"])
        self.cp(cF[:, NL:T], P[:, NL:T], r=[Pk], w=["cF"])
        self.tt(oth[:], la[:], P[:], ALU.subtract, r=[gk, Pk], w=[ok_])
        self.ts(oth[:, 0:NL], oth[:, 0:NL], totc, totl, ALU.add, ALU.add, r=[ok_, Pk], w=[ok_])
        self.ts(oth[:, NL:T], oth[:, NL:T], totc, None, ALU.add, r=[ok_, Pk], w=[ok_])
        self.ts(cF[:], cF[:], lp["isF"], None, ALU.mult, r=["cF", "pcols"], w=["cF"])
        self.stt(cF[:], oth[:], lp["isB"], cF[:], ALU.mult, ALU.add, r=[ok_, "cF", "pcols"], w=["cF"])
        self.ld(self.dr["cumT"][sm], cF[:], r=["cF"], w=[("cumT", sm)])
        self.ts(la[:], cF[:], -1.0, None, ALU.mult, r=["cF"], w=[gk])
        for b in range(NB):
            self.tr(ssp[:, 0:16], dtT[:, b * 128:(b + 1) * 128], self.ident_f[:16, :16], r=[fk, "ident_f"], w=["ssp"])
            self.cp(rs["dttok"][:, b, :], ssp[:, 0:16], r=["ssp"], w=[("dttok", b)])
            self.tr(ssp[:, 16:32], la[:, b * 128:(b + 1) * 128], self.ident_f[:16, :16], r=[gk, "ident_f"], w=["ssp"])
            self.cp(rs["ncum"][:, b, :], ssp[:, 16:32], r=["ssp"], w=[("ncum", b)])


Prog.layer_params_odd = _layer_params_odd
Prog.rec_inproj = _rec_inproj
Prog.rec_dt = _rec_dt


def _vis(tf_s, tf_t):
    if tf_s.max() <= tf_t.min():
        return "full"
    if tf_s.min() > tf_t.max():
        return "none"
    return "part"


def _phase_rec(self, l, sm, vt):
    cfg = self.cfg
    T, NB, NL, NC = cfg.T, cfg.NB, cfg.NL, cfg.NC
    lp, rs = self.lp, self.recst
    cst = _consts(cfg)
    TF, TB = cst["tfrow"][0], cst["tbrow"][0]
    with contextlib.ExitStack() as st2:
        tfrow = self.load_const(st2, "tfrow", [128, T])
        tbrow = self.load_const(st2, "tbrow", [128, T])
        tfcol = self.load_const(st2, "tfcol", [128, NB])
        tbcol = self.load_const(st2, "tbcol", [128, NB])
        maskF = self.load_const(st2, "maskF", [128, 4, 512])
        maskB = self.load_const(st2, "maskB", [128, 4, 512])
        ckeys = [("sb", t.name) for t in (tfrow, tbrow, tfcol, tbcol, maskF, maskB)]
        vssd = self.sb(st2, "vssd", [128, NB, 16, 64], BF16)
        for b in range(NB):
            for dh in range(16):
                H = dh % 8
                self.ts(vssd[:, b, dh, :], rs["xstok"][:, b, H * 64:(H + 1) * 64], rs["dttok"][:, b, dh:dh + 1], None, ALU.mult,
                        r=[("xstok", b), ("dttok", b)], w=[("vssd", b)])
        KT = [self.sb(st2, f"rKT{i}", [128, T], BF16) for i in range(2)]
        QT = [self.sb(st2, f"rQT{i}", [128, T], BF16) for i in range(2)]
        nbF = self.sb(st2, "nbF", [128, NB], F32)
        nbB = self.sb(st2, "nbB", [128, NB], F32)
        Sps = [self.ps(st2, f"rS{i}", [128, 512]) for i in range(2)]
        Ops = [self.ps(st2, f"rO{i}", [128, 512]) for i in range(4)]
        Dt = [self.sb(st2, f"rD{i}", [128, 512], F32) for i in range(4)]
        pre = [self.sb(st2, f"rpre{i}", [128, 512], F32) for i in range(2)]
        pt = [self.sb(st2, f"rpt{i}", [128, 512], BF16) for i in range(3)]
        osb = [self.sb(st2, f"rosb{i}", [128, 512], F32) for i in range(2)]
        crow = [self.sb(st2, f"crow{i}", [128, 512], F32) for i in range(8)]
        cnt = {"s": 0, "d": 0, "p": 0, "pt": 0, "o": 0}

        def decay(row_ap, rowkey, scale, bias, mode, mask_ap):
            d, dk = Dt[cnt["d"] % 4], ("rD", cnt["d"] % 4)
            cnt["d"] += 1
            src, sk = row_ap, rowkey
            if mode == "part":
                p_, pk_ = pre[cnt["p"] % 2], ("rpre", cnt["p"] % 2)
                cnt["p"] += 1
                if scale is None:
                    self.tt(p_[:, :mask_ap.shape[1]], row_ap, mask_ap, ALU.add, r=[rowkey] + ckeys, w=[pk_])
                else:
                    self.stt(p_[:, :mask_ap.shape[1]], row_ap, scale, mask_ap, ALU.mult, ALU.add, r=[rowkey, "pcols"] + ckeys, w=[pk_])
                src, sk = p_[:, :mask_ap.shape[1]], pk_
                self.act(d[:, :mask_ap.shape[1]], src, AF.Exp, bias=bias, r=[sk, "nb", "pcols"] + [("ncum", b_) for b_ in range(NB)], w=[dk])
            else:
                wdt = row_ap.shape[1]
                if scale is None:
                    self.act(d[:, :wdt], src, AF.Exp, bias=bias, r=[sk, "nb"] + [("ncum", b_) for b_ in range(NB)], w=[dk])
                else:
                    self.act(d[:, :wdt], src, AF.Exp, bias=bias, scale=scale, r=[sk, "nb", "pcols"], w=[dk])
            return d, dk

        def classify(c0, w, kb):
            tt_f, tt_b = TF[c0:c0 + w], TB[c0:c0 + w]
            ts_f, ts_b = TF[kb * 128:(kb + 1) * 128], TB[kb * 128:(kb + 1) * 128]
            vf, vb = _vis(ts_f, tt_f), _vis(ts_b, tt_b)
            j = (kb * 128 - c0) // 128
            return vf, vb, j

        for h in range(4):
            K, Kk = KT[h % 2], ("rKT", h % 2)
            Q, Qk = QT[h % 2], ("rQT", h % 2)
            self.ld(K[:64, :], self.dr["qk"][sm, 256 + h * 64:256 + (h + 1) * 64, :], r=[("qk", sm, 2 + h // 2)], w=[Kk])
            self.ld(Q[:64, :], self.dr["qk"][sm, h * 64:(h + 1) * 64, :], r=[("qk", sm, h // 2)], w=[Qk])
            self.ts(nbF[:], tfcol[:], lp["neglg"][:, h:h + 1], None, ALU.mult, r=ckeys + ["pcols"], w=["nb"])
            self.ts(nbB[:], tbcol[:], lp["neglg"][:, 4 + h:5 + h], None, ALU.mult, r=ckeys + ["pcols"], w=["nb"])
            for ti, (c0, w, seg) in enumerate(cfg.tiles):
                O, Ok = Ops[cnt["o"] % 4], ("rO", cnt["o"] % 4)
                ob_, obk = osb[cnt["o"] % 2], ("rosb", cnt["o"] % 2)
                cnt["o"] += 1
                contrib = []
                for kb in range(NB):
                    vf, vb, j = classify(c0, w, kb)
                    if vf != "none" or vb != "none":
                        contrib.append((kb, vf, vb, j))
                kb0 = contrib[0][0]
                self.mm(Sps[cnt["s"] % 2][:, :w], K[:64, kb0 * 128:(kb0 + 1) * 128], Q[:64, c0:c0 + w], True, True, r=[Kk, Qk], w=[("rS", cnt["s"] % 2)])
                for n, (kb, vf, vb, j) in enumerate(contrib):
                    S, Sk = Sps[cnt["s"] % 2], ("rS", cnt["s"] % 2)
                    cnt["s"] += 1
                    if n + 1 < len(contrib):
                        kn = contrib[n + 1][0]
                        self.mm(Sps[cnt["s"] % 2][:, :w], K[:64, kn * 128:(kn + 1) * 128], Q[:64, c0:c0 + w], True, True, r=[Kk, Qk], w=[("rS", cnt["s"] % 2)])
                    ds = []
                    if vf != "none":
                        ds.append(decay(tfrow[:, c0:c0 + w], ckeys[0], lp["lg"][:, h:h + 1], nbF[:, kb:kb + 1], vf, maskF[:, j, :w] if vf == "part" else None))
                    if vb != "none":
                        ds.append(decay(tbrow[:, c0:c0 + w], ckeys[1], lp["lg"][:, 4 + h:5 + h], nbB[:, kb:kb + 1], vb, maskB[:, j, :w] if vb == "part" else None))
                    d, dk = ds[0]
                    if len(ds) == 2:
                        self.tt(d[:, :w], d[:, :w], ds[1][0][:, :w], ALU.add, r=[dk, ds[1][1]], w=[dk])
                    p, pk = pt[cnt["pt"] % 3], ("rpt", cnt["pt"] % 3)
                    cnt["pt"] += 1
                    self.tt(p[:, :w], S[:, :w], d[:, :w], ALU.mult, r=[Sk, dk], w=[pk])
                    self.mm(O[:, :w], vt[:, kb, h * 128:(h + 1) * 128], p[:, :w], start=(n == 0), stop=(n == len(contrib) - 1),
                            r=[pk, ("vt", kb)], w=[Ok], inc=True)
                self.cp(ob_[:, :w], O[:, :w], r=[Ok], w=[obk], eng="act")
                self.ld(self.dr["rawT"][sm, h * 128:(h + 1) * 128, c0:c0 + w], ob_[:, :w], r=[obk], w=[("rawT", sm, ti)])
        for g in range(2):
            K, Kk = KT[g % 2], ("rKT", g % 2)
            Q, Qk = QT[g % 2], ("rQT", g % 2)
            self.ld(K[:], self.dr["qk"][sm, (4 + g) * 128:(5 + g) * 128, :], r=[("qk", sm, 4 + g)], w=[Kk])
            self.ld(Q[:], self.dr["qk"][sm, (6 + g) * 128:(7 + g) * 128, :], r=[("qk", sm, 6 + g)], w=[Qk])
            for ti, (c0, w, seg) in enumerate(cfg.tiles):
                for hh in range(4):
                    for d_ in range(2):
                        dh = d_ * 8 + g * 4 + hh
                        self.ld(crow[d_ * 4 + hh][:, :w], self.dr["cumT"][sm, dh, c0:c0 + w].partition_broadcast(128), r=[("cumT", sm)], w=[("crow", d_ * 4 + hh)])
                contrib = []
                for kb in range(NB):
                    vf, vb, j = classify(c0, w, kb)
                    if vf != "none" or vb != "none":
                        contrib.append((kb, vf, vb, j))
                ncontrib = sum((vf != "none") + (vb != "none") for (_, vf, vb, _) in contrib)
                seen = [0] * 4
                kb0 = contrib[0][0]
                self.mm(Sps[cnt["s"] % 2][:, :w], K[:, kb0 * 128:(kb0 + 1) * 128], Q[:, c0:c0 + w], True, True, r=[Kk, Qk], w=[("rS", cnt["s"] % 2)])
                for n, (kb, vf, vb, j) in enumerate(contrib):
                    S, Sk = Sps[cnt["s"] % 2], ("rS", cnt["s"] % 2)
                    cnt["s"] += 1
                    if n + 1 < len(contrib):
                        kn = contrib[n + 1][0]
                        self.mm(Sps[cnt["s"] % 2][:, :w], K[:, kn * 128:(kn + 1) * 128], Q[:, c0:c0 + w], True, True, r=[Kk, Qk], w=[("rS", cnt["s"] % 2)])
                    for hh in range(4):
                        for d_, (vis, msk) in enumerate(((vf, maskF), (vb, maskB))):
                            if vis == "none":
                                continue
                            dh = d_ * 8 + g * 4 + hh
                            d, dk = decay(crow[d_ * 4 + hh][:, :w], ("crow", d_ * 4 + hh), None, rs["ncum"][:, kb, dh:dh + 1], vis,
                                          msk[:, j, :w] if vis == "part" else None)
                            p, pk = pt[cnt["pt"] % 3], ("rpt", cnt["pt"] % 3)
                            cnt["pt"] += 1
                            self.tt(p[:, :w], S[:, :w], d[:, :w], ALU.mult, r=[Sk, dk], w=[pk])
                            self.mm(Ops[hh][:64, :w], vssd[:, kb, dh, :], p[:, :w], start=(seen[hh] == 0), stop=(seen[hh] == ncontrib - 1),
                                    r=[pk, ("vssd", kb)], w=[("rO", hh)], inc=True)
                            seen[hh] += 1
                for hh in range(4):
                    H = g * 4 + hh
                    ob_, obk = osb[hh % 2], ("rosb", hh % 2)
                    self.cp(ob_[:64, :w], Ops[hh][:64, :w], r=[("rO", hh)], w=[obk], eng="act")
                    self.ld(self.dr["rawT"][sm, 512 + H * 64:512 + (H + 1) * 64, c0:c0 + w], ob_[:64, :w], r=[obk], w=[("rawT", sm, ti)])
        self.s.barrier()


def _odd_bufs(self, st2):
    return {"raw": self.sb(st2, "oraw", [128, 8, 512], F32), "side": self.sb(st2, "oside", [128, 12, 512], F32),
            "sq": self.sb(st2, "osq", [128, 4, 512], BF16), "rs": self.sb(st2, "ors", [128, 512], F32),
            "sg": self.sb(st2, "osg", [128, 512], F32), "t": self.sb(st2, "ot", [128, 512], F32),
            "s2": self.sb(st2, "os2", [128, 4, 512], F32), "ssp": self.ps(st2, "otp", [128, 512])}


def _odd_pre(self, l, sm, ob, at, atk, c0, w):
    lp = self.lp
    raw, side, sq, rs_, sg, t, s2, ssp = (ob[k] for k in ("raw", "side", "sq", "rs", "sg", "t", "s2", "ssp"))
    rsrc = self.dr["rawT"][sm].rearrange("(k p) t -> p k t", p=128)
    ssrc = self.dr["sideT"][sm].rearrange("(k p) t -> p k t", p=128)
    self.ld(raw[:, :, :w], rsrc[:, :, c0:c0 + w], r=[("rawT", sm)], w=["oraw"])
    self.ld(side[:, :, :w], ssrc[:, :, c0:c0 + w], r=[("sideT", sm)], w=["oside"])
    for c in range(4):
        self.act(sq[:, 0, :w], raw[:, c, :w], AF.Square, r=["oraw"], w=["osq"])
        self.rstd([sq[:, 0, :w]], self.ones_bf[:], ssp[:, :w], rs_[:, :w], 128 * EPS, ["osq", "ones_bf"], ["otp"], ["ors"])
        self.act(sg[:, :w], side[:, c, :w], AF.Silu, r=["oside"], w=["osg"])
        self.tt(t[:, :w], raw[:, c, :w], rs_[:, :w], ALU.mult, r=["oraw", "ors"], w=["ot"])
        self.stt(at[:, c, :w], t[:, :w], math.sqrt(128.0), sg[:, :w], ALU.mult, ALU.mult, r=["ot", "osg"], w=[atk])
    for c in range(4):
        self.stt(t[:, :w], side[:, 8 + c, :w], lp["dsk"][:, c:c + 1], raw[:, 4 + c, :w], ALU.mult, ALU.add, r=["oside", "oraw", "pcols"], w=["ot"])
        self.act(sg[:, :w], side[:, 4 + c, :w], AF.Silu, r=["oside"], w=["osg"])
        self.tt(s2[:, c, :w], t[:, :w], sg[:, :w], ALU.mult, r=["ot", "osg"], w=["os2"])
        self.act(sq[:, c, :w], s2[:, c, :w], AF.Square, r=["os2"], w=["osq"])
    for gg in range(2):
        self.rstd([sq[:, 2 * gg, :w], sq[:, 2 * gg + 1, :w]], self.ones_bf[:], ssp[:, :w], rs_[:, :w], 256 * EPS, ["osq", "ones_bf"], ["otp"], ["ors"])
        for c in (2 * gg, 2 * gg + 1):
            self.stt(at[:, 4 + c, :w], s2[:, c, :w], lp["snw"][:, c:c + 1], rs_[:, :w], ALU.mult, ALU.mult, r=["os2", "ors", "pcols"], w=[atk])


Prog.phase_rec = _phase_rec
Prog.odd_bufs = _odd_bufs
Prog.odd_pre = _odd_pre
```
